# Optimizing a Trainium2 kernel written in Bass

```python
import math
import jax, jax.numpy as jnp
from jax import lax
import numpy as np

D_MODEL = 2048
BATCH = 16
SEQ = 2048
DEPTH = 1

HEAD_DIM = 64
D_A = D_MODEL // 2
D_B = D_MODEL - D_A
D_MIX = D_A + D_B
H_A = D_A // HEAD_DIM
KV_A = 2
G_A = H_A // KV_A
D_KV_A = KV_A * HEAD_DIM
H_B = D_B // HEAD_DIM
QKV_DIM = D_A + 2 * D_KV_A + 3 * D_B
WINDOW = 128
Q_BLOCK = 128
NUM_BUCKETS = 32
MAX_DISTANCE = 128
N_GROUPS = 4
EXPERTS_PER_GROUP = 8
N_EXPERTS = N_GROUPS * EXPERTS_PER_GROUP
TOP_K = 2
D_EXPERT = D_MODEL // 4
MOE_BLOCK = 128
ALPHA = (2.0 * DEPTH) ** 0.25
BETA = (8.0 * DEPTH) ** -0.25
ATTN_SCALE = 1.0 / math.sqrt(HEAD_DIM)
EPS = 1e-5
NEG_INF = -1e30

kernel_name = "hybrid_swa_sink_stickbreak_hmoe_deepnorm"


def layer_norm(x, g, b):
    xf = x.astype(jnp.float32)
    mu = jnp.mean(xf, axis=-1, keepdims=True)
    var = jnp.mean(jnp.square(xf - mu), axis=-1, keepdims=True)
    y = (xf - mu) * lax.rsqrt(var + EPS) * g.astype(jnp.float32) + b.astype(jnp.float32)
    return y.astype(x.dtype)


def rms_norm(x, g):
    xf = x.astype(jnp.float32)
    y = xf * lax.rsqrt(jnp.mean(jnp.square(xf), axis=-1, keepdims=True) + EPS)
    return (y * g.astype(jnp.float32)).astype(x.dtype)


def t5_bucket(dist):
    n = jnp.maximum(dist, 0)
    max_exact = NUM_BUCKETS // 2
    ratio = jnp.maximum(n, max_exact).astype(jnp.float32) / max_exact
    large = max_exact + (jnp.log(ratio) / math.log(MAX_DISTANCE / max_exact)
                         * (NUM_BUCKETS - max_exact)).astype(jnp.int32)
    large = jnp.minimum(large, NUM_BUCKETS - 1)
    return jnp.where(n < max_exact, n, large)


def sliding_sink_attention(q, k, v, sinks, rel_bias):
    B, S, _ = q.shape
    nb = S // WINDOW
    q = q.reshape(B, nb, WINDOW, KV_A, G_A, HEAD_DIM)
    k = k.reshape(B, nb, WINDOW, KV_A, HEAD_DIM)
    v = v.reshape(B, nb, WINDOW, KV_A, HEAD_DIM)
    pad = ((0, 0), (1, 0), (0, 0), (0, 0), (0, 0))
    kk = jnp.concatenate([jnp.pad(k, pad)[:, :-1], k], axis=2)
    vv = jnp.concatenate([jnp.pad(v, pad)[:, :-1], v], axis=2)
    scores = jnp.einsum('bnqhgd,bnkhd->bnhgqk', q, kk).astype(jnp.float32) * ATTN_SCALE
    qi = jnp.arange(WINDOW)[:, None]
    kj = jnp.arange(2 * WINDOW)[None, :]
    dist = qi + WINDOW - kj
    band = (dist >= 0) & (dist < WINDOW)
    not_first = (jnp.arange(nb)[:, None, None] > 0) | (kj >= WINDOW)[None]
    mask = band[None] & not_first
    bias = rel_bias[t5_bucket(dist)].astype(jnp.float32)
    bias = jnp.transpose(bias, (2, 0, 1)).reshape(KV_A, G_A, WINDOW, 2 * WINDOW)
    logits = jnp.where(mask[None, :, None, None], scores + bias, NEG_INF)
    sink = sinks.astype(jnp.float32).reshape(KV_A, G_A)[:, :, None, None]
    m = jnp.maximum(jnp.max(logits, axis=-1, keepdims=True), sink)
    p = jnp.exp(logits - m)
    probs = p / (jnp.sum(p, axis=-1, keepdims=True) + jnp.exp(sink - m))
    o = jnp.einsum('bnhgqk,bnkhd->bnqhgd', probs.astype(v.dtype), vv)
    return o.reshape(B, S, D_A)


def stick_breaking_attention(q, k, v):
    B, S, _ = q.shape
    q = q.reshape(B, S, H_B, HEAD_DIM).transpose(0, 2, 1, 3)
    k = k.reshape(B, S, H_B, HEAD_DIM).transpose(0, 2, 1, 3)
    v = v.reshape(B, S, H_B, HEAD_DIM).transpose(0, 2, 1, 3)
    outs = []
    for i in range(S // Q_BLOCK):
        end = (i + 1) * Q_BLOCK
        q_i = q[:, :, i * Q_BLOCK:end]
        k_i = k[:, :, :end]
        v_i = v[:, :, :end]
        t = i * Q_BLOCK + jnp.arange(Q_BLOCK)
        s = jnp.arange(end)
        mask = s[None, :] < t[:, None]
        z = jnp.einsum('bhqd,bhkd->bhqk', q_i, k_i).astype(jnp.float32) * ATTN_SCALE
        u = jnp.where(mask, jax.nn.log_sigmoid(-z), 0.0)
        suffix = lax.cumsum(u, axis=3, reverse=True) - u
        a = jnp.where(mask, jnp.exp(jax.nn.log_sigmoid(z) + suffix), 0.0)
        outs.append(jnp.einsum('bhqk,bhkd->bhqd', a.astype(v.dtype), v_i))
    o = jnp.concatenate(outs, axis=2)
    return o.transpose(0, 2, 1, 3).reshape(B, S, D_B)


def hybrid_mixer(h, w_in, w_out, sinks, rel_bias, norm_a, norm_b):
    qkv = h @ w_in
    cuts = np.cumsum([D_A, D_KV_A, D_KV_A, D_B, D_B]).tolist()
    q_a, k_a, v_a, q_b, k_b, v_b = jnp.split(qkv, cuts, axis=-1)
    o_a = rms_norm(sliding_sink_attention(q_a, k_a, v_a, sinks, rel_bias), norm_a)
    o_b = rms_norm(stick_breaking_attention(q_b, k_b, v_b), norm_b)
    return jnp.concatenate([o_a, o_b], axis=-1) @ w_out


def hierarchical_moe(h, w_grp, b_grp, w_rtr, b_rtr, w_gate, w_up, w_down):
    B, S, D = h.shape
    T = B * S
    xt = h.reshape(T, D)
    glog = (xt @ w_grp + b_grp).astype(jnp.float32)
    g_sel = jnp.argmax(glog, axis=-1)
    p_g = jnp.take_along_axis(jax.nn.softmax(glog, axis=-1), g_sel[:, None], axis=1)[:, 0]
    elog = (xt @ w_rtr + b_rtr).astype(jnp.float32).reshape(T, N_GROUPS, EXPERTS_PER_GROUP)
    elog = jnp.take_along_axis(elog, g_sel[:, None, None], axis=1)[:, 0]
    top_v, top_i = lax.top_k(elog, TOP_K)
    gates = p_g[:, None] * jax.nn.softmax(top_v, axis=-1)
    expert = g_sel[:, None].astype(jnp.int32) * EXPERTS_PER_GROUP + top_i.astype(jnp.int32)
    M = T * TOP_K
    e_flat = expert.reshape(M)
    tok_flat = jnp.arange(M, dtype=jnp.int32) // TOP_K
    order = jnp.argsort(e_flat)
    e_sorted = e_flat[order]
    tok_sorted = tok_flat[order]
    gate_sorted = gates.reshape(M)[order]
    counts = jax.ops.segment_sum(jnp.ones((M,), jnp.int32), e_flat, num_segments=N_EXPERTS)
    start = jnp.cumsum(counts) - counts
    padded = (counts + MOE_BLOCK - 1) // MOE_BLOCK * MOE_BLOCK
    pend = jnp.cumsum(padded)
    pstart = pend - padded
    dest = pstart[e_sorted] + (jnp.arange(M, dtype=jnp.int32) - start[e_sorted])
    m_pad = M + N_EXPERTS * MOE_BLOCK
    n_blk = m_pad // MOE_BLOCK
    slot_tok = jnp.full((m_pad,), T, jnp.int32).at[dest].set(tok_sorted)
    blk_expert = jnp.clip(jnp.searchsorted(pend, jnp.arange(n_blk) * MOE_BLOCK, side='right'),
                          0, N_EXPERTS - 1)
    x_pad = jnp.concatenate([xt, jnp.zeros((1, D), xt.dtype)], axis=0)
    xs = x_pad[slot_tok].reshape(n_blk, MOE_BLOCK, D)

    def expert_block(args):
        xb, e = args
        return (jax.nn.silu(xb @ w_gate[e]) * (xb @ w_up[e])) @ w_down[e]

    ys = lax.map(expert_block, (xs, blk_expert)).reshape(m_pad, D)
    y_assign = ys[dest] * gate_sorted[:, None].astype(ys.dtype)
    y = jax.ops.segment_sum(y_assign, tok_sorted, num_segments=T)
    return y.reshape(B, S, D)


def setup_inputs(seed: int = 0) -> dict:
    key = jax.random.key(seed)
    ks = jax.random.split(key, 24)
    f32 = jnp.float32
    D = D_MODEL
    nrm = lambda k, shape, s: jax.random.normal(k, shape, f32) * s
    x = nrm(ks[0], (BATCH, SEQ, D), 1.0)
    c = nrm(ks[1], (BATCH, D), 1.0)
    col_scale = jnp.concatenate([
        jnp.ones((D_A + D_KV_A,), f32), jnp.full((D_KV_A,), BETA, f32),
        jnp.ones((2 * D_B,), f32), jnp.full((D_B,), BETA, f32)])
    w_in = nrm(ks[2], (DEPTH, D, QKV_DIM), D ** -0.5) * col_scale
    w_out = nrm(ks[3], (DEPTH, D_MIX, D), BETA * D_MIX ** -0.5)
    sinks = nrm(ks[4], (DEPTH, H_A), 0.5)
    rel_bias = nrm(ks[5], (NUM_BUCKETS, H_A), 0.2)
    norm_a = 1.0 + nrm(ks[6], (DEPTH, D_A), 0.02)
    norm_b = 1.0 + nrm(ks[7], (DEPTH, D_B), 0.02)
    w_ada = nrm(ks[8], (DEPTH, D, 6 * D), 0.1 * D ** -0.5)
    b_ada = nrm(ks[9], (DEPTH, 6 * D), 0.02)
    ln1_g = 1.0 + nrm(ks[10], (DEPTH, D), 0.02)
    ln1_b = nrm(ks[11], (DEPTH, D), 0.02)
    ln2_g = 1.0 + nrm(ks[12], (DEPTH, D), 0.02)
    ln2_b = nrm(ks[13], (DEPTH, D), 0.02)
    w_grp = nrm(ks[14], (DEPTH, D, N_GROUPS), D ** -0.5)
    b_grp = nrm(ks[15], (DEPTH, N_GROUPS), 0.01)
    w_rtr = nrm(ks[16], (DEPTH, D, N_EXPERTS), D ** -0.5)
    b_rtr = nrm(ks[17], (DEPTH, N_EXPERTS), 0.01)
    w_gate = nrm(ks[18], (DEPTH, N_EXPERTS, D, D_EXPERT), BETA * D ** -0.5)
    w_up = nrm(ks[19], (DEPTH, N_EXPERTS, D, D_EXPERT), BETA * D ** -0.5)
    w_down = nrm(ks[20], (DEPTH, N_EXPERTS, D_EXPERT, D), BETA * D_EXPERT ** -0.5)
    return {"x": x, "c": c, "w_in": w_in, "w_out": w_out, "sinks": sinks,
            "rel_bias": rel_bias, "norm_a": norm_a, "norm_b": norm_b,
            "w_ada": w_ada, "b_ada": b_ada, "ln1_g": ln1_g, "ln1_b": ln1_b,
            "ln2_g": ln2_g, "ln2_b": ln2_b, "w_grp": w_grp, "b_grp": b_grp,
            "w_rtr": w_rtr, "b_rtr": b_rtr, "w_gate": w_gate, "w_up": w_up,
            "w_down": w_down}


def reference(x, c, w_in, w_out, sinks, rel_bias, norm_a, norm_b, w_ada, b_ada,
              ln1_g, ln1_b, ln2_g, ln2_b, w_grp, b_grp, w_rtr, b_rtr,
              w_gate, w_up, w_down):
    c_act = jax.nn.silu(c)
    for l in range(DEPTH):
        mod = c_act @ w_ada[l] + b_ada[l]
        shift1, scale1, gate1, shift2, scale2, gate2 = [
            m[:, None, :] for m in jnp.split(mod, 6, axis=-1)]
        h = x * (1.0 + scale1) + shift1
        mix = hybrid_mixer(h, w_in[l], w_out[l], sinks[l], rel_bias, norm_a[l], norm_b[l])
        x = layer_norm(ALPHA * x + (1.0 + gate1) * mix, ln1_g[l], ln1_b[l])
        h = x * (1.0 + scale2) + shift2
        ffn = hierarchical_moe(h, w_grp[l], b_grp[l], w_rtr[l], b_rtr[l],
                               w_gate[l], w_up[l], w_down[l])
        x = layer_norm(ALPHA * x + (1.0 + gate2) * ffn, ln2_g[l], ln2_b[l])
    return x
```

```python
import math
import types
import numpy as np
import ml_dtypes
import concourse.bass as bass
import concourse.mybir as mybir
from concourse.bass_utils import run_bass_kernel_spmd

F32 = mybir.dt.float32
BF16 = mybir.dt.bfloat16
I32 = mybir.dt.int32
U32 = mybir.dt.uint32
AF = mybir.ActivationFunctionType
ALU = mybir.AluOpType
AX = mybir.AxisListType

ENGS = ("pe", "act", "dve", "pool", "sp")


class Res:
    __slots__ = ("name", "w", "r", "excl", "dsem")

    def __init__(self, name, excl=False):
        self.name = name
        self.w = None
        self.r = []
        self.excl = excl
        self.dsem = None


class Tok:
    __slots__ = ("kind", "eng", "seq", "sem", "val")

    def __init__(self, kind, eng=None, seq=None, sem=None, val=None):
        self.kind = kind; self.eng = eng; self.seq = seq; self.sem = sem; self.val = val


class Op:
    __slots__ = ("fn", "waits", "signal", "dma_sem", "seq")

    def __init__(self, fn):
        self.fn = fn; self.waits = []; self.signal = False; self.dma_sem = None; self.seq = 0


def _freeze(fn):
    if fn is None or fn.__closure__ is None:
        return fn
    cells = []
    for c in fn.__closure__:
        try:
            cells.append(types.CellType(c.cell_contents))
        except ValueError:
            cells.append(c)
    return types.FunctionType(fn.__code__, fn.__globals__, fn.__name__, fn.__defaults__, tuple(cells))


class Prog:
    def __init__(self, nc):
        self.nc = nc
        self.ops = {e: [] for e in ENGS}
        self.seen = {e: {} for e in ENGS}
        self.dsems = []
        self.nres = 0
        self.ncomp = {e: 0 for e in ENGS}
        self.lastc = {e: None for e in ENGS}

    def res(self, name=None, excl=False):
        self.nres += 1
        return Res(name or f"r{self.nres}", excl)

    def _need(self, eng, op, tok, same_eng_raw=False):
        if tok is None:
            return
        if tok.kind == "e":
            if tok.eng == eng:
                if not same_eng_raw or eng == "pe":
                    return
                if tok.val < self.ncomp[eng] - 1:
                    return
            key = ("e", tok.eng)
            if self.seen[eng].get(key, 0) >= tok.seq:
                return
            self.seen[eng][key] = tok.seq
            self.ops[tok.eng][tok.seq - 1].signal = True
            op.waits.append(tok)
        else:
            key = ("d", tok.sem)
            if self.seen[eng].get(key, 0) >= tok.val:
                return
            self.seen[eng][key] = tok.val
            op.waits.append(tok)

    def _deps(self, eng, op, rd, wr):
        for r in rd:
            if r.excl:
                wr = list(wr) + [r]
                continue
            self._need(eng, op, r.w, same_eng_raw=True)
        for w in wr:
            self._need(eng, op, w.w, same_eng_raw=False)
            for t in w.r:
                self._need(eng, op, t)

    def _commit(self, tok, rd, wr):
        for r in rd:
            if r.excl:
                r.w = tok; r.r = []
            else:
                r.r.append(tok)
        for w in wr:
            w.w = tok; w.r = []

    def op(self, eng, fn, rd=(), wr=()):
        o = Op(_freeze(fn))
        self._deps(eng, o, rd, wr)
        self.ops[eng].append(o)
        o.seq = len(self.ops[eng])
        self.ncomp[eng] += 1
        tok = Tok("e", eng=eng, seq=o.seq, val=self.ncomp[eng])
        self.lastc[eng] = tok
        self._commit(tok, rd, wr)
        return tok

    def dma(self, eng, fn, sb, rd=(), wr=()):
        if sb.dsem is None:
            sb.dsem = len(self.dsems)
            self.dsems.append([0])
        o = Op(_freeze(fn))
        cur = self.dsems[sb.dsem][0]
        if cur:
            self._need(eng, o, Tok("d", sem=sb.dsem, val=cur))
        self._deps(eng, o, rd, wr)
        self.dsems[sb.dsem][0] = cur + 16
        o.dma_sem = sb.dsem
        self.ops[eng].append(o)
        o.seq = len(self.ops[eng])
        tok = Tok("d", sem=sb.dsem, val=cur + 16)
        self._commit(tok, rd, wr)
        return tok

    def barrier(self, all_res=()):
        toks = []
        for e in ENGS:
            if self.lastc[e] is not None:
                toks.append(self.lastc[e])
        for i, v in enumerate(self.dsems):
            if v[0]:
                toks.append(Tok("d", sem=i, val=v[0]))
        for e in ENGS:
            o = Op(None)
            for t in toks:
                if t.kind == "e" and t.eng == e:
                    continue
                self._need(e, o, t)
            if o.waits:
                self.ops[e].append(o)
                o.seq = len(self.ops[e])

    def final_wait(self, eng="sp"):
        o = Op(None)
        for i, v in enumerate(self.dsems):
            if v[0]:
                self._need(eng, o, Tok("d", sem=i, val=v[0]))
        for e in ENGS:
            if e != eng and self.lastc[e] is not None:
                self._need(eng, o, self.lastc[e])
        self.ops[eng].append(o)
        o.seq = len(self.ops[eng])

    def emit(self):
        nc = self.nc
        esem = {e: nc.alloc_semaphore(name=f"sem_{e}") for e in ENGS}
        dsem = [nc.alloc_semaphore(name=f"dsem{i}") for i in range(len(self.dsems))]
        sigval = {}
        for e in ENGS:
            c = 0
            vals = []
            for o in self.ops[e]:
                if o.signal:
                    c += 1
                vals.append(c)
            sigval[e] = vals
        ops = self.ops
        stats = {e: (len(ops[e]), sigval[e][-1] if sigval[e] else 0) for e in ENGS}

        def run(engname, eng):
            for o in ops[engname]:
                for t in o.waits:
                    if t.kind == "e":
                        eng.wait_ge(esem[t.eng], sigval[t.eng][t.seq - 1])
                    else:
                        eng.wait_ge(dsem[t.sem], t.val)
                if o.fn is None:
                    continue
                ins = o.fn(eng)
                if o.dma_sem is not None:
                    ins.then_inc(dsem[o.dma_sem], 16)
                elif o.signal:
                    ins.then_inc(esem[engname], 1)

        with nc.Block() as block:
            @block.tensor
            def _(e):
                run("pe", e)

            @block.scalar
            def _(e):
                run("act", e)

            @block.vector
            def _(e):
                run("dve", e)

            @block.gpsimd
            def _(e):
                run("pool", e)

            @block.sync
            def _(e):
                run("sp", e)
        return stats


D = 2048
HD = 64
QKV = 4352
NEXP = 32
DE = 512
ALPHA = 2.0 ** 0.25
EPS = 1e-5
BIG = 30000.0
CA_Q, CA_K, CA_V, CB_Q, CB_K, CB_V = 0, 1024, 1152, 1280, 2304, 3328


class T:
    __slots__ = ("ap", "r")

    def __init__(self, ap, r):
        self.ap = ap; self.r = r


class Arena:
    def __init__(self, P, tensor, nwords):
        self.P = P; self.t = tensor; self.n = nwords; self.off = 0

    def _take(self, words):
        a = self.t[:, self.off:self.off + words]
        self.off += words
        assert self.off <= self.n, f"arena overflow {self.off} > {self.n}"
        return a

    def f32(self, cols, pat=None, **kw):
        a = self._take(cols)
        return T(a.rearrange(pat, **kw) if pat else a, self.P.res())

    def i32(self, cols, pat=None, **kw):
        a = self._take(cols).bitcast(I32)
        return T(a.rearrange(pat, **kw) if pat else a, self.P.res())

    def bf(self, cols, pat=None, **kw):
        a = self._take((cols + 1) // 2).bitcast(BF16)[:, 0:cols]
        return T(a.rearrange(pat, **kw) if pat else a, self.P.res())


def host_consts(CAP):
    c = {}
    c["eoff"] = np.tile((np.arange(32, dtype=np.float32) * CAP)[None, :], (128, 1))
    c["ident"] = np.eye(128, dtype=np.float32)
    j = np.arange(128)[:, None]; s_ = np.arange(128)[None, :]
    c["negtri"] = np.where(j >= s_, -1.0, 0.0).astype(ml_dtypes.bfloat16)
    c["negsu"] = np.where(j < s_, -1.0, 0.0).astype(ml_dtypes.bfloat16)
    c["lowtri"] = np.where(j < s_, 1.0, 0.0).astype(ml_dtypes.bfloat16)
    tq = np.arange(512)[None, :]
    c["sbmask"] = np.stack([(jj * 128 + np.arange(128)[:, None] < tq) for jj in range(4)], axis=1).astype(np.float32)
    oh = np.zeros((128, 2, 128), np.float32); oh[:, 0, 0:64] = 1.0; oh[:, 1, 64:128] = 1.0
    c["oneshalf"] = oh.astype(ml_dtypes.bfloat16)
    def bucket(n):
        n = max(n, 0); me = 16
        if n < me:
            return n
        ratio = np.float32(max(n, me)) / np.float32(me)
        large = me + int(np.float32(np.log(ratio) / np.float32(math.log(128 / me))) * (32 - me))
        return min(large, 31)
    ohb = np.zeros((32, 512), np.float32); neg = np.zeros((1, 512), np.float32)
    for i in range(256):
        if i <= 127:
            ohb[bucket(127 - i), i] = 1.0
        else:
            neg[0, i] = -BIG
        if 128 <= i <= 254:
            ohb[bucket(255 - i), 256 + i] = 1.0
        else:
            neg[0, 256 + i] = -BIG
    c["ohb"] = ohb; c["negrow"] = neg
    return c


def build_nc(S=2048, NSEQ=2, CAPB=3, dbg=False):
    nc = bass.Bass("TRN2", target_bir_lowering=False)
    TK = NSEQ * S
    NT = S // 128
    NQ = S // 512
    NTT = TK // 128
    CAP = CAPB * 128
    NSLOT = NEXP * CAP

    def din(name, shape, dt=F32):
        return nc.dram_tensor(name, list(shape), dt, kind="ExternalInput").ap()

    x_d = din("x", [TK, D]); c_d = din("c", [NSEQ, D]); w_in_d = din("w_in", [D, QKV]); w_out_d = din("w_out", [D, D])
    sinks_d = din("sinks_fm", [128, 8]); relb_d = din("rel_bias", [32, 16])
    na_d = din("na_fm", [128, 8]); nb_d = din("nb_fm", [128, 8])
    w_ada_d = din("w_ada", [D, 6 * D]); b_ada_d = din("b_ada", [1, 6 * D])
    ln1g_d = din("ln1_g", [1, D]); ln1b_d = din("ln1_b", [1, D]); ln2g_d = din("ln2_g", [1, D]); ln2b_d = din("ln2_b", [1, D])
    wgrp_d = din("w_grp", [D, 4]); bgrp_d = din("b_grp", [1, 4]); wrtr_d = din("w_rtr", [D, 32]); brtr_d = din("b_rtr", [1, 32])
    wg_d = din("w_gate", [NEXP, D, DE]); wu_d = din("w_up", [NEXP, D, DE]); wd_d = din("w_down", [NEXP, DE, D])
    ident_d = din("ident", [128, 128]); negtri_d = din("negtri", [128, 128], BF16); negsu_d = din("negsu", [128, 128], BF16)
    lowtri_d = din("lowtri", [128, 128], BF16); sbmask_d = din("sbmask", [128, 4, 512]); oneshalf_d = din("oneshalf", [128, 2, 128], BF16)
    ohb_d = din("ohb", [32, 512]); negrow_d = din("negrow", [1, 512]); eoff_d = din("eoff", [128, 32])
    out_d = nc.dram_tensor("out", [TK, D], F32, kind="ExternalOutput").ap()

    def dscr(name, shape, dt):
        return nc.dram_tensor(name, list(shape), dt, kind="Internal").ap()

    mod_d = dscr("mod_scr", [NSEQ, 6 * D], F32)
    gb_d = dscr("gb_scr", [16, 512], F32)
    ogT_d = dscr("ogT_scr", [NSEQ, D, S], BF16)
    x1_d = dscr("x1_scr", [TK, D], F32)
    xs_d = dscr("xs_scr", [NSLOT, D], BF16)
    ys_d = dscr("ys_scr", [NSLOT, D], F32)
    dbg_d = {}
    if dbg:
        dbg_d["ogT"] = nc.dram_tensor("dbg_ogT", [NSEQ, D, S], BF16, kind="ExternalOutput").ap()
        dbg_d["x1"] = nc.dram_tensor("dbg_x1", [TK, D], F32, kind="ExternalOutput").ap()
        dbg_d["mod"] = nc.dram_tensor("dbg_mod", [NSEQ, 6 * D], F32, kind="ExternalOutput").ap()
        dbg_d["ssq"] = nc.dram_tensor("dbg_ssq", [NSEQ, 128, 2 * NT], F32, kind="ExternalOutput").ap()
        dbg_d["slot"] = nc.dram_tensor("dbg_slot", [128, NTT * 2], I32, kind="ExternalOutput").ap()
        dbg_d["gw"] = nc.dram_tensor("dbg_gw", [128, NTT * 2], F32, kind="ExternalOutput").ap()
        dbg_d["eb"] = nc.dram_tensor("dbg_eb", [128, 4096], BF16, kind="ExternalOutput").ap()
        for nm_ in ("den", "o", "pcf", "ppf", "psd"):
            dbg_d[nm_] = nc.dram_tensor("dbg_" + nm_, [2, 128, 512], F32, kind="ExternalOutput").ap()

    NW = 206 * 256
    arena_t = nc.alloc_sbuf_tensor("arena", [128, NW], F32)
    psum_t = nc.alloc_psum_tensor("psum", [128, 8 * 512], F32)
    P = Prog(nc)
    A = Arena(P, arena_t, NW)
    PB = [T(psum_t[:, i * 512:(i + 1) * 512], P.res(f"bank{i}", excl=True)) for i in range(8)]
    rot = {}

    def nxt(key, n):
        rot[key] = (rot.get(key, -1) + 1) % n
        return rot[key]

    def R(*ts):
        return [t.r for t in ts]

    r_mod = P.res("mod_d"); r_gb = P.res("gb_d"); r_xs = P.res("xs_d")
    r_og = [P.res(f"og_d{b}") for b in range(NSEQ)]
    r_x1 = [P.res(f"x1_d{t}") for t in range(NTT)]
    r_ys = [P.res(f"ys_d{e}") for e in range(NEXP)]

    def load(eng, t, src, extra_rd=()):
        return P.dma(eng, lambda e: e.dma_start(out=t.ap, in_=src), t.r, rd=list(extra_rd), wr=[t.r])

    def store(eng, dst, t, src_ap=None, extra_wr=()):
        sa = t.ap if src_ap is None else src_ap
        return P.dma(eng, lambda e: e.dma_start(out=dst, in_=sa), t.r, rd=[t.r], wr=list(extra_wr))

    ident = A.f32(128); negtri = A.bf(128); negsu = A.bf(128); lowtri = A.bf(128)
    onesh = A.bf(256, "p (r c) -> p r c", r=2)
    ones_f = A.f32(128); ones_b = A.bf(128)
    na_fm = A.f32(8); nb_fm = A.f32(8); esink = A.f32(8)
    slot_i = A.i32(NTT * 2, "p (t k) -> p t k", k=2)
    gatew = A.f32(NTT * 2, "p (t k) -> p t k", k=2)
    ssq = A.f32(2 * NT, "p (g t) -> p g t", g=2)
    cnt_keep = A.f32(32); eoff = A.f32(32)
    load("sp", eoff, eoff_d)
    P.op("pool", lambda e: e.memset(cnt_keep.ap, 0.0), wr=R(cnt_keep))
    load("sp", ident, ident_d); load("sp", negtri, negtri_d); load("sp", negsu, negsu_d); load("sp", lowtri, lowtri_d)
    load("sp", onesh, oneshalf_d); load("sp", na_fm, na_d); load("sp", nb_fm, nb_d); load("sp", esink, sinks_d)
    P.op("pool", lambda e: e.memset(ones_f.ap, 1.0), wr=R(ones_f))
    P.op("pool", lambda e: e.memset(ones_b.ap, 1.0), wr=R(ones_b))
    P.op("act", lambda e: e.activation(out=esink.ap, in_=esink.ap, func=AF.Exp), rd=R(esink), wr=R(esink))
    base_off = A.off

    ct = A.f32(16 * NSEQ, "p (k b) -> p k b", b=NSEQ)
    sg = A.f32(16 * NSEQ, "p (k b) -> p k b", b=NSEQ)
    with nc.allow_non_contiguous_dma(reason="tiny transposed load of c"):
        for bb in range(NSEQ):
            P.dma("sp", lambda e, bb=bb: e.dma_start(out=ct.ap[:, :, bb:bb + 1], in_=c_d[bb:bb + 1, :].rearrange("o (k p) -> p k o", p=128), allow_slow_non_contiguous=True), ct.r, wr=[ct.r])
    P.op("act", lambda e: e.activation(out=sg.ap, in_=ct.ap, func=AF.Exp, scale=-1.0), rd=R(ct), wr=R(sg))
    P.op("dve", lambda e: e.tensor_scalar_add(out=sg.ap, in0=sg.ap, scalar1=1.0), rd=R(sg), wr=R(sg))
    P.op("dve", lambda e: e.reciprocal(out=sg.ap, in_=sg.ap), rd=R(sg), wr=R(sg))
    P.op("dve", lambda e: e.tensor_tensor(out=ct.ap, in0=ct.ap, in1=sg.ap, op=ALU.mult), rd=R(ct, sg), wr=R(ct))
    wa = [A.f32(4096) for _ in range(3)]
    brow = A.f32(4096); mrow = A.f32(4096)
    w_ada_v = w_ada_d.rearrange("(k p) n -> p k n", p=128)
    for third in range(3):
        c0 = third * 4096
        P.dma("sp", lambda e, c0=c0: e.dma_start(out=brow.ap[0:1, :], in_=b_ada_d[:, c0:c0 + 4096]), brow.r, wr=[brow.r])
        for k in range(16):
            w = wa[nxt("wa", 3)]
            load("sp", w, w_ada_v[:, k, c0:c0 + 4096])
            for n8 in range(8):
                pb = PB[n8]
                P.op("pe", lambda e, k=k, w=w, pb=pb, n8=n8: e.matmul(out=pb.ap[0:NSEQ, :], lhsT=ct.ap[:, k, :], rhs=w.ap[:, n8 * 512:(n8 + 1) * 512], start=(k == 0), stop=False),
                     rd=R(ct, w), wr=R(pb))
        for n8 in range(8):
            pb = PB[n8]
            P.op("pe", lambda e, pb=pb, n8=n8: e.matmul(out=pb.ap[0:NSEQ, :], lhsT=ones_f.ap[0:1, 0:NSEQ], rhs=brow.ap[0:1, n8 * 512:(n8 + 1) * 512], start=False, stop=True),
                 rd=R(ones_f, brow), wr=R(pb))
            if n8 % 2 == 0:
                P.op("dve", lambda e, pb=pb, n8=n8: e.tensor_copy(out=mrow.ap[0:NSEQ, n8 * 512:(n8 + 1) * 512], in_=pb.ap[0:NSEQ, :]), rd=R(pb), wr=R(mrow))
            else:
                P.op("act", lambda e, pb=pb, n8=n8: e.activation(out=mrow.ap[0:NSEQ, n8 * 512:(n8 + 1) * 512], in_=pb.ap[0:NSEQ, :], func=AF.Copy), rd=R(pb), wr=R(mrow))
        store("sp", mod_d[:, c0:c0 + 4096], mrow, src_ap=mrow.ap[0:NSEQ, :], extra_wr=[r_mod])
    relb = A.f32(16); ohb = A.f32(512); negrow = A.f32(512); grow = A.f32(512)
    P.dma("sp", lambda e: e.dma_start(out=relb.ap[0:32, :], in_=relb_d), relb.r, wr=[relb.r])
    P.dma("sp", lambda e: e.dma_start(out=ohb.ap[0:32, :], in_=ohb_d), ohb.r, wr=[ohb.r])
    P.dma("sp", lambda e: e.dma_start(out=negrow.ap[0:1, :], in_=negrow_d), negrow.r, wr=[negrow.r])
    pb = PB[nxt("pj", 2)]
    P.op("pe", lambda e, pb=pb: e.matmul(out=pb.ap[0:16, :], lhsT=relb.ap[0:32, :], rhs=ohb.ap[0:32, :], start=True, stop=False), rd=R(relb, ohb), wr=R(pb))
    P.op("pe", lambda e, pb=pb: e.matmul(out=pb.ap[0:16, :], lhsT=ones_f.ap[0:1, 0:16], rhs=negrow.ap[0:1, :], start=False, stop=True), rd=R(ones_f, negrow), wr=R(pb))
    P.op("dve", lambda e, pb=pb: e.tensor_copy(out=grow.ap[0:16, :], in_=pb.ap[0:16, :]), rd=R(pb), wr=R(grow))
    store("sp", gb_d, grow, src_ap=grow.ap[0:16, :], extra_wr=[r_gb])
    if dbg:
        P.barrier()
        mt = A.f32(6 * D)
        P.dma("sp", lambda e: e.dma_start(out=mt.ap[0:NSEQ, :], in_=mod_d), mt.r, rd=[r_mod], wr=[mt.r])
        store("sp", dbg_d["mod"], mt, src_ap=mt.ap[0:NSEQ, :])
    P.barrier()
    A.off = base_off

    def bcast_row(src_row_ap):
        return bass.AP(src_row_ap.tensor, src_row_ap.offset, [[0, 128], [1, src_row_ap.shape[-1]]])

    w_in_v = w_in_d.rearrange("(k p) n -> p k n", p=128)
    w_out_v = w_out_d.rearrange("(k p) n -> p k n", p=128)

    A.off = base_off
    hT = A.bf(16 * S, "p (k s) -> p k s", k=16)
    sc1 = A.f32(16); sh1 = A.f32(16)
    wch = [A.bf(16 * 128, "p (k n) -> p k n", k=16) for _ in range(3)]
    qT = [A.bf(S) for _ in range(2)]
    sq_t = A.f32(512)
    og_t = [A.bf(512) for _ in range(2)]
    hks = [A.f32(128) for _ in range(3)]
    uni_off = A.off
    xt2 = [A.f32(2 * D, "p (i d) -> p i d", i=2) for _ in range(2)]
    A.off = uni_off
    kTa = A.bf(S); kTas = A.bf(S)
    VA = [[A.bf(NT * 128, "p (t n) -> p t n", n=128) for _ in range(2)] for _ in range(2)]
    EB = A.bf(16 * 2 * 128, "p (h c n) -> p h c n", h=16, c=2)
    pf_t = [A.f32(512) for _ in range(4)]; pb_t = [A.bf(512) for _ in range(4)]
    den_t = A.f32(512); o_t = A.f32(512)
    A.off = uni_off
    kT = [A.bf(S) for _ in range(2)]
    Vp = [[A.bf(NT * 128, "p (t n) -> p t n", n=128) for _ in range(2)] for _ in range(2)]
    sbm = A.f32(4 * 512, "p (j n) -> p j n", j=4)
    e_t = [A.f32(1024) for _ in range(5)]
    sp_t = [A.bf(1024) for _ in range(4)]; ec_t = [A.f32(1024) for _ in range(3)]; a_t = [A.bf(1024) for _ in range(3)]
    A.off = base_off
    wo = A.bf(16 * D, "p (k n) -> p k n", k=16)
    GG = A.f32(D); BB = A.f32(D); LG = A.f32(D); LB = A.f32(D)
    ogt = [A.bf(16 * 128, "p (k s) -> p k s", k=16) for _ in range(2)]
    xtl = [A.f32(D) for _ in range(2)]
    mixn = A.f32(D); h2_2 = [A.f32(D) for _ in range(2)]
    nrm2 = [A.f32(D) for _ in range(2)]; x1t2 = [A.f32(D) for _ in range(2)]
    h2b = [A.bf(D) for _ in range(3)]
    lg2 = [A.f32(40) for _ in range(2)]; smr = [A.f32(40) for _ in range(12)]
    h2T = A.f32(16 * 128, "p (k s) -> p k s", k=16)
    wr_t = A.f32(16 * 36, "p (k n) -> p k n", k=16); brt = A.f32(36)
    rstd = A.f32(2 * NT, "p (g t) -> p g t", g=2)
    st6 = A.f32(24); mv = A.f32(2); sm = [None] * 10 + [A.f32(8), A.f32(8)]
    ohbf = A.bf(32)

    for b in range(NSEQ):
        with nc.allow_non_contiguous_dma(reason="tiny feature-major load of adaLN shift/scale"):
            P.dma("sp", lambda e, b=b: e.dma_start(out=sh1.ap, in_=mod_d[b:b + 1, 0:D].rearrange("o (k p) -> p (o k)", p=128), allow_slow_non_contiguous=True), sh1.r, rd=[r_mod], wr=[sh1.r])
            P.dma("sp", lambda e, b=b: e.dma_start(out=sc1.ap, in_=mod_d[b:b + 1, D:2 * D].rearrange("o (k p) -> p (o k)", p=128), allow_slow_non_contiguous=True), sc1.r, rd=[r_mod], wr=[sc1.r])
        P.op("dve", lambda e: e.tensor_scalar_add(out=sc1.ap, in0=sc1.ap, scalar1=1.0), rd=R(sc1), wr=R(sc1))
        P.op("dve", lambda e: e.memset(ssq.ap, 0.0), wr=R(ssq))
        for tb in range(S // 256):
            tok0 = b * S + tb * 256
            xt = xt2[tb % 2]
            load("sp", xt, x_d[tok0:tok0 + 256, :].rearrange("(i p) d -> p i d", p=128))
            for k2 in range(8):
                pbk = PB[nxt("pj", 2)]
                for kk in range(2):
                    k = k2 * 2 + kk
                    for i in range(2):
                        P.op("pe", lambda e, pbk=pbk, kk=kk, i=i, k=k: e.transpose(out=pbk.ap[:, kk * 256 + i * 128: kk * 256 + (i + 1) * 128],
                                                                                 in_=xt.ap[:, i, k * 128:(k + 1) * 128], identity=ident.ap),
                             rd=R(xt, ident), wr=R(pbk))
                for kk in range(2):
                    k = k2 * 2 + kk
                    dst = hT.ap[:, k, tb * 256:(tb + 1) * 256]
                    if kk == 0:
                        P.op("act", lambda e, pbk=pbk, kk=kk, k=k, dst=dst: e.activation(out=dst, in_=pbk.ap[:, kk * 256:(kk + 1) * 256], func=AF.Identity,
                                                                                       scale=sc1.ap[:, k:k + 1], bias=sh1.ap[:, k:k + 1]),
                             rd=R(pbk, sc1, sh1), wr=R(hT))
                    else:
                        P.op("dve", lambda e, pbk=pbk, kk=kk, k=k, dst=dst: e.tensor_scalar(out=dst, in0=pbk.ap[:, kk * 256:(kk + 1) * 256], scalar1=sc1.ap[:, k:k + 1],
                                                                                          scalar2=sh1.ap[:, k:k + 1], op0=ALU.mult, op1=ALU.add),
                             rd=R(pbk, sc1, sh1), wr=R(hT))

        P.barrier()
        for r in range(2):
            for rr in range(2):
                P.op("pool", lambda e, t=VA[r][rr]: e.memset(t.ap, 0.0), wr=R(VA[r][rr]))
        for h in range(16):
            for cpv in range(2):
                hk = hks[(2 * h + cpv) % 3]
                src = bass.AP(gb_d.tensor, gb_d.offset + h * 512 + cpv * 256, [[1, 128], [1, 128]])
                P.dma("sp", lambda e, src=src: e.dma_start(out=hk.ap, in_=src), hk.r, rd=[r_gb], wr=[hk.r])
                P.op("act", lambda e: e.activation(out=hk.ap, in_=hk.ap, func=AF.Exp), rd=R(hk), wr=R(hk))
                rev = bass.AP(hk.ap.tensor, hk.ap.offset + 127, [[hk.ap.ap[0][0], 128], [-1, 128]])
                P.op("pool", lambda e, h=h, cpv=cpv, rev=rev: e.tensor_copy(out=EB.ap[:, h, cpv, :], in_=rev), rd=R(hk), wr=R(EB))

        def load_w(c0):
            w = wch[nxt("wch", 3)]
            P.dma("pool", lambda e, w=w, c0=c0: e.dma_start(out=w.ap, in_=w_in_v[:, :, c0:c0 + 128]), w.r, wr=[w.r])
            return w

        def proj_fm(w, dst, tbs=None, bank=None, evac_eng=None):
            for tb in (range(NQ) if tbs is None else tbs):
                pbk = PB[nxt("pj", 2)] if bank is None else bank
                for k in range(16):
                    P.op("pe", lambda e, k=k, tb=tb, pbk=pbk: e.matmul(out=pbk.ap, lhsT=w.ap[:, k, :], rhs=hT.ap[:, k, tb * 512:(tb + 1) * 512], start=(k == 0), stop=(k == 15)),
                         rd=R(w, hT), wr=R(pbk))
                eng = evac_eng if evac_eng else ("act" if tb % 2 == 0 else "dve")
                if eng == "act":
                    P.op("act", lambda e, tb=tb, pbk=pbk: e.activation(out=dst.ap[:, tb * 512:(tb + 1) * 512], in_=pbk.ap, func=AF.Copy), rd=R(pbk), wr=R(dst))
                else:
                    P.op("dve", lambda e, tb=tb, pbk=pbk: e.tensor_copy(out=dst.ap[:, tb * 512:(tb + 1) * 512], in_=pbk.ap), rd=R(pbk), wr=R(dst))

        def proj_tm(w, evac, t4s=None, bank=None):
            for t4 in (range(NQ) if t4s is None else t4s):
                pbk = PB[nxt("pj", 2)] if bank is None else bank
                for i in range(4):
                    tk = t4 * 4 + i
                    for k in range(16):
                        P.op("pe", lambda e, k=k, i=i, tk=tk, pbk=pbk: e.matmul(out=pbk.ap[:, i * 128:(i + 1) * 128], lhsT=hT.ap[:, k, tk * 128:(tk + 1) * 128], rhs=w.ap[:, k, :],
                                                                               start=(k == 0), stop=(k == 15)),
                             rd=R(w, hT), wr=R(pbk))
                evac(pbk, t4)

        def ssq_update(grp, qs, sq, bank=None):
            pbk = PB[nxt("pj", 2)] if bank is None else bank
            for i in range(4):
                P.op("pe", lambda e, i=i, pbk=pbk: e.matmul(out=pbk.ap[:, i:i + 1], lhsT=sq.ap[:, i * 128:(i + 1) * 128], rhs=ones_f.ap[:, 0:1], start=True, stop=True),
                     rd=R(sq, ones_f), wr=R(pbk))
            P.op("dve", lambda e, pbk=pbk: e.tensor_tensor(out=ssq.ap[:, grp, qs * 4:qs * 4 + 4], in0=pbk.ap[:, 0:4], in1=ssq.ap[:, grp, qs * 4:qs * 4 + 4], op=ALU.add),
                 rd=R(pbk, ssq), wr=R(ssq))

        w = load_w(CA_K)
        proj_fm(w, kTa)
        wsw = wch[nxt("wch", 3)]
        P.op("pool", lambda e, w=w, wsw=wsw: e.tensor_copy(out=wsw.ap[:, :, 0:64], in_=w.ap[:, :, 64:128]), rd=R(w), wr=R(wsw))
        P.op("pool", lambda e, w=w, wsw=wsw: e.tensor_copy(out=wsw.ap[:, :, 64:128], in_=w.ap[:, :, 0:64]), rd=R(w), wr=R(wsw))
        wsave = w
        w = wsw
        proj_fm(w, kTas)
        w = load_w(CA_V)

        def evac_va(pbk, t4):
            v4 = pbk.ap.rearrange("p (i n) -> p i n", i=4)
            P.op("act", lambda e: e.activation(out=VA[0][0].ap[:, t4 * 4:t4 * 4 + 4, 0:64], in_=v4[:, :, 0:64], func=AF.Copy), rd=R(pbk), wr=R(VA[0][0]))
            P.op("dve", lambda e: e.tensor_copy(out=VA[0][1].ap[:, t4 * 4:t4 * 4 + 4, 64:128], in_=v4[:, :, 0:64]), rd=R(pbk), wr=R(VA[0][1]))
            P.op("act", lambda e: e.activation(out=VA[1][0].ap[:, t4 * 4:t4 * 4 + 4, 0:64], in_=v4[:, :, 64:128], func=AF.Copy), rd=R(pbk), wr=R(VA[1][0]))
            P.op("dve", lambda e: e.tensor_copy(out=VA[1][1].ap[:, t4 * 4:t4 * 4 + 4, 64:128], in_=v4[:, :, 64:128]), rd=R(pbk), wr=R(VA[1][1]))
        proj_tm(w, evac_va)

        swa_units = [(j, qs, r) for j in range(8) for qs in range(NQ) for r in range(2)]
        qts = {}; psod = {}

        swa_bg = []

        def swa_prefetch(jn, bank):
            box = {}

            def mk(tb):
                def run():
                    if tb == 0:
                        box["w"] = load_w(CA_Q + jn * 128)
                        qts[jn] = qT[nxt("qT", 2)]
                    proj_fm(box["w"], qts[jn], tbs=[tb], bank=bank, evac_eng="act")
                return run
            return [mk(tb) for tb in range(NQ)]

        for tsk in swa_prefetch(0, None):
            tsk()

        def w1(u):
            j, qs, r = swa_units[u]
            if qs == 0 and r == 0:
                while swa_bg:
                    swa_bg.pop(0)()
                if j + 1 < 8:
                    swa_bg.extend(swa_prefetch(j + 1, PB[5]))
            elif swa_bg:
                swa_bg.pop(0)()
            q = qts[j]; kv = j // 4
            KT = kTa if kv == r else kTas
            rows = slice(r * 64, r * 64 + 64)
            pcur = PB[2 * (u % 2)]; pprev = PB[2 * (u % 2) + 1]
            for i in range(4):
                tq = qs * 4 + i
                P.op("pe", lambda e, i=i, tq=tq: e.matmul(out=pcur.ap[:, i * 128:(i + 1) * 128], lhsT=KT.ap[rows, tq * 128:(tq + 1) * 128],
                                                         rhs=q.ap[rows, tq * 128:(tq + 1) * 128], start=True, stop=True), rd=R(KT, q), wr=R(pcur))
            for i in range(4):
                tq = qs * 4 + i
                if tq == 0:
                    continue
                P.op("pe", lambda e, i=i, tq=tq: e.matmul(out=pprev.ap[:, i * 128:(i + 1) * 128], lhsT=KT.ap[rows, (tq - 1) * 128:tq * 128],
                                                         rhs=q.ap[rows, tq * 128:(tq + 1) * 128], start=True, stop=True), rd=R(KT, q), wr=R(pprev))
            pcf = pf_t[2 * (u % 2)]; ppf = pf_t[2 * (u % 2) + 1]
            c0 = 128 if qs == 0 else 0
            P.op("act", lambda e: e.activation(out=pcf.ap, in_=pcur.ap, func=AF.Exp, scale=0.125), rd=R(pcur), wr=R(pcf))
            P.op("act", lambda e: e.activation(out=ppf.ap[:, c0:512], in_=pprev.ap[:, c0:512], func=AF.Exp, scale=0.125), rd=R(pprev), wr=R(ppf))

        def w2(u):
            j, qs, r = swa_units[u]
            h = 2 * j + r
            pcf = pf_t[2 * (u % 2)]; ppf = pf_t[2 * (u % 2) + 1]
            pcb = pb_t[2 * (u % 2)]; ppb = pb_t[2 * (u % 2) + 1]
            c0 = 128 if qs == 0 else 0
            n4 = 3 if qs == 0 else 4
            ebc = bass.AP(EB.ap.tensor, EB.ap[:, h, 0, :].offset, [[EB.ap.ap[0][0], 128], [0, 4], [1, 128]])
            ebp2 = bass.AP(EB.ap.tensor, EB.ap[:, h, 1, :].offset, [[EB.ap.ap[0][0], 128], [0, n4], [1, 128]])
            P.op("pool", lambda e: e.tensor_tensor(out=pcb.ap.rearrange("p (i n) -> p i n", i=4), in0=pcf.ap.rearrange("p (i n) -> p i n", i=4), in1=ebc, op=ALU.mult),
                 rd=R(pcf, EB), wr=R(pcb))
            P.op("dve", lambda e: e.tensor_tensor(out=ppb.ap[:, c0:512].rearrange("p (i n) -> p i n", i=n4), in0=ppf.ap[:, c0:512].rearrange("p (i n) -> p i n", i=n4),
                                                  in1=ebp2, op=ALU.mult), rd=R(ppf, EB), wr=R(ppb))

        def w3(u):
            j, qs, r = swa_units[u]
            kv = j // 4
            pcb = pb_t[2 * (u % 2)]; ppb = pb_t[2 * (u % 2) + 1]
            if r == 0:
                psod[(j, qs)] = (PB[6 + nxt("pso", 2)], PB[4])
            pso, psd = psod[(j, qs)]
            Vt = VA[kv][r]
            first = (r == 0)
            for i in range(4):
                tq = qs * 4 + i
                srcs = [(pcb, tq)] + ([(ppb, tq - 1)] if tq > 0 else [])
                for (pt, blk) in srcs:
                    P.op("pe", lambda e, i=i, pt=pt, blk=blk, st=first: e.matmul(out=pso.ap[:, i * 128:(i + 1) * 128], lhsT=Vt.ap[:, blk, :], rhs=pt.ap[:, i * 128:(i + 1) * 128],
                                                                                start=st, stop=True, skip_group_check=True), rd=R(Vt, pt), wr=R(pso))
                    first = False
            c0 = 128 if qs == 0 else 0
            P.op("pe", lambda e, st=(r == 0): e.matmul(out=psd.ap, lhsT=onesh.ap[:, r, :], rhs=pcb.ap, start=st, stop=True, skip_group_check=True), rd=R(onesh, pcb), wr=R(psd))
            P.op("pe", lambda e: e.matmul(out=psd.ap[:, c0:512], lhsT=onesh.ap[:, r, :], rhs=ppb.ap[:, c0:512], start=False, stop=True, skip_group_check=True), rd=R(onesh, ppb), wr=R(psd))
            if r == 1:
                P.op("dve", lambda e: e.tensor_scalar(out=den_t.ap, in0=psd.ap, scalar1=esink.ap[:, j:j + 1], scalar2=None, op0=ALU.add), rd=R(psd, esink), wr=R(den_t))
                P.op("dve", lambda e: e.reciprocal(out=den_t.ap, in_=den_t.ap), rd=R(den_t), wr=R(den_t))
                P.op("dve", lambda e: e.tensor_tensor(out=o_t.ap, in0=pso.ap, in1=den_t.ap, op=ALU.mult), rd=R(pso, den_t), wr=R(o_t))
                og = og_t[nxt("og", 2)]
                P.op("act", lambda e: e.activation(out=og.ap, in_=o_t.ap, func=AF.Copy, scale=na_fm.ap[:, j:j + 1]), rd=R(o_t, na_fm), wr=R(og))
                P.op("act", lambda e: e.activation(out=sq_t.ap, in_=o_t.ap, func=AF.Square), rd=R(o_t), wr=R(sq_t))
                store("sp", ogT_d[b, j * 128:(j + 1) * 128, qs * 512:(qs + 1) * 512], og, extra_wr=[r_og[b]])
                ssq_update(0, qs, sq_t, bank=PB[5])

        nsw = len(swa_units)
        for i in range(nsw + 2):
            if i < nsw:
                w1(i)
            if 0 <= i - 1 < nsw:
                w2(i - 1)
            if 0 <= i - 2 < nsw:
                w3(i - 2)

        P.barrier()
        load("sp", sbm, sbmask_d)
        for r in range(2):
            for rr in range(2):
                P.op("pool", lambda e, t=Vp[r][rr]: e.memset(t.ap, 0.0), wr=R(Vp[r][rr]))
        pair_ap = lambda g: psum_t[:, (2 * g) * 512:(2 * g + 2) * 512]
        sb_in = {}

        def sb_prefetch(jn, bank):
            box = {}
            q_ = qT[nxt("qT", 2)]; k_ = kT[nxt("kT", 2)]
            vi = nxt("Vp", 2)
            V0_ = Vp[vi][0]; V1_ = Vp[vi][1]
            sb_in[jn] = (q_, k_, V0_, V1_)

            def evac_vb(pbk, t4):
                v4 = pbk.ap.rearrange("p (i n) -> p i n", i=4)
                P.op("dve", lambda e: e.tensor_copy(out=V0_.ap[:, t4 * 4:t4 * 4 + 4, 0:64], in_=v4[:, :, 0:64]), rd=R(pbk), wr=R(V0_))
                P.op("dve", lambda e: e.tensor_copy(out=V1_.ap[:, t4 * 4:t4 * 4 + 4, 64:128], in_=v4[:, :, 64:128]), rd=R(pbk), wr=R(V1_))

            def mk(kind, tb):
                def run():
                    if tb == 0:
                        box[kind] = load_w({"q": CB_Q, "k": CB_K, "v": CB_V}[kind] + jn * 128)
                    if kind == "q":
                        proj_fm(box[kind], q_, tbs=[tb], bank=bank, evac_eng="dve")
                    elif kind == "k":
                        proj_fm(box[kind], k_, tbs=[tb], bank=bank, evac_eng="dve")
                    else:
                        proj_tm(box[kind], evac_vb, t4s=[tb], bank=bank)
                return run
            return [mk(kind, tb) for kind in ("q", "k", "v") for tb in range(NQ)]

        for tsk in sb_prefetch(0, None):
            tsk()
        for j in range(8):
            q, kk_, V0, V1 = sb_in[j]
            sb_bg = sb_prefetch(j + 1, PB[7]) if j + 1 < 8 else []
            units = [(qs, kb) for qs in range(NQ) for kb in range(4 * qs + 3, -1, -1)]
            nun = len(units)
            psos = {}

            def s1a(u, q=q, kk_=kk_):
                qs, kb = units[u]
                g = u % 2
                et = e_t[u % 5]
                for r in range(2):
                    rows = slice(r * 64, r * 64 + 64)
                    pss = PB[2 * g + r]
                    P.op("pe", lambda e, r=r: e.matmul(out=pss.ap, lhsT=kk_.ap[rows, kb * 128:(kb + 1) * 128], rhs=q.ap[rows, qs * 512:(qs + 1) * 512], start=True, stop=True),
                         rd=R(kk_, q), wr=R(pss))
                P.op("act", lambda e: e.activation(out=et.ap, in_=pair_ap(g), func=AF.Exp, scale=0.125), rd=R(PB[2 * g], PB[2 * g + 1]), wr=R(et))
                if kb >= 4 * qs:
                    jj = kb - 4 * qs
                    mk = bass.AP(sbm.ap.tensor, sbm.ap[:, jj, :].offset, [[sbm.ap.ap[0][0], 128], [0, 2], [1, 512]])
                    ev = et.ap.rearrange("p (r n) -> p r n", r=2)
                    P.op("pool", lambda e: e.tensor_tensor(out=ev, in0=ev, in1=mk, op=ALU.mult), rd=R(et, sbm), wr=R(et))

            def s1b(u):
                et = e_t[u % 5]; spt = sp_t[u % 4]
                P.op("act", lambda e: e.activation(out=spt.ap, in_=et.ap, func=AF.Ln, bias=1.0, scale=1.0), rd=R(et), wr=R(spt))

            def s2a(u):
                qs, kb = units[u]
                spt = sp_t[u % 4]; ect = ec_t[u % 3]
                st = (kb == 4 * qs + 3)
                for r in range(2):
                    psc = PB[4 + r]
                    P.op("pe", lambda e, r=r: e.matmul(out=psc.ap, lhsT=negtri.ap, rhs=spt.ap[:, r * 512:(r + 1) * 512], start=st, stop=True, skip_group_check=True),
                         rd=R(negtri, spt), wr=R(psc))
                P.op("act", lambda e: e.activation(out=ect.ap, in_=pair_ap(2), func=AF.Exp), rd=R(PB[4], PB[5]), wr=R(ect))

            def s2b1(u):
                qs, kb = units[u]
                spt = sp_t[u % 4]
                if kb > 0:
                    for r in range(2):
                        psc = PB[4 + r]
                        P.op("pe", lambda e, r=r: e.matmul(out=psc.ap, lhsT=negsu.ap, rhs=spt.ap[:, r * 512:(r + 1) * 512], start=False, stop=True, skip_group_check=True),
                             rd=R(negsu, spt), wr=R(psc))

            def s2b(u, j=j, V0=V0, V1=V1):
                qs, kb = units[u]
                spt = sp_t[u % 4]; ect = ec_t[u % 3]; et = e_t[u % 5]; at = a_t[u % 3]
                P.op("dve", lambda e: e.tensor_tensor(out=at.ap, in0=et.ap, in1=ect.ap, op=ALU.mult), rd=R(et, ect), wr=R(at))
                st = qs not in psos
                if st:
                    psos[qs] = PB[6]
                pso = psos[qs]
                for r in range(2):
                    Vt = V0 if r == 0 else V1
                    P.op("pe", lambda e, r=r, Vt=Vt, st=(st and r == 0): e.matmul(out=pso.ap, lhsT=Vt.ap[:, kb, :], rhs=at.ap[:, r * 512:(r + 1) * 512], start=st, stop=True, skip_group_check=True),
                         rd=R(Vt, at), wr=R(pso))
                if kb == 0:
                    og = og_t[nxt("og", 2)]
                    P.op("act", lambda e: e.activation(out=og.ap, in_=pso.ap, func=AF.Copy, scale=nb_fm.ap[:, j:j + 1]), rd=R(pso, nb_fm), wr=R(og))
                    P.op("act", lambda e: e.activation(out=sq_t.ap, in_=pso.ap, func=AF.Square), rd=R(pso), wr=R(sq_t))
                    store("sp", ogT_d[b, (8 + j) * 128:(9 + j) * 128, qs * 512:(qs + 1) * 512], og, extra_wr=[r_og[b]])
                    ssq_update(1, qs, sq_t, bank=PB[7])

            for i in range(nun + 3):
                if sb_bg and i % 3 == 1:
                    sb_bg.pop(0)()
                if i < nun:
                    s1a(i)
                if 0 <= i - 3 < nun:
                    s2b1(i - 3)
                if 0 <= i - 2 < nun:
                    s2a(i - 2)
                if 0 <= i - 1 < nun:
                    s1b(i - 1)
                if 0 <= i - 3 < nun:
                    s2b(i - 3)
            while sb_bg:
                sb_bg.pop(0)()
        if dbg:
            P.barrier()
            store("sp", dbg_d["ssq"][b], ssq, src_ap=ssq.ap.rearrange("p g t -> p (g t)"))
        P.barrier()

        G1 = mixn
        load("sp", G1, bcast_row(mod_d[b:b + 1, 2 * D:3 * D]), extra_rd=[r_mod])
        P.op("dve", lambda e: e.tensor_scalar_add(out=G1.ap, in0=G1.ap, scalar1=1.0), rd=R(G1), wr=R(G1))
        for k in range(16):
            stw = nrm2[k % 2]
            load("sp", stw, w_out_v[:, k, :])
            P.op("dve", lambda e, k=k, stw=stw: e.tensor_tensor(out=wo.ap[:, k, :], in0=stw.ap, in1=G1.ap, op=ALU.mult), rd=R(stw, G1), wr=R(wo))
        load("sp", BB, bcast_row(mod_d[b:b + 1, 3 * D:4 * D]), extra_rd=[r_mod])
        load("sp", GG, bcast_row(mod_d[b:b + 1, 4 * D:5 * D]), extra_rd=[r_mod])
        load("sp", LG, bcast_row(ln1g_d)); load("sp", LB, bcast_row(ln1b_d))
        P.op("dve", lambda e: e.tensor_scalar_add(out=GG.ap, in0=GG.ap, scalar1=1.0), rd=R(GG), wr=R(GG))
        tmpx = x1t2[0]
        P.op("dve", lambda e: e.tensor_tensor(out=tmpx.ap, in0=LB.ap, in1=GG.ap, op=ALU.mult), rd=R(LB, GG), wr=R(tmpx))
        P.op("dve", lambda e: e.tensor_tensor(out=BB.ap, in0=BB.ap, in1=tmpx.ap, op=ALU.add), rd=R(BB, tmpx), wr=R(BB))
        P.op("dve", lambda e: e.tensor_tensor(out=GG.ap, in0=GG.ap, in1=LG.ap, op=ALU.mult), rd=R(GG, LG), wr=R(GG))
        P.dma("sp", lambda e: e.dma_start(out=wr_t.ap[:, :, 0:4], in_=wgrp_d.rearrange("(k p) n -> p k n", p=128)), wr_t.r, wr=[wr_t.r])
        P.dma("sp", lambda e: e.dma_start(out=wr_t.ap[:, :, 4:36], in_=wrtr_d.rearrange("(k p) n -> p k n", p=128)), wr_t.r, wr=[wr_t.r])
        P.dma("sp", lambda e: e.dma_start(out=brt.ap[0:1, 0:4], in_=bgrp_d), brt.r, wr=[brt.r])
        P.dma("sp", lambda e: e.dma_start(out=brt.ap[0:1, 4:36], in_=brtr_d), brt.r, wr=[brt.r])
        P.op("act", lambda e: e.activation(out=rstd.ap, in_=ssq.ap, func=AF.Ln, scale=1.0 / 1024.0, bias=EPS), rd=R(ssq), wr=R(rstd))
        P.op("act", lambda e: e.activation(out=rstd.ap, in_=rstd.ap, func=AF.Exp, scale=-0.5), rd=R(rstd), wr=R(rstd))

        ogT_v = ogT_d[b].rearrange("(k p) s -> p k s", p=128)

        def XL(t):
            og = ogt[t % 2]; xx = xtl[t % 2]; gt = b * NT + t
            P.dma("sp", lambda e: e.dma_start(out=og.ap, in_=ogT_v[:, :, t * 128:(t + 1) * 128]), og.r, rd=[r_og[b]], wr=[og.r])
            load("sp", xx, x_d[gt * 128:(gt + 1) * 128, :])
            P.op("act", lambda e: e.activation(out=mixn.ap, in_=xx.ap, func=AF.Copy, scale=ALPHA), rd=R(xx), wr=R(mixn))

        def XN(t, nb):
            og = ogt[t % 2]
            pa = PB[2 + nxt("pa3", 3)]; pbb = PB[5 + nxt("pb3", 3)]
            for k in range(8):
                P.op("pe", lambda e, k=k: e.matmul(out=pa.ap, lhsT=og.ap[:, k, :], rhs=wo.ap[:, k, nb * 512:(nb + 1) * 512], start=(k == 0), stop=(k == 7)),
                     rd=R(og, wo), wr=R(pa))
            for k in range(8, 16):
                P.op("pe", lambda e, k=k: e.matmul(out=pbb.ap, lhsT=og.ap[:, k, :], rhs=wo.ap[:, k, nb * 512:(nb + 1) * 512], start=(k == 8), stop=(k == 15)),
                     rd=R(og, wo), wr=R(pbb))
            mv_ = mixn.ap[:, nb * 512:(nb + 1) * 512]
            P.op("dve", lambda e: e.scalar_tensor_tensor(out=mv_, in0=pa.ap, scalar=rstd.ap[:, 0, t:t + 1], in1=mv_, op0=ALU.mult, op1=ALU.add), rd=R(pa, rstd, mixn), wr=R(mixn))
            P.op("dve", lambda e: e.scalar_tensor_tensor(out=mv_, in0=pbb.ap, scalar=rstd.ap[:, 1, t:t + 1], in1=mv_, op0=ALU.mult, op1=ALU.add), rd=R(pbb, rstd, mixn), wr=R(mixn))

        def XC(t):
            layer_norm(P, mixn, nrm2[t % 2], st6, mv, sm)

        def YA1(t):
            gt = b * NT + t
            nrm = nrm2[t % 2]; x1t = x1t2[t % 2]
            P.op("dve", lambda e: e.tensor_tensor(out=x1t.ap, in0=nrm.ap, in1=LG.ap, op=ALU.mult), rd=R(nrm, LG), wr=R(x1t))
            P.op("dve", lambda e: e.tensor_tensor(out=x1t.ap, in0=x1t.ap, in1=LB.ap, op=ALU.add), rd=R(x1t, LB), wr=R(x1t))
            store("sp", x1_d[gt * 128:(gt + 1) * 128, :], x1t, extra_wr=[r_x1[gt]])

        def YA2(t):
            nrm = nrm2[t % 2]; hb = h2b[t % 3]; h2 = h2_2[t % 2]
            P.op("dve", lambda e: e.tensor_tensor(out=h2.ap, in0=nrm.ap, in1=GG.ap, op=ALU.mult), rd=R(nrm, GG), wr=R(h2))
            P.op("dve", lambda e: e.tensor_tensor(out=h2.ap, in0=h2.ap, in1=BB.ap, op=ALU.add), rd=R(h2, BB), wr=R(h2))
            P.op("act", lambda e: e.activation(out=hb.ap, in_=h2.ap, func=AF.Copy), rd=R(h2), wr=R(hb))

        def YB(t):
            h2 = h2_2[t % 2]
            for k4 in range(4):
                pbk = PB[nxt("pj", 2)]
                for kk in range(4):
                    k = k4 * 4 + kk
                    P.op("pe", lambda e, kk=kk, k=k: e.transpose(out=pbk.ap[:, kk * 128:(kk + 1) * 128], in_=h2.ap[:, k * 128:(k + 1) * 128], identity=ident.ap),
                         rd=R(h2, ident), wr=R(pbk))
                if k4 % 2 == 0:
                    P.op("act", lambda e: e.activation(out=h2T.ap[:, k4 * 4:k4 * 4 + 4, :], in_=pbk.ap.rearrange("p (k s) -> p k s", k=4), func=AF.Copy), rd=R(pbk), wr=R(h2T))
                else:
                    P.op("dve", lambda e: e.tensor_copy(out=h2T.ap[:, k4 * 4:k4 * 4 + 4, :], in_=pbk.ap.rearrange("p (k s) -> p k s", k=4)), rd=R(pbk), wr=R(h2T))
            pr = PB[nxt("pj", 2)]
            for k in range(16):
                P.op("pe", lambda e, k=k: e.matmul(out=pr.ap[:, 0:36], lhsT=h2T.ap[:, k, :], rhs=wr_t.ap[:, k, :], start=(k == 0), stop=False), rd=R(h2T, wr_t), wr=R(pr))
            P.op("pe", lambda e: e.matmul(out=pr.ap[:, 0:36], lhsT=ones_f.ap[0:1, :], rhs=brt.ap[0:1, :], start=False, stop=True), rd=R(ones_f, brt), wr=R(pr))
            lg = lg2[t % 2]
            P.op("dve", lambda e: e.tensor_copy(out=lg.ap[:, 0:36], in_=pr.ap[:, 0:36]), rd=R(pr), wr=R(lg))

        def ZA(t):
            route_a(P, lg2[t % 2], smr, gatew, b * NT + t)

        def ZB(t):
            gt = b * NT + t; hb = h2b[t % 3]
            route_b(P, smr, cnt_keep, eoff, ohbf, lowtri, ones_b, PB, nxt, slot_i, gt)
            for k in range(2):
                P.dma("pool", lambda e, k=k: e.indirect_dma_start(out=xs_d, out_offset=bass.IndirectOffsetOnAxis(ap=slot_i.ap[:, gt, k:k + 1], axis=0),
                                                                 in_=hb.ap, in_offset=None),
                      hb.r, rd=[hb.r, slot_i.r], wr=[r_xs])

        for stp in range(NT + 3):
            tx, ta, tb_, tz = stp, stp - 1, stp - 2, stp - 3
            okx = tx < NT; oka = 0 <= ta < NT; okb = 0 <= tb_ < NT; okz = 0 <= tz < NT
            if okb:
                YB(tb_)
            if okx:
                XL(tx)
            if okz:
                ZA(tz)
            if okx:
                XN(tx, 0)
            if okz:
                ZB(tz)
            if okx:
                XN(tx, 1)
            if oka:
                YA1(ta)
            if okx:
                XN(tx, 2)
            if oka:
                YA2(ta)
            if okx:
                XN(tx, 3); XC(tx)
        P.barrier()
    if dbg:
        A.off = base_off
        tmpb = A.bf(S)
        for b in range(NSEQ):
            for k in range(16):
                P.dma("sp", lambda e, b=b, k=k: e.dma_start(out=tmpb.ap, in_=ogT_d[b, k * 128:(k + 1) * 128, :]), tmpb.r, rd=[r_og[b]], wr=[tmpb.r])
                store("sp", dbg_d["ogT"][b, k * 128:(k + 1) * 128, :], tmpb)
        tmpf = A.f32(D)
        for gt in range(NTT):
            P.dma("sp", lambda e, gt=gt: e.dma_start(out=tmpf.ap, in_=x1_d[gt * 128:(gt + 1) * 128, :]), tmpf.r, rd=[r_x1[gt]], wr=[tmpf.r])
            store("sp", dbg_d["x1"][gt * 128:(gt + 1) * 128, :], tmpf)
        store("sp", dbg_d["slot"], slot_i, src_ap=slot_i.ap.rearrange("p t k -> p (t k)"))
        store("sp", dbg_d["gw"], gatew, src_ap=gatew.ap.rearrange("p t k -> p (t k)"))
        P.barrier()

    A.off = base_off
    wslot = [A.bf(16 * 512) for _ in range(6)]
    stg = [A.f32(2048) for _ in range(4)]
    xsb = [A.bf(D) for _ in range(4)]
    xsT = [A.bf(16 * CAP, "p (k s) -> p k s", k=16) for _ in range(2)]
    actT = [A.bf(4 * CAP, "p (f s) -> p f s", f=4) for _ in range(2)]
    sgt = [A.f32(CAP) for _ in range(2)]
    yt = [A.f32(D) for _ in range(2)]
    identb = A.bf(128)
    P.op("dve", lambda e: e.tensor_copy(out=identb.ap, in_=ident.ap), rd=R(ident), wr=R(identb))

    def cast_task(dst, c, src, pat, kw):
        def run():
            st = stg[nxt("stg", 4)]
            sv = st.ap.rearrange(pat, **kw) if pat else st.ap
            P.dma("sp", lambda e: e.dma_start(out=sv, in_=src), st.r, wr=[st.r])
            dv = dst.ap[:, c * 2048:(c + 1) * 2048]
            if nxt("casteng", 2) == 0:
                P.op("act", lambda e: e.activation(out=dv, in_=st.ap, func=AF.Copy), rd=R(st), wr=R(dst))
            else:
                P.op("dve", lambda e: e.tensor_copy(out=dv, in_=st.ap), rd=R(st), wr=R(dst))
        return run

    def expert_loads(ex):
        wg = wslot[nxt("ws", 6)]; wu = wslot[nxt("ws", 6)]; wdn = wslot[nxt("ws", 6)]
        gv = wg_d[ex].rearrange("(p k) f -> p k f", k=16); uv = wu_d[ex].rearrange("(p k) f -> p k f", k=16)
        dvw = wd_d[ex].rearrange("(k p) f -> p k f", p=128)
        tasks = []
        xbs = []
        for cb in range(CAPB):
            xb = xsb[nxt("xsb", 4)]
            xbs.append(xb)
            row0 = ex * CAP + cb * 128

            def xload(xb=xb, row0=row0):
                P.dma("sp", lambda e: e.dma_start(out=xb.ap, in_=xs_d[row0:row0 + 128, :]), xb.r, rd=[r_xs], wr=[xb.r])
            tasks.append(xload)
        for c in range(4):
            tasks.append(cast_task(wg, c, gv[:, 4 * c:4 * c + 4, :], "p (k f) -> p k f", dict(k=4)))
            tasks.append(cast_task(wu, c, uv[:, 4 * c:4 * c + 4, :], "p (k f) -> p k f", dict(k=4)))
        for c in range(4):
            tasks.append(cast_task(wdn, c, dvw[:, c, :], None, {}))
        return (wg, wu, wdn), xbs, tasks

    cur_w, cur_x, tasks0 = expert_loads(0)
    for tsk in tasks0:
        tsk()
    for ex in range(NEXP):
        wg, wu, wdn = cur_w
        xbs = cur_x
        if ex + 1 < NEXP:
            cur_w, cur_x, bg = expert_loads(ex + 1)
        else:
            bg = []

        def pump(n=1):
            for _ in range(n):
                if bg:
                    bg.pop(0)()

        wgv = wg.ap.rearrange("p (k f) -> p k f", k=16); wuv = wu.ap.rearrange("p (k f) -> p k f", k=16); wdv = wdn.ap.rearrange("p (k f) -> p k f", k=4)
        xT = xsT[ex % 2]; aT = actT[ex % 2]
        for cb in range(CAPB):
            xb = xbs[cb]
            for k8 in range(2):
                pbk = PB[nxt("pj", 2)]
                pv = pbk.ap.bitcast(BF16)
                for kk in range(8):
                    k = k8 * 8 + kk
                    P.op("pe", lambda e, pv=pv, kk=kk, k=k, xb=xb: e.transpose(out=pv[:, kk * 128:(kk + 1) * 128], in_=xb.ap.rearrange("s (p k) -> s k p", k=16)[:, k, :], identity=identb.ap),
                         rd=R(xb, identb), wr=R(pbk))
                dstv = xT.ap[:, k8 * 8:k8 * 8 + 8, cb * 128:(cb + 1) * 128]
                if k8 == 0:
                    P.op("act", lambda e, pv=pv, dstv=dstv: e.activation(out=dstv, in_=pv.rearrange("p (k s) -> p k s", k=8), func=AF.Copy), rd=R(pbk), wr=R(xT))
                else:
                    P.op("dve", lambda e, pv=pv, dstv=dstv: e.tensor_copy(out=dstv, in_=pv.rearrange("p (k s) -> p k s", k=8)), rd=R(pbk), wr=R(xT))
            pump(1)
        for fc in range(4):
            pg = PB[2 + nxt("pss", 2)]; pu = PB[4 + nxt("psd", 2)]
            for k in range(16):
                P.op("pe", lambda e, k=k, fc=fc, pg=pg: e.matmul(out=pg.ap[:, 0:CAP], lhsT=wgv[:, k, fc * 128:(fc + 1) * 128], rhs=xT.ap[:, k, :], start=(k == 0), stop=(k == 15)),
                     rd=R(wg, xT), wr=R(pg))
            pump(1)
            for k in range(16):
                P.op("pe", lambda e, k=k, fc=fc, pu=pu: e.matmul(out=pu.ap[:, 0:CAP], lhsT=wuv[:, k, fc * 128:(fc + 1) * 128], rhs=xT.ap[:, k, :], start=(k == 0), stop=(k == 15)),
                     rd=R(wu, xT), wr=R(pu))
            sgx = sgt[fc % 2]
            P.op("act", lambda e, pg=pg, sgx=sgx: e.activation(out=sgx.ap, in_=pg.ap[:, 0:CAP], func=AF.Silu), rd=R(pg), wr=R(sgx))
            P.op("dve", lambda e, pu=pu, sgx=sgx, fc=fc: e.tensor_tensor(out=aT.ap[:, fc, :], in0=pu.ap[:, 0:CAP], in1=sgx.ap, op=ALU.mult), rd=R(pu, sgx), wr=R(aT))
            pump(1)
        for cb in range(CAPB):
            y = yt[nxt("yt", 2)]
            for nb in range(4):
                py = PB[6 + nxt("pso", 2)]
                for fc in range(4):
                    P.op("pe", lambda e, fc=fc, nb=nb, cb=cb, py=py: e.matmul(out=py.ap, lhsT=aT.ap[:, fc, cb * 128:(cb + 1) * 128], rhs=wdv[:, fc, nb * 512:(nb + 1) * 512], start=(fc == 0), stop=(fc == 3)),
                         rd=R(aT, wdn), wr=R(py))
                if nb % 2 == 0:
                    P.op("act", lambda e, nb=nb, py=py, y=y: e.activation(out=y.ap[:, nb * 512:(nb + 1) * 512], in_=py.ap, func=AF.Copy), rd=R(py), wr=R(y))
                else:
                    P.op("dve", lambda e, nb=nb, py=py, y=y: e.tensor_copy(out=y.ap[:, nb * 512:(nb + 1) * 512], in_=py.ap), rd=R(py), wr=R(y))
                if nb % 2 == 1:
                    pump(1)
            row0 = ex * CAP + cb * 128
            store("act", ys_d[row0:row0 + 128, :], y, extra_wr=[r_ys[ex]])
        pump(len(bg))
    P.barrier()

    A.off = base_off
    G2 = [A.f32(D) for _ in range(NSEQ)]
    L2G = A.f32(D); L2B = A.f32(D)
    y1 = [A.f32(D) for _ in range(2)]; y2 = [A.f32(D) for _ in range(2)]; xx1 = [A.f32(D) for _ in range(2)]
    f2 = [A.f32(D) for _ in range(2)]; nrmc = [A.f32(D) for _ in range(2)]; o_out = [A.f32(D) for _ in range(2)]
    st6 = A.f32(24); mv = A.f32(2); sm = [A.f32(40) for _ in range(12)]
    for b in range(NSEQ):
        load("sp", G2[b], bcast_row(mod_d[b:b + 1, 5 * D:6 * D]), extra_rd=[r_mod])
        P.op("pool", lambda e, b=b: e.tensor_scalar_add(out=G2[b].ap, in0=G2[b].ap, scalar1=1.0), rd=R(G2[b]), wr=R(G2[b]))
    load("sp", L2G, bcast_row(ln2g_d)); load("sp", L2B, bcast_row(ln2b_d))

    def CL(gt):
        ya = y1[gt % 2]; yb = y2[gt % 2]; xa = xx1[gt % 2]
        P.dma("pool", lambda e: e.indirect_dma_start(out=ya.ap, out_offset=None, in_=ys_d, in_offset=bass.IndirectOffsetOnAxis(ap=slot_i.ap[:, gt, 0:1], axis=0)),
              ya.r, rd=R(slot_i) + r_ys, wr=[ya.r])
        P.dma("pool", lambda e: e.indirect_dma_start(out=yb.ap, out_offset=None, in_=ys_d, in_offset=bass.IndirectOffsetOnAxis(ap=slot_i.ap[:, gt, 1:2], axis=0)),
              yb.r, rd=R(slot_i) + r_ys, wr=[yb.r])
        P.dma("sp", lambda e: e.dma_start(out=xa.ap, in_=x1_d[gt * 128:(gt + 1) * 128, :]), xa.r, rd=[r_x1[gt]], wr=[xa.r])

    def C1a(gt):
        ya = y1[gt % 2]; yb = y2[gt % 2]; f_t = f2[gt % 2]; b = gt // NT
        P.op("act", lambda e: e.activation(out=f_t.ap, in_=ya.ap, func=AF.Copy, scale=gatew.ap[:, gt, 0:1]), rd=R(ya, gatew), wr=R(f_t))
        P.op("dve", lambda e: e.scalar_tensor_tensor(out=f_t.ap, in0=yb.ap, scalar=gatew.ap[:, gt, 1:2], in1=f_t.ap, op0=ALU.mult, op1=ALU.add), rd=R(yb, gatew, f_t), wr=R(f_t))
        P.op("dve", lambda e: e.tensor_tensor(out=f_t.ap, in0=f_t.ap, in1=G2[b].ap, op=ALU.mult), rd=R(f_t, G2[b]), wr=R(f_t))

    def C1b(gt):
        xa = xx1[gt % 2]; f_t = f2[gt % 2]
        P.op("dve", lambda e: e.scalar_tensor_tensor(out=f_t.ap, in0=xa.ap, scalar=ALPHA, in1=f_t.ap, op0=ALU.mult, op1=ALU.add), rd=R(xa, f_t), wr=R(f_t))

    def C2a(gt):
        layer_norm(P, f2[gt % 2], nrmc[gt % 2], st6, mv, sm)

    def C2b(gt):
        oo = o_out[gt % 2]; nrm = nrmc[gt % 2]
        P.op("dve", lambda e: e.tensor_tensor(out=oo.ap, in0=nrm.ap, in1=L2G.ap, op=ALU.mult), rd=R(nrm, L2G), wr=R(oo))
        P.op("dve", lambda e: e.tensor_tensor(out=oo.ap, in0=oo.ap, in1=L2B.ap, op=ALU.add), rd=R(oo, L2B), wr=R(oo))
        store("sp", out_d[gt * 128:(gt + 1) * 128, :], oo)

    CL(0)
    for stp in range(NTT + 2):
        if stp + 1 < NTT:
            CL(stp + 1)
        if stp < NTT:
            C1a(stp)
        if 0 <= stp - 1 < NTT:
            C2a(stp - 1)
        if stp < NTT:
            C1b(stp)
        if 0 <= stp - 2 < NTT:
            C2b(stp - 2)
    P.final_wait("sp")
    stats = P.emit()
    return nc, stats


def layer_norm(P, y, nrm, st6, mv, sm):
    R = lambda *ts: [t.r for t in ts]
    for i in range(4):
        P.op("dve", lambda e, i=i: e.bn_stats(out=st6.ap[:, i * 6:(i + 1) * 6], in_=y.ap[:, i * 512:(i + 1) * 512]), rd=R(y), wr=R(st6))
    P.op("dve", lambda e: e.bn_aggr(out=mv.ap, in_=st6.ap), rd=R(st6), wr=R(mv))
    rs = sm[10]; nm = sm[11]
    P.op("act", lambda e: e.activation(out=rs.ap[:, 0:1], in_=mv.ap[:, 1:2], func=AF.Ln, bias=EPS, scale=1.0), rd=R(mv), wr=R(rs))
    P.op("act", lambda e: e.activation(out=rs.ap[:, 0:1], in_=rs.ap[:, 0:1], func=AF.Exp, scale=-0.5), rd=R(rs), wr=R(rs))
    P.op("dve", lambda e: e.scalar_tensor_tensor(out=nm.ap[:, 0:1], in0=mv.ap[:, 0:1], scalar=-1.0, in1=rs.ap[:, 0:1], op0=ALU.mult, op1=ALU.mult), rd=R(mv, rs), wr=R(nm))
    P.op("act", lambda e: e.activation(out=nrm.ap, in_=y.ap, func=AF.Identity, scale=rs.ap[:, 0:1], bias=nm.ap[:, 0:1]), rd=R(y, rs, nm), wr=R(nrm))


def route_a(P, lgt, sm, gatew, gt):
    R = lambda *ts: [t.r for t in ts]
    lg = lgt
    _, gmx, goh, gpen, elm, m1, oh1, elm2, m2, oh2 = sm[0:10]
    tmp = sm[10]
    dv = lambda op, rd, wr: P.op("dve", op, rd=R(*rd), wr=R(*wr))
    dv(lambda e: e.reduce_max(out=gmx.ap[:, 0:1], in_=lg.ap[:, 0:4], axis=AX.X), [lg], [gmx])
    dv(lambda e: e.tensor_scalar(out=goh.ap[:, 0:4], in0=lg.ap[:, 0:4], scalar1=gmx.ap[:, 0:1], scalar2=None, op0=ALU.is_equal), [lg, gmx], [goh])
    dv(lambda e: e.tensor_scalar(out=gmx.ap[:, 1:2], in0=gmx.ap[:, 0:1], scalar1=-1.0, scalar2=None, op0=ALU.mult), [gmx], [gmx])
    P.op("act", lambda e: e.activation(out=tmp.ap[:, 0:4], in_=lg.ap[:, 0:4], func=AF.Exp, bias=gmx.ap[:, 1:2], scale=1.0, accum_out=tmp.ap[:, 4:5]), rd=R(lg, gmx), wr=R(tmp))
    dv(lambda e: e.tensor_scalar(out=gpen.ap[:, 0:4], in0=goh.ap[:, 0:4], scalar1=-1.0, scalar2=BIG, op0=ALU.add, op1=ALU.mult), [goh], [gpen])
    gp_b = bass.AP(gpen.ap.tensor, gpen.ap.offset, [[gpen.ap.ap[0][0], 128], [1, 4], [0, 8]])
    dv(lambda e: e.tensor_tensor(out=elm.ap[:, 0:32].rearrange("p (g j) -> p g j", g=4), in0=lg.ap[:, 4:36].rearrange("p (g j) -> p g j", g=4), in1=gp_b, op=ALU.add), [lg, gpen], [elm])
    dv(lambda e: e.reduce_max(out=m1.ap[:, 0:1], in_=elm.ap[:, 0:32], axis=AX.X), [elm], [m1])
    dv(lambda e: e.tensor_scalar(out=oh1.ap[:, 0:32], in0=elm.ap[:, 0:32], scalar1=m1.ap[:, 0:1], scalar2=None, op0=ALU.is_equal), [elm, m1], [oh1])
    dv(lambda e: e.scalar_tensor_tensor(out=elm2.ap[:, 0:32], in0=oh1.ap[:, 0:32], scalar=-BIG, in1=elm.ap[:, 0:32], op0=ALU.mult, op1=ALU.add), [oh1, elm], [elm2])
    dv(lambda e: e.reduce_max(out=m2.ap[:, 0:1], in_=elm2.ap[:, 0:32], axis=AX.X), [elm2], [m2])
    dv(lambda e: e.tensor_scalar(out=oh2.ap[:, 0:32], in0=elm2.ap[:, 0:32], scalar1=m2.ap[:, 0:1], scalar2=None, op0=ALU.is_equal), [elm2, m2], [oh2])
    dv(lambda e: e.tensor_tensor(out=m2.ap[:, 1:2], in0=m2.ap[:, 0:1], in1=m1.ap[:, 0:1], op=ALU.subtract), [m2, m1], [m2])
    P.op("act", lambda e: e.activation(out=m2.ap[:, 2:3], in_=m2.ap[:, 1:2], func=AF.Exp), rd=R(m2), wr=R(m2))
    dv(lambda e: e.reciprocal(out=tmp.ap[:, 5:6], in_=tmp.ap[:, 4:5]), [tmp], [tmp])
    dv(lambda e: e.tensor_scalar_add(out=m2.ap[:, 3:4], in0=m2.ap[:, 2:3], scalar1=1.0), [m2], [m2])
    dv(lambda e: e.reciprocal(out=m2.ap[:, 4:5], in_=m2.ap[:, 3:4]), [m2], [m2])
    dv(lambda e: e.tensor_tensor(out=gatew.ap[:, gt, 0:1], in0=m2.ap[:, 4:5], in1=tmp.ap[:, 5:6], op=ALU.mult), [m2, tmp], [gatew])
    dv(lambda e: e.tensor_tensor(out=gatew.ap[:, gt, 1:2], in0=gatew.ap[:, gt, 0:1], in1=m2.ap[:, 2:3], op=ALU.mult), [gatew, m2], [gatew])


def route_b(P, sm, cnt, eoff, ohbf, lowtri, ones_b, PB, nxt, slot_i, gt):
    R = lambda *ts: [t.r for t in ts]
    m1 = sm[5]; oh1 = sm[6]; oh2 = sm[9]; tmp2 = sm[11]
    dv = lambda op, rd, wr: P.op("dve", op, rd=R(*rd), wr=R(*wr))
    dv(lambda e: e.tensor_tensor(out=ohbf.ap[:, 0:32], in0=oh1.ap[:, 0:32], in1=oh2.ap[:, 0:32], op=ALU.add), [oh1, oh2], [ohbf])
    pp = PB[nxt("pj", 2)]
    P.op("pe", lambda e: e.matmul(out=pp.ap[:, 0:32], lhsT=lowtri.ap, rhs=ohbf.ap[:, 0:32], start=True, stop=True), rd=R(lowtri, ohbf), wr=R(pp))
    P.op("pe", lambda e: e.matmul(out=pp.ap[:, 32:64], lhsT=ones_b.ap, rhs=ohbf.ap[:, 0:32], start=True, stop=True, skip_group_check=True), rd=R(ones_b, ohbf), wr=R(pp))
    dv(lambda e: e.tensor_tensor(out=tmp2.ap[:, 0:32], in0=cnt.ap, in1=eoff.ap, op=ALU.add), [cnt, eoff], [tmp2])
    dv(lambda e: e.tensor_tensor(out=tmp2.ap[:, 0:32], in0=pp.ap[:, 0:32], in1=tmp2.ap[:, 0:32], op=ALU.add), [pp, tmp2], [tmp2])
    dv(lambda e: e.tensor_tensor(out=cnt.ap, in0=pp.ap[:, 32:64], in1=cnt.ap, op=ALU.add), [pp, cnt], [cnt])
    dv(lambda e: e.tensor_tensor(out=oh1.ap[:, 0:32], in0=oh1.ap[:, 0:32], in1=tmp2.ap[:, 0:32], op=ALU.mult), [oh1, tmp2], [oh1])
    dv(lambda e: e.tensor_tensor(out=oh2.ap[:, 0:32], in0=oh2.ap[:, 0:32], in1=tmp2.ap[:, 0:32], op=ALU.mult), [oh2, tmp2], [oh2])
    dv(lambda e: e.reduce_sum(out=m1.ap[:, 1:2], in_=oh1.ap[:, 0:32], axis=AX.X), [oh1], [m1])
    dv(lambda e: e.reduce_sum(out=m1.ap[:, 2:3], in_=oh2.ap[:, 0:32], axis=AX.X), [oh2], [m1])
    dv(lambda e: e.tensor_copy(out=slot_i.ap[:, gt, 0:2], in_=m1.ap[:, 1:3]), [m1], [slot_i])


_NC_CACHE = {}


def make_in_map(inputs, core, ncores, S, NSEQ, CAPB):
    f = lambda a: np.ascontiguousarray(np.asarray(a, dtype=np.float32))
    x = np.asarray(inputs["x"]); c = np.asarray(inputs["c"])
    b0 = core * NSEQ
    m = {}
    m["x"] = f(x[b0:b0 + NSEQ, :S]).reshape(NSEQ * S, D)
    m["c"] = f(c[b0:b0 + NSEQ])
    m["w_in"] = f(inputs["w_in"][0]); m["w_out"] = f(inputs["w_out"][0])
    sinks = np.asarray(inputs["sinks"][0], np.float32)
    m["sinks_fm"] = f(np.stack([sinks[2 * j + (np.arange(128) // 64)] for j in range(8)], axis=1))
    m["rel_bias"] = f(inputs["rel_bias"])
    m["na_fm"] = f(np.asarray(inputs["norm_a"][0]).reshape(8, 128).T)
    m["nb_fm"] = f(np.asarray(inputs["norm_b"][0]).reshape(8, 128).T)
    m["w_ada"] = f(inputs["w_ada"][0]); m["b_ada"] = f(inputs["b_ada"][0]).reshape(1, 6 * D)
    for k in ("ln1_g", "ln1_b", "ln2_g", "ln2_b"):
        m[k] = f(inputs[k][0]).reshape(1, D)
    m["w_grp"] = f(inputs["w_grp"][0]); m["b_grp"] = f(inputs["b_grp"][0]).reshape(1, 4)
    m["w_rtr"] = f(inputs["w_rtr"][0]); m["b_rtr"] = f(inputs["b_rtr"][0]).reshape(1, 32)
    m["w_gate"] = f(inputs["w_gate"][0]); m["w_up"] = f(inputs["w_up"][0]); m["w_down"] = f(inputs["w_down"][0])
    m.update(host_consts(CAPB * 128))
    return m


def kernel(**inputs):
    NCORES = 8
    S, NSEQ, CAPB = 2048, 2, 3
    key = (S, NSEQ, CAPB)
    if key not in _NC_CACHE:
        _NC_CACHE[key] = build_nc(S, NSEQ, CAPB)[0]
    nc = _NC_CACHE[key]
    shared = make_in_map(inputs, 0, NCORES, S, NSEQ, CAPB)
    in_maps = []
    for core in range(NCORES):
        m = dict(shared)
        b0 = core * NSEQ
        m["x"] = np.ascontiguousarray(np.asarray(inputs["x"], np.float32)[b0:b0 + NSEQ]).reshape(NSEQ * S, D)
        m["c"] = np.ascontiguousarray(np.asarray(inputs["c"], np.float32)[b0:b0 + NSEQ])
        in_maps.append(m)
    res = run_bass_kernel_spmd(nc, in_maps, core_ids=list(range(NCORES)))
    out = np.concatenate([np.asarray(r["out"]).reshape(NSEQ, S, D) for r in res.results], axis=0)
    return out.astype(np.float32)
```

```python
import math
import types
import numpy as np
import ml_dtypes
import concourse.bass as bass
import concourse.mybir as mybir
from concourse.bass_utils import run_bass_kernel_spmd

F32 = mybir.dt.float32
BF16 = mybir.dt.bfloat16
I32 = mybir.dt.int32
U32 = mybir.dt.uint32
AF = mybir.ActivationFunctionType
ALU = mybir.AluOpType
AX = mybir.AxisListType

ENGS = ("pe", "act", "dve", "pool", "sp")


class Res:
    __slots__ = ("name", "w", "r", "excl", "dsem")

    def __init__(self, name, excl=False):
        self.name = name
        self.w = None
        self.r = []
        self.excl = excl
        self.dsem = None


class Tok:
    __slots__ = ("kind", "eng", "seq", "sem", "val")

    def __init__(self, kind, eng=None, seq=None, sem=None, val=None):
        self.kind = kind; self.eng = eng; self.seq = seq; self.sem = sem; self.val = val


class Op:
    __slots__ = ("fn", "waits", "signal", "dma_sem", "seq")

    def __init__(self, fn):
        self.fn = fn; self.waits = []; self.signal = False; self.dma_sem = None; self.seq = 0


def _freeze(fn):
    if fn is None or fn.__closure__ is None:
        return fn
    cells = []
    for c in fn.__closure__:
        try:
            cells.append(types.CellType(c.cell_contents))
        except ValueError:
            cells.append(c)
    return types.FunctionType(fn.__code__, fn.__globals__, fn.__name__, fn.__defaults__, tuple(cells))


class Prog:
    def __init__(self, nc):
        self.nc = nc
        self.ops = {e: [] for e in ENGS}
        self.seen = {e: {} for e in ENGS}
        self.dsems = []
        self.nres = 0
        self.ncomp = {e: 0 for e in ENGS}
        self.lastc = {e: None for e in ENGS}

    def res(self, name=None, excl=False):
        self.nres += 1
        return Res(name or f"r{self.nres}", excl)

    def _need(self, eng, op, tok, same_eng_raw=False):
        if tok is None:
            return
        if tok.kind == "e":
            if tok.eng == eng:
                if not same_eng_raw or eng == "pe":
                    return
                if tok.val < self.ncomp[eng] - 1:
                    return
            key = ("e", tok.eng)
            if self.seen[eng].get(key, 0) >= tok.seq:
                return
            self.seen[eng][key] = tok.seq
            self.ops[tok.eng][tok.seq - 1].signal = True
            op.waits.append(tok)
        else:
            key = ("d", tok.sem)
            if self.seen[eng].get(key, 0) >= tok.val:
                return
            self.seen[eng][key] = tok.val
            op.waits.append(tok)

    def _deps(self, eng, op, rd, wr):
        for r in rd:
            if r.excl:
                wr = list(wr) + [r]
                continue
            self._need(eng, op, r.w, same_eng_raw=True)
        for w in wr:
            self._need(eng, op, w.w, same_eng_raw=False)
            for t in w.r:
                self._need(eng, op, t)

    def _commit(self, tok, rd, wr):
        for r in rd:
            if r.excl:
                r.w = tok; r.r = []
            else:
                r.r.append(tok)
        for w in wr:
            w.w = tok; w.r = []

    def op(self, eng, fn, rd=(), wr=()):
        o = Op(_freeze(fn))
        self._deps(eng, o, rd, wr)
        self.ops[eng].append(o)
        o.seq = len(self.ops[eng])
        self.ncomp[eng] += 1
        tok = Tok("e", eng=eng, seq=o.seq, val=self.ncomp[eng])
        self.lastc[eng] = tok
        self._commit(tok, rd, wr)
        return tok

    def dma(self, eng, fn, sb, rd=(), wr=()):
        if sb.dsem is None:
            sb.dsem = len(self.dsems)
            self.dsems.append([0])
        o = Op(_freeze(fn))
        cur = self.dsems[sb.dsem][0]
        if cur:
            self._need(eng, o, Tok("d", sem=sb.dsem, val=cur))
        self._deps(eng, o, rd, wr)
        self.dsems[sb.dsem][0] = cur + 16
        o.dma_sem = sb.dsem
        self.ops[eng].append(o)
        o.seq = len(self.ops[eng])
        tok = Tok("d", sem=sb.dsem, val=cur + 16)
        self._commit(tok, rd, wr)
        return tok

    def barrier(self, all_res=()):
        toks = []
        for e in ENGS:
            if self.lastc[e] is not None:
                toks.append(self.lastc[e])
        for i, v in enumerate(self.dsems):
            if v[0]:
                toks.append(Tok("d", sem=i, val=v[0]))
        for e in ENGS:
            o = Op(None)
            for t in toks:
                if t.kind == "e" and t.eng == e:
                    continue
                self._need(e, o, t)
            if o.waits:
                self.ops[e].append(o)
                o.seq = len(self.ops[e])

    def final_wait(self, eng="sp"):
        o = Op(None)
        for i, v in enumerate(self.dsems):
            if v[0]:
                self._need(eng, o, Tok("d", sem=i, val=v[0]))
        for e in ENGS:
            if e != eng and self.lastc[e] is not None:
                self._need(eng, o, self.lastc[e])
        self.ops[eng].append(o)
        o.seq = len(self.ops[eng])

    def emit(self):
        nc = self.nc
        esem = {e: nc.alloc_semaphore(name=f"sem_{e}") for e in ENGS}
        dsem = [nc.alloc_semaphore(name=f"dsem{i}") for i in range(len(self.dsems))]
        sigval = {}
        for e in ENGS:
            c = 0
            vals = []
            for o in self.ops[e]:
                if o.signal:
                    c += 1
                vals.append(c)
            sigval[e] = vals
        ops = self.ops
        stats = {e: (len(ops[e]), sigval[e][-1] if sigval[e] else 0) for e in ENGS}

        def run(engname, eng):
            for o in ops[engname]:
                for t in o.waits:
                    if t.kind == "e":
                        eng.wait_ge(esem[t.eng], sigval[t.eng][t.seq - 1])
                    else:
                        eng.wait_ge(dsem[t.sem], t.val)
                if o.fn is None:
                    continue
                ins = o.fn(eng)
                if o.dma_sem is not None:
                    ins.then_inc(dsem[o.dma_sem], 16)
                elif o.signal:
                    ins.then_inc(esem[engname], 1)

        with nc.Block() as block:
            @block.tensor
            def _(e):
                run("pe", e)

            @block.scalar
            def _(e):
                run("act", e)

            @block.vector
            def _(e):
                run("dve", e)

            @block.gpsimd
            def _(e):
                run("pool", e)

            @block.sync
            def _(e):
                run("sp", e)
        return stats


D = 2048
HD = 64
QKV = 4352
NEXP = 32
DE = 512
ALPHA = 2.0 ** 0.25
EPS = 1e-5
BIG = 30000.0
CA_Q, CA_K, CA_V, CB_Q, CB_K, CB_V = 0, 1024, 1152, 1280, 2304, 3328


class T:
    __slots__ = ("ap", "r")

    def __init__(self, ap, r):
        self.ap = ap; self.r = r


class Arena:
    def __init__(self, P, tensor, nwords):
        self.P = P; self.t = tensor; self.n = nwords; self.off = 0

    def _take(self, words):
        a = self.t[:, self.off:self.off + words]
        self.off += words
        assert self.off <= self.n, f"arena overflow {self.off} > {self.n}"
        return a

    def f32(self, cols, pat=None, **kw):
        a = self._take(cols)
        return T(a.rearrange(pat, **kw) if pat else a, self.P.res())

    def i32(self, cols, pat=None, **kw):
        a = self._take(cols).bitcast(I32)
        return T(a.rearrange(pat, **kw) if pat else a, self.P.res())

    def bf(self, cols, pat=None, **kw):
        a = self._take((cols + 1) // 2).bitcast(BF16)[:, 0:cols]
        return T(a.rearrange(pat, **kw) if pat else a, self.P.res())


def host_consts(CAP):
    c = {}
    c["eoff"] = np.tile((np.arange(32, dtype=np.float32) * CAP)[None, :], (128, 1))
    c["ident"] = np.eye(128, dtype=np.float32)
    j = np.arange(128)[:, None]; s_ = np.arange(128)[None, :]
    c["negtri"] = np.where(j >= s_, -1.0, 0.0).astype(ml_dtypes.bfloat16)
    c["negsu"] = np.where(j < s_, -1.0, 0.0).astype(ml_dtypes.bfloat16)
    c["lowtri"] = np.where(j < s_, 1.0, 0.0).astype(ml_dtypes.bfloat16)
    tq = np.arange(512)[None, :]
    c["sbmask"] = np.stack([(jj * 128 + np.arange(128)[:, None] < tq) for jj in range(4)], axis=1).astype(np.float32)
    oh = np.zeros((128, 2, 128), np.float32); oh[:, 0, 0:64] = 1.0; oh[:, 1, 64:128] = 1.0
    c["oneshalf"] = oh.astype(ml_dtypes.bfloat16)
    def bucket(n):
        n = max(n, 0); me = 16
        if n < me:
            return n
        ratio = np.float32(max(n, me)) / np.float32(me)
        large = me + int(np.float32(np.log(ratio) / np.float32(math.log(128 / me))) * (32 - me))
        return min(large, 31)
    ohb = np.zeros((32, 512), np.float32); neg = np.zeros((1, 512), np.float32)
    for i in range(256):
        if i <= 127:
            ohb[bucket(127 - i), i] = 1.0
        else:
            neg[0, i] = -BIG
        if 128 <= i <= 254:
            ohb[bucket(255 - i), 256 + i] = 1.0
        else:
            neg[0, 256 + i] = -BIG
    c["ohb"] = ohb; c["negrow"] = neg
    return c


def build_nc(S=2048, NSEQ=2, CAPB=3, dbg=False):
    nc = bass.Bass("TRN2", target_bir_lowering=False)
    TK = NSEQ * S
    NT = S // 128
    NQ = S // 512
    NTT = TK // 128
    CAP = CAPB * 128
    NSLOT = NEXP * CAP

    def din(name, shape, dt=F32):
        return nc.dram_tensor(name, list(shape), dt, kind="ExternalInput").ap()

    x_d = din("x", [TK, D]); c_d = din("c", [NSEQ, D]); w_in_d = din("w_in", [D, QKV]); w_out_d = din("w_out", [D, D])
    sinks_d = din("sinks_fm", [128, 8]); relb_d = din("rel_bias", [32, 16])
    na_d = din("na_fm", [128, 8]); nb_d = din("nb_fm", [128, 8])
    w_ada_d = din("w_ada", [D, 6 * D]); b_ada_d = din("b_ada", [1, 6 * D])
    ln1g_d = din("ln1_g", [1, D]); ln1b_d = din("ln1_b", [1, D]); ln2g_d = din("ln2_g", [1, D]); ln2b_d = din("ln2_b", [1, D])
    wgrp_d = din("w_grp", [D, 4]); bgrp_d = din("b_grp", [1, 4]); wrtr_d = din("w_rtr", [D, 32]); brtr_d = din("b_rtr", [1, 32])
    wg_d = din("w_gate", [NEXP, D, DE]); wu_d = din("w_up", [NEXP, D, DE]); wd_d = din("w_down", [NEXP, DE, D])
    ident_d = din("ident", [128, 128]); negtri_d = din("negtri", [128, 128], BF16); negsu_d = din("negsu", [128, 128], BF16)
    lowtri_d = din("lowtri", [128, 128], BF16); sbmask_d = din("sbmask", [128, 4, 512]); oneshalf_d = din("oneshalf", [128, 2, 128], BF16)
    ohb_d = din("ohb", [32, 512]); negrow_d = din("negrow", [1, 512]); eoff_d = din("eoff", [128, 32])
    out_d = nc.dram_tensor("out", [TK, D], F32, kind="ExternalOutput").ap()

    def dscr(name, shape, dt):
        return nc.dram_tensor(name, list(shape), dt, kind="Internal").ap()

    mod_d = dscr("mod_scr", [NSEQ, 6 * D], F32)
    gb_d = dscr("gb_scr", [16, 512], F32)
    ogT_d = dscr("ogT_scr", [NSEQ, D, S], BF16)
    x1_d = dscr("x1_scr", [TK, D], F32)
    xs_d = dscr("xs_scr", [NSLOT, D], BF16)
    ys_d = dscr("ys_scr", [NSLOT, D], F32)
    dbg_d = {}
    if dbg:
        dbg_d["ogT"] = nc.dram_tensor("dbg_ogT", [NSEQ, D, S], BF16, kind="ExternalOutput").ap()
        dbg_d["x1"] = nc.dram_tensor("dbg_x1", [TK, D], F32, kind="ExternalOutput").ap()
        dbg_d["mod"] = nc.dram_tensor("dbg_mod", [NSEQ, 6 * D], F32, kind="ExternalOutput").ap()
        dbg_d["ssq"] = nc.dram_tensor("dbg_ssq", [NSEQ, 128, 2 * NT], F32, kind="ExternalOutput").ap()
        dbg_d["slot"] = nc.dram_tensor("dbg_slot", [128, NTT * 2], I32, kind="ExternalOutput").ap()
        dbg_d["gw"] = nc.dram_tensor("dbg_gw", [128, NTT * 2], F32, kind="ExternalOutput").ap()
        dbg_d["eb"] = nc.dram_tensor("dbg_eb", [128, 4096], BF16, kind="ExternalOutput").ap()
        for nm_ in ("den", "o", "pcf", "ppf", "psd"):
            dbg_d[nm_] = nc.dram_tensor("dbg_" + nm_, [2, 128, 512], F32, kind="ExternalOutput").ap()

    NW = 206 * 256
    arena_t = nc.alloc_sbuf_tensor("arena", [128, NW], F32)
    psum_t = nc.alloc_psum_tensor("psum", [128, 8 * 512], F32)
    P = Prog(nc)
    A = Arena(P, arena_t, NW)
    PB = [T(psum_t[:, i * 512:(i + 1) * 512], P.res(f"bank{i}", excl=True)) for i in range(8)]
    rot = {}

    def nxt(key, n):
        rot[key] = (rot.get(key, -1) + 1) % n
        return rot[key]

    def R(*ts):
        return [t.r for t in ts]

    r_mod = P.res("mod_d"); r_gb = P.res("gb_d"); r_xs = P.res("xs_d")
    r_og = [P.res(f"og_d{b}") for b in range(NSEQ)]
    r_x1 = [P.res(f"x1_d{t}") for t in range(NTT)]
    r_ys = [P.res(f"ys_d{e}") for e in range(NEXP)]

    def load(eng, t, src, extra_rd=()):
        return P.dma(eng, lambda e: e.dma_start(out=t.ap, in_=src), t.r, rd=list(extra_rd), wr=[t.r])

    def store(eng, dst, t, src_ap=None, extra_wr=()):
        sa = t.ap if src_ap is None else src_ap
        return P.dma(eng, lambda e: e.dma_start(out=dst, in_=sa), t.r, rd=[t.r], wr=list(extra_wr))

    ident = A.f32(128); negtri = A.bf(128); negsu = A.bf(128); lowtri = A.bf(128)
    onesh = A.bf(256, "p (r c) -> p r c", r=2)
    ones_f = A.f32(128); ones_b = A.bf(128)
    na_fm = A.f32(8); nb_fm = A.f32(8); esink = A.f32(8)
    slot_i = A.i32(NTT * 2, "p (t k) -> p t k", k=2)
    gatew = A.f32(NTT * 2, "p (t k) -> p t k", k=2)
    ssq = A.f32(2 * NT, "p (g t) -> p g t", g=2)
    cnt_keep = A.f32(32); eoff = A.f32(32)
    load("sp", eoff, eoff_d)
    P.op("pool", lambda e: e.memset(cnt_keep.ap, 0.0), wr=R(cnt_keep))
    load("sp", ident, ident_d); load("sp", negtri, negtri_d); load("sp", negsu, negsu_d); load("sp", lowtri, lowtri_d)
    load("sp", onesh, oneshalf_d); load("sp", na_fm, na_d); load("sp", nb_fm, nb_d); load("sp", esink, sinks_d)
    P.op("pool", lambda e: e.memset(ones_f.ap, 1.0), wr=R(ones_f))
    P.op("pool", lambda e: e.memset(ones_b.ap, 1.0), wr=R(ones_b))
    P.op("act", lambda e: e.activation(out=esink.ap, in_=esink.ap, func=AF.Exp), rd=R(esink), wr=R(esink))
    base_off = A.off

    ct = A.f32(16 * NSEQ, "p (k b) -> p k b", b=NSEQ)
    sg = A.f32(16 * NSEQ, "p (k b) -> p k b", b=NSEQ)
    with nc.allow_non_contiguous_dma(reason="tiny transposed load of c"):
        for bb in range(NSEQ):
            P.dma("sp", lambda e, bb=bb: e.dma_start(out=ct.ap[:, :, bb:bb + 1], in_=c_d[bb:bb + 1, :].rearrange("o (k p) -> p k o", p=128), allow_slow_non_contiguous=True), ct.r, wr=[ct.r])
    P.op("act", lambda e: e.activation(out=sg.ap, in_=ct.ap, func=AF.Exp, scale=-1.0), rd=R(ct), wr=R(sg))
    P.op("dve", lambda e: e.tensor_scalar_add(out=sg.ap, in0=sg.ap, scalar1=1.0), rd=R(sg), wr=R(sg))
    P.op("dve", lambda e: e.reciprocal(out=sg.ap, in_=sg.ap), rd=R(sg), wr=R(sg))
    P.op("dve", lambda e: e.tensor_tensor(out=ct.ap, in0=ct.ap, in1=sg.ap, op=ALU.mult), rd=R(ct, sg), wr=R(ct))
    wa = [A.f32(4096) for _ in range(3)]
    brow = A.f32(4096); mrow = A.f32(4096)
    w_ada_v = w_ada_d.rearrange("(k p) n -> p k n", p=128)
    for third in range(3):
        c0 = third * 4096
        P.dma("sp", lambda e, c0=c0: e.dma_start(out=brow.ap[0:1, :], in_=b_ada_d[:, c0:c0 + 4096]), brow.r, wr=[brow.r])
        for k in range(16):
            w = wa[nxt("wa", 3)]
            load("sp", w, w_ada_v[:, k, c0:c0 + 4096])
            for n8 in range(8):
                pb = PB[n8]
                P.op("pe", lambda e, k=k, w=w, pb=pb, n8=n8: e.matmul(out=pb.ap[0:NSEQ, :], lhsT=ct.ap[:, k, :], rhs=w.ap[:, n8 * 512:(n8 + 1) * 512], start=(k == 0), stop=False),
                     rd=R(ct, w), wr=R(pb))
        for n8 in range(8):
            pb = PB[n8]
            P.op("pe", lambda e, pb=pb, n8=n8: e.matmul(out=pb.ap[0:NSEQ, :], lhsT=ones_f.ap[0:1, 0:NSEQ], rhs=brow.ap[0:1, n8 * 512:(n8 + 1) * 512], start=False, stop=True),
                 rd=R(ones_f, brow), wr=R(pb))
            if n8 % 2 == 0:
                P.op("dve", lambda e, pb=pb, n8=n8: e.tensor_copy(out=mrow.ap[0:NSEQ, n8 * 512:(n8 + 1) * 512], in_=pb.ap[0:NSEQ, :]), rd=R(pb), wr=R(mrow))
            else:
                P.op("act", lambda e, pb=pb, n8=n8: e.activation(out=mrow.ap[0:NSEQ, n8 * 512:(n8 + 1) * 512], in_=pb.ap[0:NSEQ, :], func=AF.Copy), rd=R(pb), wr=R(mrow))
        store("sp", mod_d[:, c0:c0 + 4096], mrow, src_ap=mrow.ap[0:NSEQ, :], extra_wr=[r_mod])
    relb = A.f32(16); ohb = A.f32(512); negrow = A.f32(512); grow = A.f32(512)
    P.dma("sp", lambda e: e.dma_start(out=relb.ap[0:32, :], in_=relb_d), relb.r, wr=[relb.r])
    P.dma("sp", lambda e: e.dma_start(out=ohb.ap[0:32, :], in_=ohb_d), ohb.r, wr=[ohb.r])
    P.dma("sp", lambda e: e.dma_start(out=negrow.ap[0:1, :], in_=negrow_d), negrow.r, wr=[negrow.r])
    pb = PB[nxt("pj", 2)]
    P.op("pe", lambda e, pb=pb: e.matmul(out=pb.ap[0:16, :], lhsT=relb.ap[0:32, :], rhs=ohb.ap[0:32, :], start=True, stop=False), rd=R(relb, ohb), wr=R(pb))
    P.op("pe", lambda e, pb=pb: e.matmul(out=pb.ap[0:16, :], lhsT=ones_f.ap[0:1, 0:16], rhs=negrow.ap[0:1, :], start=False, stop=True), rd=R(ones_f, negrow), wr=R(pb))
    P.op("dve", lambda e, pb=pb: e.tensor_copy(out=grow.ap[0:16, :], in_=pb.ap[0:16, :]), rd=R(pb), wr=R(grow))
    store("sp", gb_d, grow, src_ap=grow.ap[0:16, :], extra_wr=[r_gb])
    if dbg:
        P.barrier()
        mt = A.f32(6 * D)
        P.dma("sp", lambda e: e.dma_start(out=mt.ap[0:NSEQ, :], in_=mod_d), mt.r, rd=[r_mod], wr=[mt.r])
        store("sp", dbg_d["mod"], mt, src_ap=mt.ap[0:NSEQ, :])
    P.barrier()
    A.off = base_off

    def bcast_row(src_row_ap):
        return bass.AP(src_row_ap.tensor, src_row_ap.offset, [[0, 128], [1, src_row_ap.shape[-1]]])

    w_in_v = w_in_d.rearrange("(k p) n -> p k n", p=128)
    w_out_v = w_out_d.rearrange("(k p) n -> p k n", p=128)

    A.off = base_off
    hT = A.bf(16 * S, "p (k s) -> p k s", k=16)
    sc1 = A.f32(16); sh1 = A.f32(16)
    wch = [A.bf(16 * 128, "p (k n) -> p k n", k=16) for _ in range(3)]
    qT = [A.bf(S) for _ in range(2)]
    sq_t = A.f32(512)
    og_t = [A.bf(512) for _ in range(2)]
    hks = [A.f32(128) for _ in range(3)]
    uni_off = A.off
    xt2 = [A.f32(2 * D, "p (i d) -> p i d", i=2) for _ in range(2)]
    A.off = uni_off
    kTa = A.bf(S); kTas = A.bf(S)
    VA = [[A.bf(NT * 128, "p (t n) -> p t n", n=128) for _ in range(2)] for _ in range(2)]
    EB = A.bf(16 * 2 * 128, "p (h c n) -> p h c n", h=16, c=2)
    pf_t = [A.f32(512) for _ in range(4)]; pb_t = [A.bf(512) for _ in range(4)]
    den_t = A.f32(512); o_t = A.f32(512)
    A.off = uni_off
    kT = [A.bf(S) for _ in range(2)]
    Vp = [[A.bf(NT * 128, "p (t n) -> p t n", n=128) for _ in range(2)] for _ in range(2)]
    sbm = A.f32(4 * 512, "p (j n) -> p j n", j=4)
    e_t = [A.f32(1024) for _ in range(5)]
    sp_t = [A.bf(1024) for _ in range(4)]; ec_t = [A.f32(1024) for _ in range(3)]; a_t = [A.bf(1024) for _ in range(3)]
    A.off = base_off
    wo = A.bf(16 * D, "p (k n) -> p k n", k=16)
    GG = A.f32(D); BB = A.f32(D); LG = A.f32(D); LB = A.f32(D)
    ogt = [A.bf(16 * 128, "p (k s) -> p k s", k=16) for _ in range(2)]
    xtl = [A.f32(D) for _ in range(2)]
    mixn = A.f32(D); h2_2 = [A.f32(D) for _ in range(2)]
    nrm2 = [A.f32(D) for _ in range(2)]; x1t2 = [A.f32(D) for _ in range(2)]
    h2b = [A.bf(D) for _ in range(3)]
    lg2 = [A.f32(40) for _ in range(2)]; smr = [A.f32(40) for _ in range(12)]
    h2T = A.f32(16 * 128, "p (k s) -> p k s", k=16)
    wr_t = A.f32(16 * 36, "p (k n) -> p k n", k=16); brt = A.f32(36)
    rstd = A.f32(2 * NT, "p (g t) -> p g t", g=2)
    st6 = A.f32(24); mv = A.f32(2); sm = [None] * 10 + [A.f32(8), A.f32(8)]
    ohbf = A.bf(32)

    for b in range(NSEQ):
        with nc.allow_non_contiguous_dma(reason="tiny feature-major load of adaLN shift/scale"):
            P.dma("sp", lambda e, b=b: e.dma_start(out=sh1.ap, in_=mod_d[b:b + 1, 0:D].rearrange("o (k p) -> p (o k)", p=128), allow_slow_non_contiguous=True), sh1.r, rd=[r_mod], wr=[sh1.r])
            P.dma("sp", lambda e, b=b: e.dma_start(out=sc1.ap, in_=mod_d[b:b + 1, D:2 * D].rearrange("o (k p) -> p (o k)", p=128), allow_slow_non_contiguous=True), sc1.r, rd=[r_mod], wr=[sc1.r])
        P.op("dve", lambda e: e.tensor_scalar_add(out=sc1.ap, in0=sc1.ap, scalar1=1.0), rd=R(sc1), wr=R(sc1))
        P.op("dve", lambda e: e.memset(ssq.ap, 0.0), wr=R(ssq))
        for tb in range(S // 256):
            tok0 = b * S + tb * 256
            xt = xt2[tb % 2]
            load("sp", xt, x_d[tok0:tok0 + 256, :].rearrange("(i p) d -> p i d", p=128))
            for k2 in range(8):
                pbk = PB[nxt("pj", 2)]
                for kk in range(2):
                    k = k2 * 2 + kk
                    for i in range(2):
                        P.op("pe", lambda e, pbk=pbk, kk=kk, i=i, k=k: e.transpose(out=pbk.ap[:, kk * 256 + i * 128: kk * 256 + (i + 1) * 128],
                                                                                 in_=xt.ap[:, i, k * 128:(k + 1) * 128], identity=ident.ap),
                             rd=R(xt, ident), wr=R(pbk))
                for kk in range(2):
                    k = k2 * 2 + kk
                    dst = hT.ap[:, k, tb * 256:(tb + 1) * 256]
                    if kk == 0:
                        P.op("act", lambda e, pbk=pbk, kk=kk, k=k, dst=dst: e.activation(out=dst, in_=pbk.ap[:, kk * 256:(kk + 1) * 256], func=AF.Identity,
                                                                                       scale=sc1.ap[:, k:k + 1], bias=sh1.ap[:, k:k + 1]),
                             rd=R(pbk, sc1, sh1), wr=R(hT))
                    else:
                        P.op("dve", lambda e, pbk=pbk, kk=kk, k=k, dst=dst: e.tensor_scalar(out=dst, in0=pbk.ap[:, kk * 256:(kk + 1) * 256], scalar1=sc1.ap[:, k:k + 1],
                                                                                          scalar2=sh1.ap[:, k:k + 1], op0=ALU.mult, op1=ALU.add),
                             rd=R(pbk, sc1, sh1), wr=R(hT))

        P.barrier()
        for r in range(2):
            for rr in range(2):
                P.op("pool", lambda e, t=VA[r][rr]: e.memset(t.ap, 0.0), wr=R(VA[r][rr]))
        for h in range(16):
            for cpv in range(2):
                hk = hks[(2 * h + cpv) % 3]
                src = bass.AP(gb_d.tensor, gb_d.offset + h * 512 + cpv * 256, [[1, 128], [1, 128]])
                P.dma("sp", lambda e, src=src: e.dma_start(out=hk.ap, in_=src), hk.r, rd=[r_gb], wr=[hk.r])
                P.op("act", lambda e: e.activation(out=hk.ap, in_=hk.ap, func=AF.Exp), rd=R(hk), wr=R(hk))
                rev = bass.AP(hk.ap.tensor, hk.ap.offset + 127, [[hk.ap.ap[0][0], 128], [-1, 128]])
                P.op("pool", lambda e, h=h, cpv=cpv, rev=rev: e.tensor_copy(out=EB.ap[:, h, cpv, :], in_=rev), rd=R(hk), wr=R(EB))

        def load_w(c0):
            w = wch[nxt("wch", 3)]
            P.dma("pool", lambda e, w=w, c0=c0: e.dma_start(out=w.ap, in_=w_in_v[:, :, c0:c0 + 128]), w.r, wr=[w.r])
            return w

        def proj_fm(w, dst):
            for tb in range(NQ):
                pbk = PB[nxt("pj", 2)]
                for k in range(16):
                    P.op("pe", lambda e, k=k, tb=tb, pbk=pbk: e.matmul(out=pbk.ap, lhsT=w.ap[:, k, :], rhs=hT.ap[:, k, tb * 512:(tb + 1) * 512], start=(k == 0), stop=(k == 15)),
                         rd=R(w, hT), wr=R(pbk))
                if tb % 2 == 0:
                    P.op("act", lambda e, tb=tb, pbk=pbk: e.activation(out=dst.ap[:, tb * 512:(tb + 1) * 512], in_=pbk.ap, func=AF.Copy), rd=R(pbk), wr=R(dst))
                else:
                    P.op("dve", lambda e, tb=tb, pbk=pbk: e.tensor_copy(out=dst.ap[:, tb * 512:(tb + 1) * 512], in_=pbk.ap), rd=R(pbk), wr=R(dst))

        def proj_tm(w, evac):
            for t4 in range(NQ):
                pbk = PB[nxt("pj", 2)]
                for i in range(4):
                    tk = t4 * 4 + i
                    for k in range(16):
                        P.op("pe", lambda e, k=k, i=i, tk=tk, pbk=pbk: e.matmul(out=pbk.ap[:, i * 128:(i + 1) * 128], lhsT=hT.ap[:, k, tk * 128:(tk + 1) * 128], rhs=w.ap[:, k, :],
                                                                               start=(k == 0), stop=(k == 15)),
                             rd=R(w, hT), wr=R(pbk))
                evac(pbk, t4)

        def ssq_update(grp, qs, sq):
            pbk = PB[nxt("pj", 2)]
            for i in range(4):
                P.op("pe", lambda e, i=i, pbk=pbk: e.matmul(out=pbk.ap[:, i:i + 1], lhsT=sq.ap[:, i * 128:(i + 1) * 128], rhs=ones_f.ap[:, 0:1], start=True, stop=True),
                     rd=R(sq, ones_f), wr=R(pbk))
            P.op("dve", lambda e, pbk=pbk: e.tensor_tensor(out=ssq.ap[:, grp, qs * 4:qs * 4 + 4], in0=pbk.ap[:, 0:4], in1=ssq.ap[:, grp, qs * 4:qs * 4 + 4], op=ALU.add),
                 rd=R(pbk, ssq), wr=R(ssq))

        w = load_w(CA_K)
        proj_fm(w, kTa)
        wsw = wch[nxt("wch", 3)]
        P.op("pool", lambda e, w=w, wsw=wsw: e.tensor_copy(out=wsw.ap[:, :, 0:64], in_=w.ap[:, :, 64:128]), rd=R(w), wr=R(wsw))
        P.op("pool", lambda e, w=w, wsw=wsw: e.tensor_copy(out=wsw.ap[:, :, 64:128], in_=w.ap[:, :, 0:64]), rd=R(w), wr=R(wsw))
        wsave = w
        w = wsw
        proj_fm(w, kTas)
        w = load_w(CA_V)

        def evac_va(pbk, t4):
            v4 = pbk.ap.rearrange("p (i n) -> p i n", i=4)
            P.op("act", lambda e: e.activation(out=VA[0][0].ap[:, t4 * 4:t4 * 4 + 4, 0:64], in_=v4[:, :, 0:64], func=AF.Copy), rd=R(pbk), wr=R(VA[0][0]))
            P.op("dve", lambda e: e.tensor_copy(out=VA[0][1].ap[:, t4 * 4:t4 * 4 + 4, 64:128], in_=v4[:, :, 0:64]), rd=R(pbk), wr=R(VA[0][1]))
            P.op("act", lambda e: e.activation(out=VA[1][0].ap[:, t4 * 4:t4 * 4 + 4, 0:64], in_=v4[:, :, 64:128], func=AF.Copy), rd=R(pbk), wr=R(VA[1][0]))
            P.op("dve", lambda e: e.tensor_copy(out=VA[1][1].ap[:, t4 * 4:t4 * 4 + 4, 64:128], in_=v4[:, :, 64:128]), rd=R(pbk), wr=R(VA[1][1]))
        proj_tm(w, evac_va)

        swa_units = [(j, qs, r) for j in range(8) for qs in range(NQ) for r in range(2)]
        qts = {}; psod = {}

        def w1(u):
            j, qs, r = swa_units[u]
            if qs == 0 and r == 0:
                w = load_w(CA_Q + j * 128)
                qts[j] = qT[nxt("qT", 2)]
                proj_fm(w, qts[j])
            q = qts[j]; kv = j // 4
            KT = kTa if kv == r else kTas
            rows = slice(r * 64, r * 64 + 64)
            pcur = PB[2 * (u % 2)]; pprev = PB[2 * (u % 2) + 1]
            for i in range(4):
                tq = qs * 4 + i
                P.op("pe", lambda e, i=i, tq=tq: e.matmul(out=pcur.ap[:, i * 128:(i + 1) * 128], lhsT=KT.ap[rows, tq * 128:(tq + 1) * 128],
                                                         rhs=q.ap[rows, tq * 128:(tq + 1) * 128], start=True, stop=True), rd=R(KT, q), wr=R(pcur))
            for i in range(4):
                tq = qs * 4 + i
                if tq == 0:
                    continue
                P.op("pe", lambda e, i=i, tq=tq: e.matmul(out=pprev.ap[:, i * 128:(i + 1) * 128], lhsT=KT.ap[rows, (tq - 1) * 128:tq * 128],
                                                         rhs=q.ap[rows, tq * 128:(tq + 1) * 128], start=True, stop=True), rd=R(KT, q), wr=R(pprev))
            pcf = pf_t[2 * (u % 2)]; ppf = pf_t[2 * (u % 2) + 1]
            c0 = 128 if qs == 0 else 0
            P.op("act", lambda e: e.activation(out=pcf.ap, in_=pcur.ap, func=AF.Exp, scale=0.125), rd=R(pcur), wr=R(pcf))
            P.op("act", lambda e: e.activation(out=ppf.ap[:, c0:512], in_=pprev.ap[:, c0:512], func=AF.Exp, scale=0.125), rd=R(pprev), wr=R(ppf))

        def w2(u):
            j, qs, r = swa_units[u]
            h = 2 * j + r
            pcf = pf_t[2 * (u % 2)]; ppf = pf_t[2 * (u % 2) + 1]
            pcb = pb_t[2 * (u % 2)]; ppb = pb_t[2 * (u % 2) + 1]
            c0 = 128 if qs == 0 else 0
            n4 = 3 if qs == 0 else 4
            ebc = bass.AP(EB.ap.tensor, EB.ap[:, h, 0, :].offset, [[EB.ap.ap[0][0], 128], [0, 4], [1, 128]])
            ebp2 = bass.AP(EB.ap.tensor, EB.ap[:, h, 1, :].offset, [[EB.ap.ap[0][0], 128], [0, n4], [1, 128]])
            P.op("pool", lambda e: e.tensor_tensor(out=pcb.ap.rearrange("p (i n) -> p i n", i=4), in0=pcf.ap.rearrange("p (i n) -> p i n", i=4), in1=ebc, op=ALU.mult),
                 rd=R(pcf, EB), wr=R(pcb))
            P.op("dve", lambda e: e.tensor_tensor(out=ppb.ap[:, c0:512].rearrange("p (i n) -> p i n", i=n4), in0=ppf.ap[:, c0:512].rearrange("p (i n) -> p i n", i=n4),
                                                  in1=ebp2, op=ALU.mult), rd=R(ppf, EB), wr=R(ppb))

        def w3(u):
            j, qs, r = swa_units[u]
            kv = j // 4
            pcb = pb_t[2 * (u % 2)]; ppb = pb_t[2 * (u % 2) + 1]
            if r == 0:
                psod[(j, qs)] = (PB[6 + nxt("pso", 2)], PB[4 + nxt("psd", 2)])
            pso, psd = psod[(j, qs)]
            Vt = VA[kv][r]
            first = (r == 0)
            for i in range(4):
                tq = qs * 4 + i
                srcs = [(pcb, tq)] + ([(ppb, tq - 1)] if tq > 0 else [])
                for (pt, blk) in srcs:
                    P.op("pe", lambda e, i=i, pt=pt, blk=blk, st=first: e.matmul(out=pso.ap[:, i * 128:(i + 1) * 128], lhsT=Vt.ap[:, blk, :], rhs=pt.ap[:, i * 128:(i + 1) * 128],
                                                                                start=st, stop=True, skip_group_check=True), rd=R(Vt, pt), wr=R(pso))
                    first = False
            c0 = 128 if qs == 0 else 0
            P.op("pe", lambda e, st=(r == 0): e.matmul(out=psd.ap, lhsT=onesh.ap[:, r, :], rhs=pcb.ap, start=st, stop=True, skip_group_check=True), rd=R(onesh, pcb), wr=R(psd))
            P.op("pe", lambda e: e.matmul(out=psd.ap[:, c0:512], lhsT=onesh.ap[:, r, :], rhs=ppb.ap[:, c0:512], start=False, stop=True, skip_group_check=True), rd=R(onesh, ppb), wr=R(psd))
            if r == 1:
                P.op("dve", lambda e: e.tensor_scalar(out=den_t.ap, in0=psd.ap, scalar1=esink.ap[:, j:j + 1], scalar2=None, op0=ALU.add), rd=R(psd, esink), wr=R(den_t))
                P.op("dve", lambda e: e.reciprocal(out=den_t.ap, in_=den_t.ap), rd=R(den_t), wr=R(den_t))
                P.op("dve", lambda e: e.tensor_tensor(out=o_t.ap, in0=pso.ap, in1=den_t.ap, op=ALU.mult), rd=R(pso, den_t), wr=R(o_t))
                og = og_t[nxt("og", 2)]
                P.op("act", lambda e: e.activation(out=og.ap, in_=o_t.ap, func=AF.Copy, scale=na_fm.ap[:, j:j + 1]), rd=R(o_t, na_fm), wr=R(og))
                P.op("act", lambda e: e.activation(out=sq_t.ap, in_=o_t.ap, func=AF.Square), rd=R(o_t), wr=R(sq_t))
                store("sp", ogT_d[b, j * 128:(j + 1) * 128, qs * 512:(qs + 1) * 512], og, extra_wr=[r_og[b]])
                ssq_update(0, qs, sq_t)

        nsw = len(swa_units)
        for i in range(nsw + 2):
            if i < nsw:
                w1(i)
            if 0 <= i - 1 < nsw:
                w2(i - 1)
            if 0 <= i - 2 < nsw:
                w3(i - 2)

        P.barrier()
        load("sp", sbm, sbmask_d)
        for r in range(2):
            for rr in range(2):
                P.op("pool", lambda e, t=Vp[r][rr]: e.memset(t.ap, 0.0), wr=R(Vp[r][rr]))
        pair_ap = lambda g: psum_t[:, (2 * g) * 512:(2 * g + 2) * 512]
        for j in range(8):
            w = load_w(CB_Q + j * 128)
            q = qT[nxt("qT", 2)]
            proj_fm(w, q)
            w = load_w(CB_K + j * 128)
            kk_ = kT[nxt("kT", 2)]
            proj_fm(w, kk_)
            w = load_w(CB_V + j * 128)
            vi = nxt("Vp", 2)
            V0 = Vp[vi][0]; V1 = Vp[vi][1]

            def evac_vb(pbk, t4, V0=V0, V1=V1):
                v4 = pbk.ap.rearrange("p (i n) -> p i n", i=4)
                P.op("act", lambda e: e.activation(out=V0.ap[:, t4 * 4:t4 * 4 + 4, 0:64], in_=v4[:, :, 0:64], func=AF.Copy), rd=R(pbk), wr=R(V0))
                P.op("dve", lambda e: e.tensor_copy(out=V1.ap[:, t4 * 4:t4 * 4 + 4, 64:128], in_=v4[:, :, 64:128]), rd=R(pbk), wr=R(V1))
            proj_tm(w, evac_vb)
            units = [(qs, kb) for qs in range(NQ) for kb in range(4 * qs + 3, -1, -1)]
            nun = len(units)
            psos = {}

            def s1a(u, q=q, kk_=kk_):
                qs, kb = units[u]
                g = u % 2
                et = e_t[u % 5]
                for r in range(2):
                    rows = slice(r * 64, r * 64 + 64)
                    pss = PB[2 * g + r]
                    P.op("pe", lambda e, r=r: e.matmul(out=pss.ap, lhsT=kk_.ap[rows, kb * 128:(kb + 1) * 128], rhs=q.ap[rows, qs * 512:(qs + 1) * 512], start=True, stop=True),
                         rd=R(kk_, q), wr=R(pss))
                P.op("act", lambda e: e.activation(out=et.ap, in_=pair_ap(g), func=AF.Exp, scale=0.125), rd=R(PB[2 * g], PB[2 * g + 1]), wr=R(et))
                if kb >= 4 * qs:
                    jj = kb - 4 * qs
                    mk = bass.AP(sbm.ap.tensor, sbm.ap[:, jj, :].offset, [[sbm.ap.ap[0][0], 128], [0, 2], [1, 512]])
                    ev = et.ap.rearrange("p (r n) -> p r n", r=2)
                    P.op("pool", lambda e: e.tensor_tensor(out=ev, in0=ev, in1=mk, op=ALU.mult), rd=R(et, sbm), wr=R(et))

            def s1b(u):
                et = e_t[u % 5]; spt = sp_t[u % 4]
                P.op("act", lambda e: e.activation(out=spt.ap, in_=et.ap, func=AF.Ln, bias=1.0, scale=1.0), rd=R(et), wr=R(spt))

            def s2a(u):
                qs, kb = units[u]
                spt = sp_t[u % 4]; ect = ec_t[u % 3]
                st = (kb == 4 * qs + 3)
                for r in range(2):
                    psc = PB[4 + r]
                    P.op("pe", lambda e, r=r: e.matmul(out=psc.ap, lhsT=negtri.ap, rhs=spt.ap[:, r * 512:(r + 1) * 512], start=st, stop=True, skip_group_check=True),
                         rd=R(negtri, spt), wr=R(psc))
                P.op("act", lambda e: e.activation(out=ect.ap, in_=pair_ap(2), func=AF.Exp), rd=R(PB[4], PB[5]), wr=R(ect))

            def s2b1(u):
                qs, kb = units[u]
                spt = sp_t[u % 4]
                if kb > 0:
                    for r in range(2):
                        psc = PB[4 + r]
                        P.op("pe", lambda e, r=r: e.matmul(out=psc.ap, lhsT=negsu.ap, rhs=spt.ap[:, r * 512:(r + 1) * 512], start=False, stop=True, skip_group_check=True),
                             rd=R(negsu, spt), wr=R(psc))

            def s2b(u, j=j, V0=V0, V1=V1):
                qs, kb = units[u]
                spt = sp_t[u % 4]; ect = ec_t[u % 3]; et = e_t[u % 5]; at = a_t[u % 3]
                P.op("dve", lambda e: e.tensor_tensor(out=at.ap, in0=et.ap, in1=ect.ap, op=ALU.mult), rd=R(et, ect), wr=R(at))
                st = qs not in psos
                if st:
                    psos[qs] = PB[6 + nxt("pso", 2)]
                pso = psos[qs]
                for r in range(2):
                    Vt = V0 if r == 0 else V1
                    P.op("pe", lambda e, r=r, Vt=Vt, st=(st and r == 0): e.matmul(out=pso.ap, lhsT=Vt.ap[:, kb, :], rhs=at.ap[:, r * 512:(r + 1) * 512], start=st, stop=True, skip_group_check=True),
                         rd=R(Vt, at), wr=R(pso))
                if kb == 0:
                    og = og_t[nxt("og", 2)]
                    P.op("act", lambda e: e.activation(out=og.ap, in_=pso.ap, func=AF.Copy, scale=nb_fm.ap[:, j:j + 1]), rd=R(pso, nb_fm), wr=R(og))
                    P.op("act", lambda e: e.activation(out=sq_t.ap, in_=pso.ap, func=AF.Square), rd=R(pso), wr=R(sq_t))
                    store("sp", ogT_d[b, (8 + j) * 128:(9 + j) * 128, qs * 512:(qs + 1) * 512], og, extra_wr=[r_og[b]])
                    ssq_update(1, qs, sq_t)

            for i in range(nun + 3):
                if i < nun:
                    s1a(i)
                if 0 <= i - 3 < nun:
                    s2b1(i - 3)
                if 0 <= i - 2 < nun:
                    s2a(i - 2)
                if 0 <= i - 1 < nun:
                    s1b(i - 1)
                if 0 <= i - 3 < nun:
                    s2b(i - 3)
        if dbg:
            P.barrier()
            store("sp", dbg_d["ssq"][b], ssq, src_ap=ssq.ap.rearrange("p g t -> p (g t)"))
        P.barrier()

        G1 = mixn
        load("sp", G1, bcast_row(mod_d[b:b + 1, 2 * D:3 * D]), extra_rd=[r_mod])
        P.op("dve", lambda e: e.tensor_scalar_add(out=G1.ap, in0=G1.ap, scalar1=1.0), rd=R(G1), wr=R(G1))
        for k in range(16):
            stw = nrm2[k % 2]
            load("sp", stw, w_out_v[:, k, :])
            P.op("dve", lambda e, k=k, stw=stw: e.tensor_tensor(out=wo.ap[:, k, :], in0=stw.ap, in1=G1.ap, op=ALU.mult), rd=R(stw, G1), wr=R(wo))
        load("sp", BB, bcast_row(mod_d[b:b + 1, 3 * D:4 * D]), extra_rd=[r_mod])
        load("sp", GG, bcast_row(mod_d[b:b + 1, 4 * D:5 * D]), extra_rd=[r_mod])
        load("sp", LG, bcast_row(ln1g_d)); load("sp", LB, bcast_row(ln1b_d))
        P.op("dve", lambda e: e.tensor_scalar_add(out=GG.ap, in0=GG.ap, scalar1=1.0), rd=R(GG), wr=R(GG))
        tmpx = x1t2[0]
        P.op("dve", lambda e: e.tensor_tensor(out=tmpx.ap, in0=LB.ap, in1=GG.ap, op=ALU.mult), rd=R(LB, GG), wr=R(tmpx))
        P.op("dve", lambda e: e.tensor_tensor(out=BB.ap, in0=BB.ap, in1=tmpx.ap, op=ALU.add), rd=R(BB, tmpx), wr=R(BB))
        P.op("dve", lambda e: e.tensor_tensor(out=GG.ap, in0=GG.ap, in1=LG.ap, op=ALU.mult), rd=R(GG, LG), wr=R(GG))
        P.dma("sp", lambda e: e.dma_start(out=wr_t.ap[:, :, 0:4], in_=wgrp_d.rearrange("(k p) n -> p k n", p=128)), wr_t.r, wr=[wr_t.r])
        P.dma("sp", lambda e: e.dma_start(out=wr_t.ap[:, :, 4:36], in_=wrtr_d.rearrange("(k p) n -> p k n", p=128)), wr_t.r, wr=[wr_t.r])
        P.dma("sp", lambda e: e.dma_start(out=brt.ap[0:1, 0:4], in_=bgrp_d), brt.r, wr=[brt.r])
        P.dma("sp", lambda e: e.dma_start(out=brt.ap[0:1, 4:36], in_=brtr_d), brt.r, wr=[brt.r])
        P.op("act", lambda e: e.activation(out=rstd.ap, in_=ssq.ap, func=AF.Ln, scale=1.0 / 1024.0, bias=EPS), rd=R(ssq), wr=R(rstd))
        P.op("act", lambda e: e.activation(out=rstd.ap, in_=rstd.ap, func=AF.Exp, scale=-0.5), rd=R(rstd), wr=R(rstd))

        ogT_v = ogT_d[b].rearrange("(k p) s -> p k s", p=128)

        def XL(t):
            og = ogt[t % 2]; xx = xtl[t % 2]; gt = b * NT + t
            P.dma("sp", lambda e: e.dma_start(out=og.ap, in_=ogT_v[:, :, t * 128:(t + 1) * 128]), og.r, rd=[r_og[b]], wr=[og.r])
            load("sp", xx, x_d[gt * 128:(gt + 1) * 128, :])
            P.op("act", lambda e: e.activation(out=mixn.ap, in_=xx.ap, func=AF.Copy, scale=ALPHA), rd=R(xx), wr=R(mixn))

        def XN(t, nb):
            og = ogt[t % 2]
            pa = PB[2 + nxt("pa3", 3)]; pbb = PB[5 + nxt("pb3", 3)]
            for k in range(8):
                P.op("pe", lambda e, k=k: e.matmul(out=pa.ap, lhsT=og.ap[:, k, :], rhs=wo.ap[:, k, nb * 512:(nb + 1) * 512], start=(k == 0), stop=(k == 7)),
                     rd=R(og, wo), wr=R(pa))
            for k in range(8, 16):
                P.op("pe", lambda e, k=k: e.matmul(out=pbb.ap, lhsT=og.ap[:, k, :], rhs=wo.ap[:, k, nb * 512:(nb + 1) * 512], start=(k == 8), stop=(k == 15)),
                     rd=R(og, wo), wr=R(pbb))
            mv_ = mixn.ap[:, nb * 512:(nb + 1) * 512]
            P.op("dve", lambda e: e.scalar_tensor_tensor(out=mv_, in0=pa.ap, scalar=rstd.ap[:, 0, t:t + 1], in1=mv_, op0=ALU.mult, op1=ALU.add), rd=R(pa, rstd, mixn), wr=R(mixn))
            P.op("dve", lambda e: e.scalar_tensor_tensor(out=mv_, in0=pbb.ap, scalar=rstd.ap[:, 1, t:t + 1], in1=mv_, op0=ALU.mult, op1=ALU.add), rd=R(pbb, rstd, mixn), wr=R(mixn))

        def XC(t):
            layer_norm(P, mixn, nrm2[t % 2], st6, mv, sm)

        def YA1(t):
            gt = b * NT + t
            nrm = nrm2[t % 2]; x1t = x1t2[t % 2]
            P.op("dve", lambda e: e.tensor_tensor(out=x1t.ap, in0=nrm.ap, in1=LG.ap, op=ALU.mult), rd=R(nrm, LG), wr=R(x1t))
            P.op("dve", lambda e: e.tensor_tensor(out=x1t.ap, in0=x1t.ap, in1=LB.ap, op=ALU.add), rd=R(x1t, LB), wr=R(x1t))
            store("sp", x1_d[gt * 128:(gt + 1) * 128, :], x1t, extra_wr=[r_x1[gt]])

        def YA2(t):
            nrm = nrm2[t % 2]; hb = h2b[t % 3]; h2 = h2_2[t % 2]
            P.op("dve", lambda e: e.tensor_tensor(out=h2.ap, in0=nrm.ap, in1=GG.ap, op=ALU.mult), rd=R(nrm, GG), wr=R(h2))
            P.op("dve", lambda e: e.tensor_tensor(out=h2.ap, in0=h2.ap, in1=BB.ap, op=ALU.add), rd=R(h2, BB), wr=R(h2))
            P.op("act", lambda e: e.activation(out=hb.ap, in_=h2.ap, func=AF.Copy), rd=R(h2), wr=R(hb))

        def YB(t):
            h2 = h2_2[t % 2]
            for k4 in range(4):
                pbk = PB[nxt("pj", 2)]
                for kk in range(4):
                    k = k4 * 4 + kk
                    P.op("pe", lambda e, kk=kk, k=k: e.transpose(out=pbk.ap[:, kk * 128:(kk + 1) * 128], in_=h2.ap[:, k * 128:(k + 1) * 128], identity=ident.ap),
                         rd=R(h2, ident), wr=R(pbk))
                if k4 % 2 == 0:
                    P.op("act", lambda e: e.activation(out=h2T.ap[:, k4 * 4:k4 * 4 + 4, :], in_=pbk.ap.rearrange("p (k s) -> p k s", k=4), func=AF.Copy), rd=R(pbk), wr=R(h2T))
                else:
                    P.op("dve", lambda e: e.tensor_copy(out=h2T.ap[:, k4 * 4:k4 * 4 + 4, :], in_=pbk.ap.rearrange("p (k s) -> p k s", k=4)), rd=R(pbk), wr=R(h2T))
            pr = PB[nxt("pj", 2)]
            for k in range(16):
                P.op("pe", lambda e, k=k: e.matmul(out=pr.ap[:, 0:36], lhsT=h2T.ap[:, k, :], rhs=wr_t.ap[:, k, :], start=(k == 0), stop=False), rd=R(h2T, wr_t), wr=R(pr))
            P.op("pe", lambda e: e.matmul(out=pr.ap[:, 0:36], lhsT=ones_f.ap[0:1, :], rhs=brt.ap[0:1, :], start=False, stop=True), rd=R(ones_f, brt), wr=R(pr))
            lg = lg2[t % 2]
            P.op("dve", lambda e: e.tensor_copy(out=lg.ap[:, 0:36], in_=pr.ap[:, 0:36]), rd=R(pr), wr=R(lg))

        def ZA(t):
            route_a(P, lg2[t % 2], smr, gatew, b * NT + t)

        def ZB(t):
            gt = b * NT + t; hb = h2b[t % 3]
            route_b(P, smr, cnt_keep, eoff, ohbf, lowtri, ones_b, PB, nxt, slot_i, gt)
            for k in range(2):
                P.dma("pool", lambda e, k=k: e.indirect_dma_start(out=xs_d, out_offset=bass.IndirectOffsetOnAxis(ap=slot_i.ap[:, gt, k:k + 1], axis=0),
                                                                 in_=hb.ap, in_offset=None),
                      hb.r, rd=[hb.r, slot_i.r], wr=[r_xs])

        for stp in range(NT + 3):
            tx, ta, tb_, tz = stp, stp - 1, stp - 2, stp - 3
            okx = tx < NT; oka = 0 <= ta < NT; okb = 0 <= tb_ < NT; okz = 0 <= tz < NT
            if okb:
                YB(tb_)
            if okx:
                XL(tx)
            if okz:
                ZA(tz)
            if okx:
                XN(tx, 0)
            if okz:
                ZB(tz)
            if okx:
                XN(tx, 1)
            if oka:
                YA1(ta)
            if okx:
                XN(tx, 2)
            if oka:
                YA2(ta)
            if okx:
                XN(tx, 3); XC(tx)
        P.barrier()
    if dbg:
        A.off = base_off
        tmpb = A.bf(S)
        for b in range(NSEQ):
            for k in range(16):
                P.dma("sp", lambda e, b=b, k=k: e.dma_start(out=tmpb.ap, in_=ogT_d[b, k * 128:(k + 1) * 128, :]), tmpb.r, rd=[r_og[b]], wr=[tmpb.r])
                store("sp", dbg_d["ogT"][b, k * 128:(k + 1) * 128, :], tmpb)
        tmpf = A.f32(D)
        for gt in range(NTT):
            P.dma("sp", lambda e, gt=gt: e.dma_start(out=tmpf.ap, in_=x1_d[gt * 128:(gt + 1) * 128, :]), tmpf.r, rd=[r_x1[gt]], wr=[tmpf.r])
            store("sp", dbg_d["x1"][gt * 128:(gt + 1) * 128, :], tmpf)
        store("sp", dbg_d["slot"], slot_i, src_ap=slot_i.ap.rearrange("p t k -> p (t k)"))
        store("sp", dbg_d["gw"], gatew, src_ap=gatew.ap.rearrange("p t k -> p (t k)"))
        P.barrier()

    A.off = base_off
    wslot = [A.bf(16 * 512) for _ in range(6)]
    stg = [A.f32(2048) for _ in range(4)]
    xsb = [A.bf(D) for _ in range(4)]
    xsT = [A.bf(16 * CAP, "p (k s) -> p k s", k=16) for _ in range(2)]
    actT = [A.bf(4 * CAP, "p (f s) -> p f s", f=4) for _ in range(2)]
    sgt = [A.f32(CAP) for _ in range(2)]
    yt = [A.f32(D) for _ in range(2)]
    identb = A.bf(128)
    P.op("dve", lambda e: e.tensor_copy(out=identb.ap, in_=ident.ap), rd=R(ident), wr=R(identb))

    def cast_task(dst, c, src, pat, kw):
        def run():
            st = stg[nxt("stg", 4)]
            sv = st.ap.rearrange(pat, **kw) if pat else st.ap
            P.dma("sp", lambda e: e.dma_start(out=sv, in_=src), st.r, wr=[st.r])
            dv = dst.ap[:, c * 2048:(c + 1) * 2048]
            if nxt("casteng", 2) == 0:
                P.op("act", lambda e: e.activation(out=dv, in_=st.ap, func=AF.Copy), rd=R(st), wr=R(dst))
            else:
                P.op("dve", lambda e: e.tensor_copy(out=dv, in_=st.ap), rd=R(st), wr=R(dst))
        return run

    def expert_loads(ex):
        wg = wslot[nxt("ws", 6)]; wu = wslot[nxt("ws", 6)]; wdn = wslot[nxt("ws", 6)]
        gv = wg_d[ex].rearrange("(p k) f -> p k f", k=16); uv = wu_d[ex].rearrange("(p k) f -> p k f", k=16)
        dvw = wd_d[ex].rearrange("(k p) f -> p k f", p=128)
        tasks = []
        xbs = []
        for cb in range(CAPB):
            xb = xsb[nxt("xsb", 4)]
            xbs.append(xb)
            row0 = ex * CAP + cb * 128

            def xload(xb=xb, row0=row0):
                P.dma("sp", lambda e: e.dma_start(out=xb.ap, in_=xs_d[row0:row0 + 128, :]), xb.r, rd=[r_xs], wr=[xb.r])
            tasks.append(xload)
        for c in range(4):
            tasks.append(cast_task(wg, c, gv[:, 4 * c:4 * c + 4, :], "p (k f) -> p k f", dict(k=4)))
            tasks.append(cast_task(wu, c, uv[:, 4 * c:4 * c + 4, :], "p (k f) -> p k f", dict(k=4)))
        for c in range(4):
            tasks.append(cast_task(wdn, c, dvw[:, c, :], None, {}))
        return (wg, wu, wdn), xbs, tasks

    cur_w, cur_x, tasks0 = expert_loads(0)
    for tsk in tasks0:
        tsk()
    for ex in range(NEXP):
        wg, wu, wdn = cur_w
        xbs = cur_x
        if ex + 1 < NEXP:
            cur_w, cur_x, bg = expert_loads(ex + 1)
        else:
            bg = []

        def pump(n=1):
            for _ in range(n):
                if bg:
                    bg.pop(0)()

        wgv = wg.ap.rearrange("p (k f) -> p k f", k=16); wuv = wu.ap.rearrange("p (k f) -> p k f", k=16); wdv = wdn.ap.rearrange("p (k f) -> p k f", k=4)
        xT = xsT[ex % 2]; aT = actT[ex % 2]
        for cb in range(CAPB):
            xb = xbs[cb]
            for k8 in range(2):
                pbk = PB[nxt("pj", 2)]
                pv = pbk.ap.bitcast(BF16)
                for kk in range(8):
                    k = k8 * 8 + kk
                    P.op("pe", lambda e, pv=pv, kk=kk, k=k, xb=xb: e.transpose(out=pv[:, kk * 128:(kk + 1) * 128], in_=xb.ap.rearrange("s (p k) -> s k p", k=16)[:, k, :], identity=identb.ap),
                         rd=R(xb, identb), wr=R(pbk))
                dstv = xT.ap[:, k8 * 8:k8 * 8 + 8, cb * 128:(cb + 1) * 128]
                if k8 == 0:
                    P.op("act", lambda e, pv=pv, dstv=dstv: e.activation(out=dstv, in_=pv.rearrange("p (k s) -> p k s", k=8), func=AF.Copy), rd=R(pbk), wr=R(xT))
                else:
                    P.op("dve", lambda e, pv=pv, dstv=dstv: e.tensor_copy(out=dstv, in_=pv.rearrange("p (k s) -> p k s", k=8)), rd=R(pbk), wr=R(xT))
            pump(1)
        for fc in range(4):
            pg = PB[2 + nxt("pss", 2)]; pu = PB[4 + nxt("psd", 2)]
            for k in range(16):
                P.op("pe", lambda e, k=k, fc=fc, pg=pg: e.matmul(out=pg.ap[:, 0:CAP], lhsT=wgv[:, k, fc * 128:(fc + 1) * 128], rhs=xT.ap[:, k, :], start=(k == 0), stop=(k == 15)),
                     rd=R(wg, xT), wr=R(pg))
            pump(1)
            for k in range(16):
                P.op("pe", lambda e, k=k, fc=fc, pu=pu: e.matmul(out=pu.ap[:, 0:CAP], lhsT=wuv[:, k, fc * 128:(fc + 1) * 128], rhs=xT.ap[:, k, :], start=(k == 0), stop=(k == 15)),
                     rd=R(wu, xT), wr=R(pu))
            sgx = sgt[fc % 2]
            P.op("act", lambda e, pg=pg, sgx=sgx: e.activation(out=sgx.ap, in_=pg.ap[:, 0:CAP], func=AF.Silu), rd=R(pg), wr=R(sgx))
            P.op("dve", lambda e, pu=pu, sgx=sgx, fc=fc: e.tensor_tensor(out=aT.ap[:, fc, :], in0=pu.ap[:, 0:CAP], in1=sgx.ap, op=ALU.mult), rd=R(pu, sgx), wr=R(aT))
            pump(1)
        for cb in range(CAPB):
            y = yt[nxt("yt", 2)]
            for nb in range(4):
                py = PB[6 + nxt("pso", 2)]
                for fc in range(4):
                    P.op("pe", lambda e, fc=fc, nb=nb, cb=cb, py=py: e.matmul(out=py.ap, lhsT=aT.ap[:, fc, cb * 128:(cb + 1) * 128], rhs=wdv[:, fc, nb * 512:(nb + 1) * 512], start=(fc == 0), stop=(fc == 3)),
                         rd=R(aT, wdn), wr=R(py))
                if nb % 2 == 0:
                    P.op("act", lambda e, nb=nb, py=py, y=y: e.activation(out=y.ap[:, nb * 512:(nb + 1) * 512], in_=py.ap, func=AF.Copy), rd=R(py), wr=R(y))
                else:
                    P.op("dve", lambda e, nb=nb, py=py, y=y: e.tensor_copy(out=y.ap[:, nb * 512:(nb + 1) * 512], in_=py.ap), rd=R(py), wr=R(y))
                if nb % 2 == 1:
                    pump(1)
            row0 = ex * CAP + cb * 128
            store("act", ys_d[row0:row0 + 128, :], y, extra_wr=[r_ys[ex]])
        pump(len(bg))
    P.barrier()

    A.off = base_off
    G2 = [A.f32(D) for _ in range(NSEQ)]
    L2G = A.f32(D); L2B = A.f32(D)
    y1 = [A.f32(D) for _ in range(2)]; y2 = [A.f32(D) for _ in range(2)]; xx1 = [A.f32(D) for _ in range(2)]
    f2 = [A.f32(D) for _ in range(2)]; nrmc = [A.f32(D) for _ in range(2)]; o_out = [A.f32(D) for _ in range(2)]
    st6 = A.f32(24); mv = A.f32(2); sm = [A.f32(40) for _ in range(12)]
    for b in range(NSEQ):
        load("sp", G2[b], bcast_row(mod_d[b:b + 1, 5 * D:6 * D]), extra_rd=[r_mod])
        P.op("pool", lambda e, b=b: e.tensor_scalar_add(out=G2[b].ap, in0=G2[b].ap, scalar1=1.0), rd=R(G2[b]), wr=R(G2[b]))
    load("sp", L2G, bcast_row(ln2g_d)); load("sp", L2B, bcast_row(ln2b_d))

    def CL(gt):
        ya = y1[gt % 2]; yb = y2[gt % 2]; xa = xx1[gt % 2]
        P.dma("pool", lambda e: e.indirect_dma_start(out=ya.ap, out_offset=None, in_=ys_d, in_offset=bass.IndirectOffsetOnAxis(ap=slot_i.ap[:, gt, 0:1], axis=0)),
              ya.r, rd=R(slot_i) + r_ys, wr=[ya.r])
        P.dma("pool", lambda e: e.indirect_dma_start(out=yb.ap, out_offset=None, in_=ys_d, in_offset=bass.IndirectOffsetOnAxis(ap=slot_i.ap[:, gt, 1:2], axis=0)),
              yb.r, rd=R(slot_i) + r_ys, wr=[yb.r])
        P.dma("sp", lambda e: e.dma_start(out=xa.ap, in_=x1_d[gt * 128:(gt + 1) * 128, :]), xa.r, rd=[r_x1[gt]], wr=[xa.r])

    def C1a(gt):
        ya = y1[gt % 2]; yb = y2[gt % 2]; f_t = f2[gt % 2]; b = gt // NT
        P.op("act", lambda e: e.activation(out=f_t.ap, in_=ya.ap, func=AF.Copy, scale=gatew.ap[:, gt, 0:1]), rd=R(ya, gatew), wr=R(f_t))
        P.op("dve", lambda e: e.scalar_tensor_tensor(out=f_t.ap, in0=yb.ap, scalar=gatew.ap[:, gt, 1:2], in1=f_t.ap, op0=ALU.mult, op1=ALU.add), rd=R(yb, gatew, f_t), wr=R(f_t))
        P.op("dve", lambda e: e.tensor_tensor(out=f_t.ap, in0=f_t.ap, in1=G2[b].ap, op=ALU.mult), rd=R(f_t, G2[b]), wr=R(f_t))

    def C1b(gt):
        xa = xx1[gt % 2]; f_t = f2[gt % 2]
        P.op("dve", lambda e: e.scalar_tensor_tensor(out=f_t.ap, in0=xa.ap, scalar=ALPHA, in1=f_t.ap, op0=ALU.mult, op1=ALU.add), rd=R(xa, f_t), wr=R(f_t))

    def C2a(gt):
        layer_norm(P, f2[gt % 2], nrmc[gt % 2], st6, mv, sm)

    def C2b(gt):
        oo = o_out[gt % 2]; nrm = nrmc[gt % 2]
        P.op("dve", lambda e: e.tensor_tensor(out=oo.ap, in0=nrm.ap, in1=L2G.ap, op=ALU.mult), rd=R(nrm, L2G), wr=R(oo))
        P.op("dve", lambda e: e.tensor_tensor(out=oo.ap, in0=oo.ap, in1=L2B.ap, op=ALU.add), rd=R(oo, L2B), wr=R(oo))
        store("sp", out_d[gt * 128:(gt + 1) * 128, :], oo)

    CL(0)
    for stp in range(NTT + 2):
        if stp + 1 < NTT:
            CL(stp + 1)
        if stp < NTT:
            C1a(stp)
        if 0 <= stp - 1 < NTT:
            C2a(stp - 1)
        if stp < NTT:
            C1b(stp)
        if 0 <= stp - 2 < NTT:
            C2b(stp - 2)
    P.final_wait("sp")
    stats = P.emit()
    return nc, stats


def layer_norm(P, y, nrm, st6, mv, sm):
    R = lambda *ts: [t.r for t in ts]
    for i in range(4):
        P.op("dve", lambda e, i=i: e.bn_stats(out=st6.ap[:, i * 6:(i + 1) * 6], in_=y.ap[:, i * 512:(i + 1) * 512]), rd=R(y), wr=R(st6))
    P.op("dve", lambda e: e.bn_aggr(out=mv.ap, in_=st6.ap), rd=R(st6), wr=R(mv))
    rs = sm[10]; nm = sm[11]
    P.op("act", lambda e: e.activation(out=rs.ap[:, 0:1], in_=mv.ap[:, 1:2], func=AF.Ln, bias=EPS, scale=1.0), rd=R(mv), wr=R(rs))
    P.op("act", lambda e: e.activation(out=rs.ap[:, 0:1], in_=rs.ap[:, 0:1], func=AF.Exp, scale=-0.5), rd=R(rs), wr=R(rs))
    P.op("dve", lambda e: e.scalar_tensor_tensor(out=nm.ap[:, 0:1], in0=mv.ap[:, 0:1], scalar=-1.0, in1=rs.ap[:, 0:1], op0=ALU.mult, op1=ALU.mult), rd=R(mv, rs), wr=R(nm))
    P.op("act", lambda e: e.activation(out=nrm.ap, in_=y.ap, func=AF.Identity, scale=rs.ap[:, 0:1], bias=nm.ap[:, 0:1]), rd=R(y, rs, nm), wr=R(nrm))


def route_a(P, lgt, sm, gatew, gt):
    R = lambda *ts: [t.r for t in ts]
    lg = lgt
    _, gmx, goh, gpen, elm, m1, oh1, elm2, m2, oh2 = sm[0:10]
    tmp = sm[10]
    dv = lambda op, rd, wr: P.op("dve", op, rd=R(*rd), wr=R(*wr))
    dv(lambda e: e.reduce_max(out=gmx.ap[:, 0:1], in_=lg.ap[:, 0:4], axis=AX.X), [lg], [gmx])
    dv(lambda e: e.tensor_scalar(out=goh.ap[:, 0:4], in0=lg.ap[:, 0:4], scalar1=gmx.ap[:, 0:1], scalar2=None, op0=ALU.is_equal), [lg, gmx], [goh])
    dv(lambda e: e.tensor_scalar(out=gmx.ap[:, 1:2], in0=gmx.ap[:, 0:1], scalar1=-1.0, scalar2=None, op0=ALU.mult), [gmx], [gmx])
    P.op("act", lambda e: e.activation(out=tmp.ap[:, 0:4], in_=lg.ap[:, 0:4], func=AF.Exp, bias=gmx.ap[:, 1:2], scale=1.0, accum_out=tmp.ap[:, 4:5]), rd=R(lg, gmx), wr=R(tmp))
    dv(lambda e: e.tensor_scalar(out=gpen.ap[:, 0:4], in0=goh.ap[:, 0:4], scalar1=-1.0, scalar2=BIG, op0=ALU.add, op1=ALU.mult), [goh], [gpen])
    gp_b = bass.AP(gpen.ap.tensor, gpen.ap.offset, [[gpen.ap.ap[0][0], 128], [1, 4], [0, 8]])
    dv(lambda e: e.tensor_tensor(out=elm.ap[:, 0:32].rearrange("p (g j) -> p g j", g=4), in0=lg.ap[:, 4:36].rearrange("p (g j) -> p g j", g=4), in1=gp_b, op=ALU.add), [lg, gpen], [elm])
    dv(lambda e: e.reduce_max(out=m1.ap[:, 0:1], in_=elm.ap[:, 0:32], axis=AX.X), [elm], [m1])
    dv(lambda e: e.tensor_scalar(out=oh1.ap[:, 0:32], in0=elm.ap[:, 0:32], scalar1=m1.ap[:, 0:1], scalar2=None, op0=ALU.is_equal), [elm, m1], [oh1])
    dv(lambda e: e.scalar_tensor_tensor(out=elm2.ap[:, 0:32], in0=oh1.ap[:, 0:32], scalar=-BIG, in1=elm.ap[:, 0:32], op0=ALU.mult, op1=ALU.add), [oh1, elm], [elm2])
    dv(lambda e: e.reduce_max(out=m2.ap[:, 0:1], in_=elm2.ap[:, 0:32], axis=AX.X), [elm2], [m2])
    dv(lambda e: e.tensor_scalar(out=oh2.ap[:, 0:32], in0=elm2.ap[:, 0:32], scalar1=m2.ap[:, 0:1], scalar2=None, op0=ALU.is_equal), [elm2, m2], [oh2])
    dv(lambda e: e.tensor_tensor(out=m2.ap[:, 1:2], in0=m2.ap[:, 0:1], in1=m1.ap[:, 0:1], op=ALU.subtract), [m2, m1], [m2])
    P.op("act", lambda e: e.activation(out=m2.ap[:, 2:3], in_=m2.ap[:, 1:2], func=AF.Exp), rd=R(m2), wr=R(m2))
    dv(lambda e: e.reciprocal(out=tmp.ap[:, 5:6], in_=tmp.ap[:, 4:5]), [tmp], [tmp])
    dv(lambda e: e.tensor_scalar_add(out=m2.ap[:, 3:4], in0=m2.ap[:, 2:3], scalar1=1.0), [m2], [m2])
    dv(lambda e: e.reciprocal(out=m2.ap[:, 4:5], in_=m2.ap[:, 3:4]), [m2], [m2])
    dv(lambda e: e.tensor_tensor(out=gatew.ap[:, gt, 0:1], in0=m2.ap[:, 4:5], in1=tmp.ap[:, 5:6], op=ALU.mult), [m2, tmp], [gatew])
    dv(lambda e: e.tensor_tensor(out=gatew.ap[:, gt, 1:2], in0=gatew.ap[:, gt, 0:1], in1=m2.ap[:, 2:3], op=ALU.mult), [gatew, m2], [gatew])


def route_b(P, sm, cnt, eoff, ohbf, lowtri, ones_b, PB, nxt, slot_i, gt):
    R = lambda *ts: [t.r for t in ts]
    m1 = sm[5]; oh1 = sm[6]; oh2 = sm[9]; tmp2 = sm[11]
    dv = lambda op, rd, wr: P.op("dve", op, rd=R(*rd), wr=R(*wr))
    dv(lambda e: e.tensor_tensor(out=ohbf.ap[:, 0:32], in0=oh1.ap[:, 0:32], in1=oh2.ap[:, 0:32], op=ALU.add), [oh1, oh2], [ohbf])
    pp = PB[nxt("pj", 2)]
    P.op("pe", lambda e: e.matmul(out=pp.ap[:, 0:32], lhsT=lowtri.ap, rhs=ohbf.ap[:, 0:32], start=True, stop=True), rd=R(lowtri, ohbf), wr=R(pp))
    P.op("pe", lambda e: e.matmul(out=pp.ap[:, 32:64], lhsT=ones_b.ap, rhs=ohbf.ap[:, 0:32], start=True, stop=True, skip_group_check=True), rd=R(ones_b, ohbf), wr=R(pp))
    dv(lambda e: e.tensor_tensor(out=tmp2.ap[:, 0:32], in0=cnt.ap, in1=eoff.ap, op=ALU.add), [cnt, eoff], [tmp2])
    dv(lambda e: e.tensor_tensor(out=tmp2.ap[:, 0:32], in0=pp.ap[:, 0:32], in1=tmp2.ap[:, 0:32], op=ALU.add), [pp, tmp2], [tmp2])
    dv(lambda e: e.tensor_tensor(out=cnt.ap, in0=pp.ap[:, 32:64], in1=cnt.ap, op=ALU.add), [pp, cnt], [cnt])
    dv(lambda e: e.tensor_tensor(out=oh1.ap[:, 0:32], in0=oh1.ap[:, 0:32], in1=tmp2.ap[:, 0:32], op=ALU.mult), [oh1, tmp2], [oh1])
    dv(lambda e: e.tensor_tensor(out=oh2.ap[:, 0:32], in0=oh2.ap[:, 0:32], in1=tmp2.ap[:, 0:32], op=ALU.mult), [oh2, tmp2], [oh2])
    dv(lambda e: e.reduce_sum(out=m1.ap[:, 1:2], in_=oh1.ap[:, 0:32], axis=AX.X), [oh1], [m1])
    dv(lambda e: e.reduce_sum(out=m1.ap[:, 2:3], in_=oh2.ap[:, 0:32], axis=AX.X), [oh2], [m1])
    dv(lambda e: e.tensor_copy(out=slot_i.ap[:, gt, 0:2], in_=m1.ap[:, 1:3]), [m1], [slot_i])


_NC_CACHE = {}


def make_in_map(inputs, core, ncores, S, NSEQ, CAPB):
    f = lambda a: np.ascontiguousarray(np.asarray(a, dtype=np.float32))
    x = np.asarray(inputs["x"]); c = np.asarray(inputs["c"])
    b0 = core * NSEQ
    m = {}
    m["x"] = f(x[b0:b0 + NSEQ, :S]).reshape(NSEQ * S, D)
    m["c"] = f(c[b0:b0 + NSEQ])
    m["w_in"] = f(inputs["w_in"][0]); m["w_out"] = f(inputs["w_out"][0])
    sinks = np.asarray(inputs["sinks"][0], np.float32)
    m["sinks_fm"] = f(np.stack([sinks[2 * j + (np.arange(128) // 64)] for j in range(8)], axis=1))
    m["rel_bias"] = f(inputs["rel_bias"])
    m["na_fm"] = f(np.asarray(inputs["norm_a"][0]).reshape(8, 128).T)
    m["nb_fm"] = f(np.asarray(inputs["norm_b"][0]).reshape(8, 128).T)
    m["w_ada"] = f(inputs["w_ada"][0]); m["b_ada"] = f(inputs["b_ada"][0]).reshape(1, 6 * D)
    for k in ("ln1_g", "ln1_b", "ln2_g", "ln2_b"):
        m[k] = f(inputs[k][0]).reshape(1, D)
    m["w_grp"] = f(inputs["w_grp"][0]); m["b_grp"] = f(inputs["b_grp"][0]).reshape(1, 4)
    m["w_rtr"] = f(inputs["w_rtr"][0]); m["b_rtr"] = f(inputs["b_rtr"][0]).reshape(1, 32)
    m["w_gate"] = f(inputs["w_gate"][0]); m["w_up"] = f(inputs["w_up"][0]); m["w_down"] = f(inputs["w_down"][0])
    m.update(host_consts(CAPB * 128))
    return m


def kernel(**inputs):
    NCORES = 8
    S, NSEQ, CAPB = 2048, 2, 3
    key = (S, NSEQ, CAPB)
    if key not in _NC_CACHE:
        _NC_CACHE[key] = build_nc(S, NSEQ, CAPB)[0]
    nc = _NC_CACHE[key]
    shared = make_in_map(inputs, 0, NCORES, S, NSEQ, CAPB)
    in_maps = []
    for core in range(NCORES):
        m = dict(shared)
        b0 = core * NSEQ
        m["x"] = np.ascontiguousarray(np.asarray(inputs["x"], np.float32)[b0:b0 + NSEQ]).reshape(NSEQ * S, D)
        m["c"] = np.ascontiguousarray(np.asarray(inputs["c"], np.float32)[b0:b0 + NSEQ])
        in_maps.append(m)
    res = run_bass_kernel_spmd(nc, in_maps, core_ids=list(range(NCORES)))
    out = np.concatenate([np.asarray(r["out"]).reshape(NSEQ, S, D) for r in res.results], axis=0)
    return out.astype(np.float32)
```

```python
import math
import types
import numpy as np
import ml_dtypes
import concourse.bass as bass
import concourse.mybir as mybir
from concourse.bass_utils import run_bass_kernel_spmd

F32 = mybir.dt.float32
BF16 = mybir.dt.bfloat16
I32 = mybir.dt.int32
U32 = mybir.dt.uint32
AF = mybir.ActivationFunctionType
ALU = mybir.AluOpType
AX = mybir.AxisListType

ENGS = ("pe", "act", "dve", "pool", "sp")


class Res:
    __slots__ = ("name", "w", "r", "excl", "dsem")

    def __init__(self, name, excl=False):
        self.name = name
        self.w = None
        self.r = []
        self.excl = excl
        self.dsem = None


class Tok:
    __slots__ = ("kind", "eng", "seq", "sem", "val")

    def __init__(self, kind, eng=None, seq=None, sem=None, val=None):
        self.kind = kind; self.eng = eng; self.seq = seq; self.sem = sem; self.val = val


class Op:
    __slots__ = ("fn", "waits", "signal", "dma_sem", "seq")

    def __init__(self, fn):
        self.fn = fn; self.waits = []; self.signal = False; self.dma_sem = None; self.seq = 0


def _freeze(fn):
    if fn is None or fn.__closure__ is None:
        return fn
    cells = []
    for c in fn.__closure__:
        try:
            cells.append(types.CellType(c.cell_contents))
        except ValueError:
            cells.append(c)
    return types.FunctionType(fn.__code__, fn.__globals__, fn.__name__, fn.__defaults__, tuple(cells))


class Prog:
    def __init__(self, nc):
        self.nc = nc
        self.ops = {e: [] for e in ENGS}
        self.seen = {e: {} for e in ENGS}
        self.dsems = []
        self.nres = 0
        self.ncomp = {e: 0 for e in ENGS}
        self.lastc = {e: None for e in ENGS}

    def res(self, name=None, excl=False):
        self.nres += 1
        return Res(name or f"r{self.nres}", excl)

    def _need(self, eng, op, tok, same_eng_raw=False, is_dma=False):
        if tok is None:
            return
        if tok.kind == "e":
            if tok.eng == eng and not is_dma:
                if not same_eng_raw or eng == "pe":
                    return
                if tok.val < self.ncomp[eng] - 1:
                    return
            key = ("e", tok.eng)
            if self.seen[eng].get(key, 0) >= tok.seq:
                return
            self.seen[eng][key] = tok.seq
            self.ops[tok.eng][tok.seq - 1].signal = True
            op.waits.append(tok)
        else:
            key = ("d", tok.sem)
            if self.seen[eng].get(key, 0) >= tok.val:
                return
            self.seen[eng][key] = tok.val
            op.waits.append(tok)

    def _deps(self, eng, op, rd, wr, is_dma=False):
        for r in rd:
            if r.excl:
                wr = list(wr) + [r]
                continue
            self._need(eng, op, r.w, same_eng_raw=True, is_dma=is_dma)
        for w in wr:
            self._need(eng, op, w.w, same_eng_raw=False, is_dma=is_dma)
            for t in w.r:
                self._need(eng, op, t, is_dma=is_dma)

    def _commit(self, tok, rd, wr):
        for r in rd:
            if r.excl:
                r.w = tok; r.r = []
            else:
                r.r.append(tok)
        for w in wr:
            w.w = tok; w.r = []

    def op(self, eng, fn, rd=(), wr=()):
        o = Op(_freeze(fn))
        self._deps(eng, o, rd, wr)
        self.ops[eng].append(o)
        o.seq = len(self.ops[eng])
        self.ncomp[eng] += 1
        tok = Tok("e", eng=eng, seq=o.seq, val=self.ncomp[eng])
        self.lastc[eng] = tok
        self._commit(tok, rd, wr)
        return tok

    def dma(self, eng, fn, sb, rd=(), wr=()):
        if sb.dsem is None:
            sb.dsem = len(self.dsems)
            self.dsems.append([0])
        o = Op(_freeze(fn))
        cur = self.dsems[sb.dsem][0]
        if cur:
            self._need(eng, o, Tok("d", sem=sb.dsem, val=cur))
        self._deps(eng, o, rd, wr, is_dma=True)
        self.dsems[sb.dsem][0] = cur + 16
        o.dma_sem = sb.dsem
        self.ops[eng].append(o)
        o.seq = len(self.ops[eng])
        tok = Tok("d", sem=sb.dsem, val=cur + 16)
        self._commit(tok, rd, wr)
        return tok

    def barrier(self, all_res=()):
        toks = []
        for e in ENGS:
            if self.lastc[e] is not None:
                toks.append(self.lastc[e])
        for i, v in enumerate(self.dsems):
            if v[0]:
                toks.append(Tok("d", sem=i, val=v[0]))
        for e in ENGS:
            o = Op(None)
            for t in toks:
                if t.kind == "e" and t.eng == e:
                    continue
                self._need(e, o, t)
            if o.waits:
                self.ops[e].append(o)
                o.seq = len(self.ops[e])

    def final_wait(self, eng="sp"):
        o = Op(None)
        for i, v in enumerate(self.dsems):
            if v[0]:
                self._need(eng, o, Tok("d", sem=i, val=v[0]))
        for e in ENGS:
            if e != eng and self.lastc[e] is not None:
                self._need(eng, o, self.lastc[e])
        self.ops[eng].append(o)
        o.seq = len(self.ops[eng])

    def emit(self):
        nc = self.nc
        esem = {e: nc.alloc_semaphore(name=f"sem_{e}") for e in ENGS}
        dsem = [nc.alloc_semaphore(name=f"dsem{i}") for i in range(len(self.dsems))]
        sigval = {}
        for e in ENGS:
            c = 0
            vals = []
            for o in self.ops[e]:
                if o.signal:
                    c += 1
                vals.append(c)
            sigval[e] = vals
        ops = self.ops
        stats = {e: (len(ops[e]), sigval[e][-1] if sigval[e] else 0) for e in ENGS}

        def run(engname, eng):
            for o in ops[engname]:
                for t in o.waits:
                    if t.kind == "e":
                        eng.wait_ge(esem[t.eng], sigval[t.eng][t.seq - 1])
                    else:
                        eng.wait_ge(dsem[t.sem], t.val)
                if o.fn is None:
                    continue
                ins = o.fn(eng)
                if o.dma_sem is not None:
                    ins.then_inc(dsem[o.dma_sem], 16)
                elif o.signal:
                    ins.then_inc(esem[engname], 1)

        with nc.Block() as block:
            @block.tensor
            def _(e):
                run("pe", e)

            @block.scalar
            def _(e):
                run("act", e)

            @block.vector
            def _(e):
                run("dve", e)

            @block.gpsimd
            def _(e):
                run("pool", e)

            @block.sync
            def _(e):
                run("sp", e)
        return stats


D = 2048
HD = 64
QKV = 4352
NEXP = 32
DE = 512
ALPHA = 2.0 ** 0.25
EPS = 1e-5
BIG = 30000.0
CA_Q, CA_K, CA_V, CB_Q, CB_K, CB_V = 0, 1024, 1152, 1280, 2304, 3328


class T:
    __slots__ = ("ap", "r")

    def __init__(self, ap, r):
        self.ap = ap; self.r = r


class Arena:
    def __init__(self, P, tensor, nwords):
        self.P = P; self.t = tensor; self.n = nwords; self.off = 0

    def _take(self, words):
        a = self.t[:, self.off:self.off + words]
        self.off += words
        assert self.off <= self.n, f"arena overflow {self.off} > {self.n}"
        return a

    def f32(self, cols, pat=None, **kw):
        a = self._take(cols)
        return T(a.rearrange(pat, **kw) if pat else a, self.P.res())

    def i32(self, cols, pat=None, **kw):
        a = self._take(cols).bitcast(I32)
        return T(a.rearrange(pat, **kw) if pat else a, self.P.res())

    def bf(self, cols, pat=None, **kw):
        a = self._take((cols + 1) // 2).bitcast(BF16)[:, 0:cols]
        return T(a.rearrange(pat, **kw) if pat else a, self.P.res())


def host_consts(CAP):
    c = {}
    c["eoff"] = np.tile((np.arange(32, dtype=np.float32) * CAP)[None, :], (128, 1))
    c["ident"] = np.eye(128, dtype=np.float32)
    j = np.arange(128)[:, None]; s_ = np.arange(128)[None, :]
    c["negtri"] = np.where(j >= s_, -1.0, 0.0).astype(ml_dtypes.bfloat16)
    c["negsu"] = np.where(j < s_, -1.0, 0.0).astype(ml_dtypes.bfloat16)
    c["lowtri"] = np.where(j < s_, 1.0, 0.0).astype(ml_dtypes.bfloat16)
    tq = np.arange(512)[None, :]
    c["sbmask"] = np.stack([(jj * 128 + np.arange(128)[:, None] < tq) for jj in range(4)], axis=1).astype(np.float32)
    oh = np.zeros((128, 2, 128), np.float32); oh[:, 0, 0:64] = 1.0; oh[:, 1, 64:128] = 1.0
    c["oneshalf"] = oh.astype(ml_dtypes.bfloat16)
    def bucket(n):
        n = max(n, 0); me = 16
        if n < me:
            return n
        ratio = np.float32(max(n, me)) / np.float32(me)
        large = me + int(np.float32(np.log(ratio) / np.float32(math.log(128 / me))) * (32 - me))
        return min(large, 31)
    ohb = np.zeros((32, 512), np.float32); neg = np.zeros((1, 512), np.float32)
    for i in range(256):
        if i <= 127:
            ohb[bucket(127 - i), i] = 1.0
        else:
            neg[0, i] = -BIG
        if 128 <= i <= 254:
            ohb[bucket(255 - i), 256 + i] = 1.0
        else:
            neg[0, 256 + i] = -BIG
    c["ohb"] = ohb; c["negrow"] = neg
    return c


def build_nc(S=2048, NSEQ=2, CAPB=3, dbg=False):
    nc = bass.Bass("TRN2", target_bir_lowering=False)
    TK = NSEQ * S
    NT = S // 128
    NQ = S // 512
    NTT = TK // 128
    CAP = CAPB * 128
    NSLOT = NEXP * CAP

    def din(name, shape, dt=F32):
        return nc.dram_tensor(name, list(shape), dt, kind="ExternalInput").ap()

    x_d = din("x", [TK, D]); c_d = din("c", [NSEQ, D]); w_in_d = din("w_in", [D, QKV]); w_out_d = din("w_out", [D, D])
    sinks_d = din("sinks_fm", [128, 8]); relb_d = din("rel_bias", [32, 16])
    na_d = din("na_fm", [128, 8]); nb_d = din("nb_fm", [128, 8])
    w_ada_d = din("w_ada", [D, 6 * D]); b_ada_d = din("b_ada", [1, 6 * D])
    ln1g_d = din("ln1_g", [1, D]); ln1b_d = din("ln1_b", [1, D]); ln2g_d = din("ln2_g", [1, D]); ln2b_d = din("ln2_b", [1, D])
    wgrp_d = din("w_grp", [D, 4]); bgrp_d = din("b_grp", [1, 4]); wrtr_d = din("w_rtr", [D, 32]); brtr_d = din("b_rtr", [1, 32])
    wg_d = din("w_gate", [NEXP, D, DE]); wu_d = din("w_up", [NEXP, D, DE]); wd_d = din("w_down", [NEXP, DE, D])
    ident_d = din("ident", [128, 128]); negtri_d = din("negtri", [128, 128], BF16); negsu_d = din("negsu", [128, 128], BF16)
    lowtri_d = din("lowtri", [128, 128], BF16); sbmask_d = din("sbmask", [128, 4, 512]); oneshalf_d = din("oneshalf", [128, 2, 128], BF16)
    ohb_d = din("ohb", [32, 512]); negrow_d = din("negrow", [1, 512]); eoff_d = din("eoff", [128, 32])
    out_d = nc.dram_tensor("out", [TK, D], F32, kind="ExternalOutput").ap()

    def dscr(name, shape, dt):
        return nc.dram_tensor(name, list(shape), dt, kind="Internal").ap()

    mod_d = dscr("mod_scr", [NSEQ, 6 * D], F32)
    gb_d = dscr("gb_scr", [16, 512], F32)
    ogT_d = dscr("ogT_scr", [NSEQ, D, S], BF16)
    x1_d = dscr("x1_scr", [TK, D], F32)
    xs_d = dscr("xs_scr", [NSLOT, D], BF16)
    ys_d = dscr("ys_scr", [NSLOT, D], F32)
    dbg_d = {}
    if dbg:
        dbg_d["ogT"] = nc.dram_tensor("dbg_ogT", [NSEQ, D, S], BF16, kind="ExternalOutput").ap()
        dbg_d["x1"] = nc.dram_tensor("dbg_x1", [TK, D], F32, kind="ExternalOutput").ap()
        dbg_d["mod"] = nc.dram_tensor("dbg_mod", [NSEQ, 6 * D], F32, kind="ExternalOutput").ap()
        dbg_d["ssq"] = nc.dram_tensor("dbg_ssq", [NSEQ, 128, 2 * NT], F32, kind="ExternalOutput").ap()
        dbg_d["slot"] = nc.dram_tensor("dbg_slot", [128, NTT * 2], I32, kind="ExternalOutput").ap()
        dbg_d["gw"] = nc.dram_tensor("dbg_gw", [128, NTT * 2], F32, kind="ExternalOutput").ap()
        dbg_d["eb"] = nc.dram_tensor("dbg_eb", [128, 4096], BF16, kind="ExternalOutput").ap()
        for nm_ in ("den", "o", "pcf", "ppf", "psd"):
            dbg_d[nm_] = nc.dram_tensor("dbg_" + nm_, [2, 128, 512], F32, kind="ExternalOutput").ap()

    NW = 206 * 256
    arena_t = nc.alloc_sbuf_tensor("arena", [128, NW], F32)
    psum_t = nc.alloc_psum_tensor("psum", [128, 8 * 512], F32)
    P = Prog(nc)
    A = Arena(P, arena_t, NW)
    PB = [T(psum_t[:, i * 512:(i + 1) * 512], P.res(f"bank{i}", excl=True)) for i in range(8)]
    rot = {}

    def nxt(key, n):
        rot[key] = (rot.get(key, -1) + 1) % n
        return rot[key]

    def R(*ts):
        return [t.r for t in ts]

    r_mod = P.res("mod_d"); r_gb = P.res("gb_d"); r_xs = P.res("xs_d")
    r_og = [P.res(f"og_d{b}") for b in range(NSEQ)]
    r_x1 = [P.res(f"x1_d{t}") for t in range(NTT)]
    r_ys = [P.res(f"ys_d{e}") for e in range(NEXP)]

    def load(eng, t, src, extra_rd=()):
        return P.dma(eng, lambda e: e.dma_start(out=t.ap, in_=src), t.r, rd=list(extra_rd), wr=[t.r])

    def store(eng, dst, t, src_ap=None, extra_wr=()):
        sa = t.ap if src_ap is None else src_ap
        return P.dma(eng, lambda e: e.dma_start(out=dst, in_=sa), t.r, rd=[t.r], wr=list(extra_wr))

    ident = A.f32(128); negtri = A.bf(128); negsu = A.bf(128); lowtri = A.bf(128)
    onesh = A.bf(256, "p (r c) -> p r c", r=2)
    ones_f = A.f32(128); ones_b = A.bf(128)
    na_fm = A.f32(8); nb_fm = A.f32(8); esink = A.f32(8)
    slot_i = A.i32(NTT * 2, "p (t k) -> p t k", k=2)
    gatew = A.f32(NTT * 2, "p (t k) -> p t k", k=2)
    ssq = A.f32(2 * NT, "p (g t) -> p g t", g=2)
    cnt_keep = A.f32(32); eoff = A.f32(32)
    load("sp", eoff, eoff_d)
    P.op("pool", lambda e: e.memset(cnt_keep.ap, 0.0), wr=R(cnt_keep))
    load("sp", ident, ident_d); load("sp", negtri, negtri_d); load("sp", negsu, negsu_d); load("sp", lowtri, lowtri_d)
    load("sp", onesh, oneshalf_d); load("sp", na_fm, na_d); load("sp", nb_fm, nb_d); load("sp", esink, sinks_d)
    P.op("pool", lambda e: e.memset(ones_f.ap, 1.0), wr=R(ones_f))
    P.op("pool", lambda e: e.memset(ones_b.ap, 1.0), wr=R(ones_b))
    P.op("act", lambda e: e.activation(out=esink.ap, in_=esink.ap, func=AF.Exp), rd=R(esink), wr=R(esink))
    base_off = A.off

    ct = A.f32(16 * NSEQ, "p (k b) -> p k b", b=NSEQ)
    sg = A.f32(16 * NSEQ, "p (k b) -> p k b", b=NSEQ)
    with nc.allow_non_contiguous_dma(reason="tiny transposed load of c"):
        for bb in range(NSEQ):
            P.dma("sp", lambda e, bb=bb: e.dma_start(out=ct.ap[:, :, bb:bb + 1], in_=c_d[bb:bb + 1, :].rearrange("o (k p) -> p k o", p=128), allow_slow_non_contiguous=True), ct.r, wr=[ct.r])
    P.op("act", lambda e: e.activation(out=sg.ap, in_=ct.ap, func=AF.Exp, scale=-1.0), rd=R(ct), wr=R(sg))
    P.op("dve", lambda e: e.tensor_scalar_add(out=sg.ap, in0=sg.ap, scalar1=1.0), rd=R(sg), wr=R(sg))
    P.op("dve", lambda e: e.reciprocal(out=sg.ap, in_=sg.ap), rd=R(sg), wr=R(sg))
    P.op("dve", lambda e: e.tensor_tensor(out=ct.ap, in0=ct.ap, in1=sg.ap, op=ALU.mult), rd=R(ct, sg), wr=R(ct))
    wa = [A.f32(4096) for _ in range(3)]
    brow = A.f32(4096); mrow = A.f32(4096)
    w_ada_v = w_ada_d.rearrange("(k p) n -> p k n", p=128)
    for third in range(3):
        c0 = third * 4096
        P.dma("sp", lambda e, c0=c0: e.dma_start(out=brow.ap[0:1, :], in_=b_ada_d[:, c0:c0 + 4096]), brow.r, wr=[brow.r])
        for k in range(16):
            w = wa[nxt("wa", 3)]
            load("sp", w, w_ada_v[:, k, c0:c0 + 4096])
            for n8 in range(8):
                pb = PB[n8]
                P.op("pe", lambda e, k=k, w=w, pb=pb, n8=n8: e.matmul(out=pb.ap[0:NSEQ, :], lhsT=ct.ap[:, k, :], rhs=w.ap[:, n8 * 512:(n8 + 1) * 512], start=(k == 0), stop=False),
                     rd=R(ct, w), wr=R(pb))
        for n8 in range(8):
            pb = PB[n8]
            P.op("pe", lambda e, pb=pb, n8=n8: e.matmul(out=pb.ap[0:NSEQ, :], lhsT=ones_f.ap[0:1, 0:NSEQ], rhs=brow.ap[0:1, n8 * 512:(n8 + 1) * 512], start=False, stop=True),
                 rd=R(ones_f, brow), wr=R(pb))
            if n8 % 2 == 0:
                P.op("dve", lambda e, pb=pb, n8=n8: e.tensor_copy(out=mrow.ap[0:NSEQ, n8 * 512:(n8 + 1) * 512], in_=pb.ap[0:NSEQ, :]), rd=R(pb), wr=R(mrow))
            else:
                P.op("act", lambda e, pb=pb, n8=n8: e.activation(out=mrow.ap[0:NSEQ, n8 * 512:(n8 + 1) * 512], in_=pb.ap[0:NSEQ, :], func=AF.Copy), rd=R(pb), wr=R(mrow))
        store("sp", mod_d[:, c0:c0 + 4096], mrow, src_ap=mrow.ap[0:NSEQ, :], extra_wr=[r_mod])
    relb = A.f32(16); ohb = A.f32(512); negrow = A.f32(512); grow = A.f32(512)
    P.dma("sp", lambda e: e.dma_start(out=relb.ap[0:32, :], in_=relb_d), relb.r, wr=[relb.r])
    P.dma("sp", lambda e: e.dma_start(out=ohb.ap[0:32, :], in_=ohb_d), ohb.r, wr=[ohb.r])
    P.dma("sp", lambda e: e.dma_start(out=negrow.ap[0:1, :], in_=negrow_d), negrow.r, wr=[negrow.r])
    pb = PB[nxt("pj", 2)]
    P.op("pe", lambda e, pb=pb: e.matmul(out=pb.ap[0:16, :], lhsT=relb.ap[0:32, :], rhs=ohb.ap[0:32, :], start=True, stop=False), rd=R(relb, ohb), wr=R(pb))
    P.op("pe", lambda e, pb=pb: e.matmul(out=pb.ap[0:16, :], lhsT=ones_f.ap[0:1, 0:16], rhs=negrow.ap[0:1, :], start=False, stop=True), rd=R(ones_f, negrow), wr=R(pb))
    P.op("dve", lambda e, pb=pb: e.tensor_copy(out=grow.ap[0:16, :], in_=pb.ap[0:16, :]), rd=R(pb), wr=R(grow))
    store("sp", gb_d, grow, src_ap=grow.ap[0:16, :], extra_wr=[r_gb])
    if dbg:
        P.barrier()
        mt = A.f32(6 * D)
        P.dma("sp", lambda e: e.dma_start(out=mt.ap[0:NSEQ, :], in_=mod_d), mt.r, rd=[r_mod], wr=[mt.r])
        store("sp", dbg_d["mod"], mt, src_ap=mt.ap[0:NSEQ, :])
    P.barrier()
    A.off = base_off

    def bcast_row(src_row_ap):
        return bass.AP(src_row_ap.tensor, src_row_ap.offset, [[0, 128], [1, src_row_ap.shape[-1]]])

    w_in_v = w_in_d.rearrange("(k p) n -> p k n", p=128)
    w_out_v = w_out_d.rearrange("(k p) n -> p k n", p=128)

    A.off = base_off
    hT = A.bf(16 * S, "p (k s) -> p k s", k=16)
    sc1 = A.f32(16); sh1 = A.f32(16)
    wch = [A.bf(16 * 128, "p (k n) -> p k n", k=16) for _ in range(3)]
    qT = [A.bf(S) for _ in range(2)]
    sq_t = A.f32(512)
    og_t = [A.bf(512) for _ in range(2)]
    hks = [A.f32(128) for _ in range(3)]
    uni_off = A.off
    xt2 = [A.f32(2 * D, "p (i d) -> p i d", i=2) for _ in range(2)]
    A.off = uni_off
    kTa = A.bf(S); kTas = A.bf(S)
    VA = [[A.bf(NT * 128, "p (t n) -> p t n", n=128) for _ in range(2)] for _ in range(2)]
    EB = A.bf(16 * 2 * 128, "p (h c n) -> p h c n", h=16, c=2)
    pf_t = [A.f32(512) for _ in range(4)]; pb_t = [A.bf(512) for _ in range(4)]
    den_t = A.f32(512); o_t = A.f32(512)
    A.off = uni_off
    kT = [A.bf(S) for _ in range(2)]
    Vp = [[A.bf(NT * 128, "p (t n) -> p t n", n=128) for _ in range(2)] for _ in range(2)]
    sbm = A.f32(4 * 512, "p (j n) -> p j n", j=4)
    e_t = [A.f32(1024) for _ in range(5)]
    sp_t = [A.bf(1024) for _ in range(4)]; ec_t = [A.f32(1024) for _ in range(3)]; a_t = [A.bf(1024) for _ in range(3)]
    A.off = base_off
    wo = A.bf(16 * D, "p (k n) -> p k n", k=16)
    GG = A.f32(D); BB = A.f32(D); LG = A.f32(D); LB = A.f32(D)
    ogt = [A.bf(16 * 128, "p (k s) -> p k s", k=16) for _ in range(2)]
    xtl = [A.f32(D) for _ in range(2)]
    mixn = A.f32(D); h2_2 = [A.f32(D) for _ in range(2)]
    nrm2 = [A.f32(D) for _ in range(2)]; x1t2 = [A.f32(D) for _ in range(2)]
    h2b = [A.bf(D) for _ in range(3)]
    lg2 = [A.f32(40) for _ in range(2)]; smr = [A.f32(40) for _ in range(12)]
    h2T = A.f32(16 * 128, "p (k s) -> p k s", k=16)
    wr_t = A.f32(16 * 36, "p (k n) -> p k n", k=16); brt = A.f32(36)
    rstd = A.f32(2 * NT, "p (g t) -> p g t", g=2)
    st6 = A.f32(24); mv = A.f32(2); sm = [None] * 10 + [A.f32(8), A.f32(8)]
    ohbf = A.bf(32)

    for b in range(NSEQ):
        with nc.allow_non_contiguous_dma(reason="tiny feature-major load of adaLN shift/scale"):
            P.dma("sp", lambda e, b=b: e.dma_start(out=sh1.ap, in_=mod_d[b:b + 1, 0:D].rearrange("o (k p) -> p (o k)", p=128), allow_slow_non_contiguous=True), sh1.r, rd=[r_mod], wr=[sh1.r])
            P.dma("sp", lambda e, b=b: e.dma_start(out=sc1.ap, in_=mod_d[b:b + 1, D:2 * D].rearrange("o (k p) -> p (o k)", p=128), allow_slow_non_contiguous=True), sc1.r, rd=[r_mod], wr=[sc1.r])
        P.op("dve", lambda e: e.tensor_scalar_add(out=sc1.ap, in0=sc1.ap, scalar1=1.0), rd=R(sc1), wr=R(sc1))
        P.op("dve", lambda e: e.memset(ssq.ap, 0.0), wr=R(ssq))
        for tb in range(S // 256):
            tok0 = b * S + tb * 256
            xt = xt2[tb % 2]
            load("sp", xt, x_d[tok0:tok0 + 256, :].rearrange("(i p) d -> p i d", p=128))
            for k2 in range(8):
                pbk = PB[nxt("pj", 2)]
                for kk in range(2):
                    k = k2 * 2 + kk
                    for i in range(2):
                        P.op("pe", lambda e, pbk=pbk, kk=kk, i=i, k=k: e.transpose(out=pbk.ap[:, kk * 256 + i * 128: kk * 256 + (i + 1) * 128],
                                                                                 in_=xt.ap[:, i, k * 128:(k + 1) * 128], identity=ident.ap),
                             rd=R(xt, ident), wr=R(pbk))
                for kk in range(2):
                    k = k2 * 2 + kk
                    dst = hT.ap[:, k, tb * 256:(tb + 1) * 256]
                    if kk == 0:
                        P.op("act", lambda e, pbk=pbk, kk=kk, k=k, dst=dst: e.activation(out=dst, in_=pbk.ap[:, kk * 256:(kk + 1) * 256], func=AF.Identity,
                                                                                       scale=sc1.ap[:, k:k + 1], bias=sh1.ap[:, k:k + 1]),
                             rd=R(pbk, sc1, sh1), wr=R(hT))
                    else:
                        P.op("dve", lambda e, pbk=pbk, kk=kk, k=k, dst=dst: e.tensor_scalar(out=dst, in0=pbk.ap[:, kk * 256:(kk + 1) * 256], scalar1=sc1.ap[:, k:k + 1],
                                                                                          scalar2=sh1.ap[:, k:k + 1], op0=ALU.mult, op1=ALU.add),
                             rd=R(pbk, sc1, sh1), wr=R(hT))

        P.barrier()
        for r in range(2):
            for rr in range(2):
                P.op("pool", lambda e, t=VA[r][rr]: e.memset(t.ap, 0.0), wr=R(VA[r][rr]))
        for h in range(16):
            for cpv in range(2):
                hk = hks[(2 * h + cpv) % 3]
                src = bass.AP(gb_d.tensor, gb_d.offset + h * 512 + cpv * 256, [[1, 128], [1, 128]])
                P.dma("sp", lambda e, src=src: e.dma_start(out=hk.ap, in_=src), hk.r, rd=[r_gb], wr=[hk.r])
                P.op("act", lambda e: e.activation(out=hk.ap, in_=hk.ap, func=AF.Exp), rd=R(hk), wr=R(hk))
                rev = bass.AP(hk.ap.tensor, hk.ap.offset + 127, [[hk.ap.ap[0][0], 128], [-1, 128]])
                P.op("pool", lambda e, h=h, cpv=cpv, rev=rev: e.tensor_copy(out=EB.ap[:, h, cpv, :], in_=rev), rd=R(hk), wr=R(EB))

        def load_w(c0):
            w = wch[nxt("wch", 3)]
            P.dma("pool", lambda e, w=w, c0=c0: e.dma_start(out=w.ap, in_=w_in_v[:, :, c0:c0 + 128]), w.r, wr=[w.r])
            return w

        def proj_fm(w, dst):
            for tb in range(NQ):
                pbk = PB[nxt("pj", 2)]
                for k in range(16):
                    P.op("pe", lambda e, k=k, tb=tb, pbk=pbk: e.matmul(out=pbk.ap, lhsT=w.ap[:, k, :], rhs=hT.ap[:, k, tb * 512:(tb + 1) * 512], start=(k == 0), stop=(k == 15)),
                         rd=R(w, hT), wr=R(pbk))
                if tb % 2 == 0:
                    P.op("act", lambda e, tb=tb, pbk=pbk: e.activation(out=dst.ap[:, tb * 512:(tb + 1) * 512], in_=pbk.ap, func=AF.Copy), rd=R(pbk), wr=R(dst))
                else:
                    P.op("dve", lambda e, tb=tb, pbk=pbk: e.tensor_copy(out=dst.ap[:, tb * 512:(tb + 1) * 512], in_=pbk.ap), rd=R(pbk), wr=R(dst))

        def proj_tm(w, evac):
            for t4 in range(NQ):
                pbk = PB[nxt("pj", 2)]
                for i in range(4):
                    tk = t4 * 4 + i
                    for k in range(16):
                        P.op("pe", lambda e, k=k, i=i, tk=tk, pbk=pbk: e.matmul(out=pbk.ap[:, i * 128:(i + 1) * 128], lhsT=hT.ap[:, k, tk * 128:(tk + 1) * 128], rhs=w.ap[:, k, :],
                                                                               start=(k == 0), stop=(k == 15)),
                             rd=R(w, hT), wr=R(pbk))
                evac(pbk, t4)

        def ssq_update(grp, qs, sq):
            pbk = PB[nxt("pj", 2)]
            for i in range(4):
                P.op("pe", lambda e, i=i, pbk=pbk: e.matmul(out=pbk.ap[:, i:i + 1], lhsT=sq.ap[:, i * 128:(i + 1) * 128], rhs=ones_f.ap[:, 0:1], start=True, stop=True),
                     rd=R(sq, ones_f), wr=R(pbk))
            P.op("dve", lambda e, pbk=pbk: e.tensor_tensor(out=ssq.ap[:, grp, qs * 4:qs * 4 + 4], in0=pbk.ap[:, 0:4], in1=ssq.ap[:, grp, qs * 4:qs * 4 + 4], op=ALU.add),
                 rd=R(pbk, ssq), wr=R(ssq))

        w = load_w(CA_K)
        proj_fm(w, kTa)
        wsw = wch[nxt("wch", 3)]
        P.op("pool", lambda e, w=w, wsw=wsw: e.tensor_copy(out=wsw.ap[:, :, 0:64], in_=w.ap[:, :, 64:128]), rd=R(w), wr=R(wsw))
        P.op("pool", lambda e, w=w, wsw=wsw: e.tensor_copy(out=wsw.ap[:, :, 64:128], in_=w.ap[:, :, 0:64]), rd=R(w), wr=R(wsw))
        wsave = w
        w = wsw
        proj_fm(w, kTas)
        w = load_w(CA_V)

        def evac_va(pbk, t4):
            v4 = pbk.ap.rearrange("p (i n) -> p i n", i=4)
            P.op("act", lambda e: e.activation(out=VA[0][0].ap[:, t4 * 4:t4 * 4 + 4, 0:64], in_=v4[:, :, 0:64], func=AF.Copy), rd=R(pbk), wr=R(VA[0][0]))
            P.op("dve", lambda e: e.tensor_copy(out=VA[0][1].ap[:, t4 * 4:t4 * 4 + 4, 64:128], in_=v4[:, :, 0:64]), rd=R(pbk), wr=R(VA[0][1]))
            P.op("act", lambda e: e.activation(out=VA[1][0].ap[:, t4 * 4:t4 * 4 + 4, 0:64], in_=v4[:, :, 64:128], func=AF.Copy), rd=R(pbk), wr=R(VA[1][0]))
            P.op("dve", lambda e: e.tensor_copy(out=VA[1][1].ap[:, t4 * 4:t4 * 4 + 4, 64:128], in_=v4[:, :, 64:128]), rd=R(pbk), wr=R(VA[1][1]))
        proj_tm(w, evac_va)

        swa_units = [(j, qs, r) for j in range(8) for qs in range(NQ) for r in range(2)]
        qts = {}; psod = {}

        def w1(u):
            j, qs, r = swa_units[u]
            if qs == 0 and r == 0:
                w = load_w(CA_Q + j * 128)
                qts[j] = qT[nxt("qT", 2)]
                proj_fm(w, qts[j])
            q = qts[j]; kv = j // 4
            KT = kTa if kv == r else kTas
            rows = slice(r * 64, r * 64 + 64)
            pcur = PB[2 * (u % 2)]; pprev = PB[2 * (u % 2) + 1]
            for i in range(4):
                tq = qs * 4 + i
                P.op("pe", lambda e, i=i, tq=tq: e.matmul(out=pcur.ap[:, i * 128:(i + 1) * 128], lhsT=KT.ap[rows, tq * 128:(tq + 1) * 128],
                                                         rhs=q.ap[rows, tq * 128:(tq + 1) * 128], start=True, stop=True), rd=R(KT, q), wr=R(pcur))
            for i in range(4):
                tq = qs * 4 + i
                if tq == 0:
                    continue
                P.op("pe", lambda e, i=i, tq=tq: e.matmul(out=pprev.ap[:, i * 128:(i + 1) * 128], lhsT=KT.ap[rows, (tq - 1) * 128:tq * 128],
                                                         rhs=q.ap[rows, tq * 128:(tq + 1) * 128], start=True, stop=True), rd=R(KT, q), wr=R(pprev))
            pcf = pf_t[2 * (u % 2)]; ppf = pf_t[2 * (u % 2) + 1]
            c0 = 128 if qs == 0 else 0
            P.op("act", lambda e: e.activation(out=pcf.ap, in_=pcur.ap, func=AF.Exp, scale=0.125), rd=R(pcur), wr=R(pcf))
            P.op("act", lambda e: e.activation(out=ppf.ap[:, c0:512], in_=pprev.ap[:, c0:512], func=AF.Exp, scale=0.125), rd=R(pprev), wr=R(ppf))

        def w2(u):
            j, qs, r = swa_units[u]
            h = 2 * j + r
            pcf = pf_t[2 * (u % 2)]; ppf = pf_t[2 * (u % 2) + 1]
            pcb = pb_t[2 * (u % 2)]; ppb = pb_t[2 * (u % 2) + 1]
            c0 = 128 if qs == 0 else 0
            n4 = 3 if qs == 0 else 4
            ebc = bass.AP(EB.ap.tensor, EB.ap[:, h, 0, :].offset, [[EB.ap.ap[0][0], 128], [0, 4], [1, 128]])
            ebp2 = bass.AP(EB.ap.tensor, EB.ap[:, h, 1, :].offset, [[EB.ap.ap[0][0], 128], [0, n4], [1, 128]])
            P.op("pool", lambda e: e.tensor_tensor(out=pcb.ap.rearrange("p (i n) -> p i n", i=4), in0=pcf.ap.rearrange("p (i n) -> p i n", i=4), in1=ebc, op=ALU.mult),
                 rd=R(pcf, EB), wr=R(pcb))
            P.op("dve", lambda e: e.tensor_tensor(out=ppb.ap[:, c0:512].rearrange("p (i n) -> p i n", i=n4), in0=ppf.ap[:, c0:512].rearrange("p (i n) -> p i n", i=n4),
                                                  in1=ebp2, op=ALU.mult), rd=R(ppf, EB), wr=R(ppb))

        def w3(u):
            j, qs, r = swa_units[u]
            kv = j // 4
            pcb = pb_t[2 * (u % 2)]; ppb = pb_t[2 * (u % 2) + 1]
            if r == 0:
                psod[(j, qs)] = (PB[6 + nxt("pso", 2)], PB[4 + nxt("psd", 2)])
            pso, psd = psod[(j, qs)]
            Vt = VA[kv][r]
            first = (r == 0)
            for i in range(4):
                tq = qs * 4 + i
                srcs = [(pcb, tq)] + ([(ppb, tq - 1)] if tq > 0 else [])
                for (pt, blk) in srcs:
                    P.op("pe", lambda e, i=i, pt=pt, blk=blk, st=first: e.matmul(out=pso.ap[:, i * 128:(i + 1) * 128], lhsT=Vt.ap[:, blk, :], rhs=pt.ap[:, i * 128:(i + 1) * 128],
                                                                                start=st, stop=True, skip_group_check=True), rd=R(Vt, pt), wr=R(pso))
                    first = False
            c0 = 128 if qs == 0 else 0
            P.op("pe", lambda e, st=(r == 0): e.matmul(out=psd.ap, lhsT=onesh.ap[:, r, :], rhs=pcb.ap, start=st, stop=True, skip_group_check=True), rd=R(onesh, pcb), wr=R(psd))
            P.op("pe", lambda e: e.matmul(out=psd.ap[:, c0:512], lhsT=onesh.ap[:, r, :], rhs=ppb.ap[:, c0:512], start=False, stop=True, skip_group_check=True), rd=R(onesh, ppb), wr=R(psd))
            if r == 1:
                P.op("dve", lambda e: e.tensor_scalar(out=den_t.ap, in0=psd.ap, scalar1=esink.ap[:, j:j + 1], scalar2=None, op0=ALU.add), rd=R(psd, esink), wr=R(den_t))
                P.op("dve", lambda e: e.reciprocal(out=den_t.ap, in_=den_t.ap), rd=R(den_t), wr=R(den_t))
                P.op("dve", lambda e: e.tensor_tensor(out=o_t.ap, in0=pso.ap, in1=den_t.ap, op=ALU.mult), rd=R(pso, den_t), wr=R(o_t))
                og = og_t[nxt("og", 2)]
                P.op("act", lambda e: e.activation(out=og.ap, in_=o_t.ap, func=AF.Copy, scale=na_fm.ap[:, j:j + 1]), rd=R(o_t, na_fm), wr=R(og))
                P.op("act", lambda e: e.activation(out=sq_t.ap, in_=o_t.ap, func=AF.Square), rd=R(o_t), wr=R(sq_t))
                store("sp", ogT_d[b, j * 128:(j + 1) * 128, qs * 512:(qs + 1) * 512], og, extra_wr=[r_og[b]])
                ssq_update(0, qs, sq_t)

        nsw = len(swa_units)
        for i in range(nsw + 2):
            if i < nsw:
                w1(i)
            if 0 <= i - 1 < nsw:
                w2(i - 1)
            if 0 <= i - 2 < nsw:
                w3(i - 2)

        P.barrier()
        load("sp", sbm, sbmask_d)
        for r in range(2):
            for rr in range(2):
                P.op("pool", lambda e, t=Vp[r][rr]: e.memset(t.ap, 0.0), wr=R(Vp[r][rr]))
        pair_ap = lambda g: psum_t[:, (2 * g) * 512:(2 * g + 2) * 512]
        for j in range(8):
            w = load_w(CB_Q + j * 128)
            q = qT[nxt("qT", 2)]
            proj_fm(w, q)
            w = load_w(CB_K + j * 128)
            kk_ = kT[nxt("kT", 2)]
            proj_fm(w, kk_)
            w = load_w(CB_V + j * 128)
            vi = nxt("Vp", 2)
            V0 = Vp[vi][0]; V1 = Vp[vi][1]

            def evac_vb(pbk, t4, V0=V0, V1=V1):
                v4 = pbk.ap.rearrange("p (i n) -> p i n", i=4)
                P.op("act", lambda e: e.activation(out=V0.ap[:, t4 * 4:t4 * 4 + 4, 0:64], in_=v4[:, :, 0:64], func=AF.Copy), rd=R(pbk), wr=R(V0))
                P.op("dve", lambda e: e.tensor_copy(out=V1.ap[:, t4 * 4:t4 * 4 + 4, 64:128], in_=v4[:, :, 64:128]), rd=R(pbk), wr=R(V1))
            proj_tm(w, evac_vb)
            units = [(qs, kb) for qs in range(NQ) for kb in range(4 * qs + 3, -1, -1)]
            nun = len(units)
            psos = {}

            def s1a(u, q=q, kk_=kk_):
                qs, kb = units[u]
                g = u % 2
                et = e_t[u % 5]
                for r in range(2):
                    rows = slice(r * 64, r * 64 + 64)
                    pss = PB[2 * g + r]
                    P.op("pe", lambda e, r=r: e.matmul(out=pss.ap, lhsT=kk_.ap[rows, kb * 128:(kb + 1) * 128], rhs=q.ap[rows, qs * 512:(qs + 1) * 512], start=True, stop=True),
                         rd=R(kk_, q), wr=R(pss))
                P.op("act", lambda e: e.activation(out=et.ap, in_=pair_ap(g), func=AF.Exp, scale=0.125), rd=R(PB[2 * g], PB[2 * g + 1]), wr=R(et))
                if kb >= 4 * qs:
                    jj = kb - 4 * qs
                    mk = bass.AP(sbm.ap.tensor, sbm.ap[:, jj, :].offset, [[sbm.ap.ap[0][0], 128], [0, 2], [1, 512]])
                    ev = et.ap.rearrange("p (r n) -> p r n", r=2)
                    P.op("pool", lambda e: e.tensor_tensor(out=ev, in0=ev, in1=mk, op=ALU.mult), rd=R(et, sbm), wr=R(et))

            def s1b(u):
                et = e_t[u % 5]; spt = sp_t[u % 4]
                P.op("act", lambda e: e.activation(out=spt.ap, in_=et.ap, func=AF.Ln, bias=1.0, scale=1.0), rd=R(et), wr=R(spt))

            def s2a(u):
                qs, kb = units[u]
                spt = sp_t[u % 4]; ect = ec_t[u % 3]
                st = (kb == 4 * qs + 3)
                for r in range(2):
                    psc = PB[4 + r]
                    P.op("pe", lambda e, r=r: e.matmul(out=psc.ap, lhsT=negtri.ap, rhs=spt.ap[:, r * 512:(r + 1) * 512], start=st, stop=True, skip_group_check=True),
                         rd=R(negtri, spt), wr=R(psc))
                P.op("act", lambda e: e.activation(out=ect.ap, in_=pair_ap(2), func=AF.Exp), rd=R(PB[4], PB[5]), wr=R(ect))

            def s2b1(u):
                qs, kb = units[u]
                spt = sp_t[u % 4]
                if kb > 0:
                    for r in range(2):
                        psc = PB[4 + r]
                        P.op("pe", lambda e, r=r: e.matmul(out=psc.ap, lhsT=negsu.ap, rhs=spt.ap[:, r * 512:(r + 1) * 512], start=False, stop=True, skip_group_check=True),
                             rd=R(negsu, spt), wr=R(psc))

            def s2b(u, j=j, V0=V0, V1=V1):
                qs, kb = units[u]
                spt = sp_t[u % 4]; ect = ec_t[u % 3]; et = e_t[u % 5]; at = a_t[u % 3]
                P.op("dve", lambda e: e.tensor_tensor(out=at.ap, in0=et.ap, in1=ect.ap, op=ALU.mult), rd=R(et, ect), wr=R(at))
                st = qs not in psos
                if st:
                    psos[qs] = PB[6 + nxt("pso", 2)]
                pso = psos[qs]
                for r in range(2):
                    Vt = V0 if r == 0 else V1
                    P.op("pe", lambda e, r=r, Vt=Vt, st=(st and r == 0): e.matmul(out=pso.ap, lhsT=Vt.ap[:, kb, :], rhs=at.ap[:, r * 512:(r + 1) * 512], start=st, stop=True, skip_group_check=True),
                         rd=R(Vt, at), wr=R(pso))
                if kb == 0:
                    og = og_t[nxt("og", 2)]
                    P.op("act", lambda e: e.activation(out=og.ap, in_=pso.ap, func=AF.Copy, scale=nb_fm.ap[:, j:j + 1]), rd=R(pso, nb_fm), wr=R(og))
                    P.op("act", lambda e: e.activation(out=sq_t.ap, in_=pso.ap, func=AF.Square), rd=R(pso), wr=R(sq_t))
                    store("sp", ogT_d[b, (8 + j) * 128:(9 + j) * 128, qs * 512:(qs + 1) * 512], og, extra_wr=[r_og[b]])
                    ssq_update(1, qs, sq_t)

            for i in range(nun + 3):
                if i < nun:
                    s1a(i)
                if 0 <= i - 3 < nun:
                    s2b1(i - 3)
                if 0 <= i - 2 < nun:
                    s2a(i - 2)
                if 0 <= i - 1 < nun:
                    s1b(i - 1)
                if 0 <= i - 3 < nun:
                    s2b(i - 3)
        if dbg:
            P.barrier()
            store("sp", dbg_d["ssq"][b], ssq, src_ap=ssq.ap.rearrange("p g t -> p (g t)"))
        P.barrier()

        G1 = mixn
        load("sp", G1, bcast_row(mod_d[b:b + 1, 2 * D:3 * D]), extra_rd=[r_mod])
        P.op("dve", lambda e: e.tensor_scalar_add(out=G1.ap, in0=G1.ap, scalar1=1.0), rd=R(G1), wr=R(G1))
        for k in range(16):
            stw = nrm2[k % 2]
            load("sp", stw, w_out_v[:, k, :])
            P.op("dve", lambda e, k=k, stw=stw: e.tensor_tensor(out=wo.ap[:, k, :], in0=stw.ap, in1=G1.ap, op=ALU.mult), rd=R(stw, G1), wr=R(wo))
        load("sp", BB, bcast_row(mod_d[b:b + 1, 3 * D:4 * D]), extra_rd=[r_mod])
        load("sp", GG, bcast_row(mod_d[b:b + 1, 4 * D:5 * D]), extra_rd=[r_mod])
        load("sp", LG, bcast_row(ln1g_d)); load("sp", LB, bcast_row(ln1b_d))
        P.op("dve", lambda e: e.tensor_scalar_add(out=GG.ap, in0=GG.ap, scalar1=1.0), rd=R(GG), wr=R(GG))
        tmpx = x1t2[0]
        P.op("dve", lambda e: e.tensor_tensor(out=tmpx.ap, in0=LB.ap, in1=GG.ap, op=ALU.mult), rd=R(LB, GG), wr=R(tmpx))
        P.op("dve", lambda e: e.tensor_tensor(out=BB.ap, in0=BB.ap, in1=tmpx.ap, op=ALU.add), rd=R(BB, tmpx), wr=R(BB))
        P.op("dve", lambda e: e.tensor_tensor(out=GG.ap, in0=GG.ap, in1=LG.ap, op=ALU.mult), rd=R(GG, LG), wr=R(GG))
        P.dma("sp", lambda e: e.dma_start(out=wr_t.ap[:, :, 0:4], in_=wgrp_d.rearrange("(k p) n -> p k n", p=128)), wr_t.r, wr=[wr_t.r])
        P.dma("sp", lambda e: e.dma_start(out=wr_t.ap[:, :, 4:36], in_=wrtr_d.rearrange("(k p) n -> p k n", p=128)), wr_t.r, wr=[wr_t.r])
        P.dma("sp", lambda e: e.dma_start(out=brt.ap[0:1, 0:4], in_=bgrp_d), brt.r, wr=[brt.r])
        P.dma("sp", lambda e: e.dma_start(out=brt.ap[0:1, 4:36], in_=brtr_d), brt.r, wr=[brt.r])
        P.op("act", lambda e: e.activation(out=rstd.ap, in_=ssq.ap, func=AF.Ln, scale=1.0 / 1024.0, bias=EPS), rd=R(ssq), wr=R(rstd))
        P.op("act", lambda e: e.activation(out=rstd.ap, in_=rstd.ap, func=AF.Exp, scale=-0.5), rd=R(rstd), wr=R(rstd))

        ogT_v = ogT_d[b].rearrange("(k p) s -> p k s", p=128)

        def XL(t):
            og = ogt[t % 2]; xx = xtl[t % 2]; gt = b * NT + t
            P.dma("sp", lambda e: e.dma_start(out=og.ap, in_=ogT_v[:, :, t * 128:(t + 1) * 128]), og.r, rd=[r_og[b]], wr=[og.r])
            load("sp", xx, x_d[gt * 128:(gt + 1) * 128, :])
            P.op("act", lambda e: e.activation(out=mixn.ap, in_=xx.ap, func=AF.Copy, scale=ALPHA), rd=R(xx), wr=R(mixn))

        def XN(t, nb):
            og = ogt[t % 2]
            pa = PB[2 + nxt("pa3", 3)]; pbb = PB[5 + nxt("pb3", 3)]
            for k in range(8):
                P.op("pe", lambda e, k=k: e.matmul(out=pa.ap, lhsT=og.ap[:, k, :], rhs=wo.ap[:, k, nb * 512:(nb + 1) * 512], start=(k == 0), stop=(k == 7)),
                     rd=R(og, wo), wr=R(pa))
            for k in range(8, 16):
                P.op("pe", lambda e, k=k: e.matmul(out=pbb.ap, lhsT=og.ap[:, k, :], rhs=wo.ap[:, k, nb * 512:(nb + 1) * 512], start=(k == 8), stop=(k == 15)),
                     rd=R(og, wo), wr=R(pbb))
            mv_ = mixn.ap[:, nb * 512:(nb + 1) * 512]
            P.op("dve", lambda e: e.scalar_tensor_tensor(out=mv_, in0=pa.ap, scalar=rstd.ap[:, 0, t:t + 1], in1=mv_, op0=ALU.mult, op1=ALU.add), rd=R(pa, rstd, mixn), wr=R(mixn))
            P.op("dve", lambda e: e.scalar_tensor_tensor(out=mv_, in0=pbb.ap, scalar=rstd.ap[:, 1, t:t + 1], in1=mv_, op0=ALU.mult, op1=ALU.add), rd=R(pbb, rstd, mixn), wr=R(mixn))

        def XC(t):
            layer_norm(P, mixn, nrm2[t % 2], st6, mv, sm)

        def YA1(t):
            gt = b * NT + t
            nrm = nrm2[t % 2]; x1t = x1t2[t % 2]
            P.op("dve", lambda e: e.tensor_tensor(out=x1t.ap, in0=nrm.ap, in1=LG.ap, op=ALU.mult), rd=R(nrm, LG), wr=R(x1t))
            P.op("dve", lambda e: e.tensor_tensor(out=x1t.ap, in0=x1t.ap, in1=LB.ap, op=ALU.add), rd=R(x1t, LB), wr=R(x1t))
            store("sp", x1_d[gt * 128:(gt + 1) * 128, :], x1t, extra_wr=[r_x1[gt]])

        def YA2(t):
            nrm = nrm2[t % 2]; hb = h2b[t % 3]; h2 = h2_2[t % 2]
            P.op("dve", lambda e: e.tensor_tensor(out=h2.ap, in0=nrm.ap, in1=GG.ap, op=ALU.mult), rd=R(nrm, GG), wr=R(h2))
            P.op("dve", lambda e: e.tensor_tensor(out=h2.ap, in0=h2.ap, in1=BB.ap, op=ALU.add), rd=R(h2, BB), wr=R(h2))
            P.op("act", lambda e: e.activation(out=hb.ap, in_=h2.ap, func=AF.Copy), rd=R(h2), wr=R(hb))

        def YB(t):
            h2 = h2_2[t % 2]
            for k4 in range(4):
                pbk = PB[nxt("pj", 2)]
                for kk in range(4):
                    k = k4 * 4 + kk
                    P.op("pe", lambda e, kk=kk, k=k: e.transpose(out=pbk.ap[:, kk * 128:(kk + 1) * 128], in_=h2.ap[:, k * 128:(k + 1) * 128], identity=ident.ap),
                         rd=R(h2, ident), wr=R(pbk))
                if k4 % 2 == 0:
                    P.op("act", lambda e: e.activation(out=h2T.ap[:, k4 * 4:k4 * 4 + 4, :], in_=pbk.ap.rearrange("p (k s) -> p k s", k=4), func=AF.Copy), rd=R(pbk), wr=R(h2T))
                else:
                    P.op("dve", lambda e: e.tensor_copy(out=h2T.ap[:, k4 * 4:k4 * 4 + 4, :], in_=pbk.ap.rearrange("p (k s) -> p k s", k=4)), rd=R(pbk), wr=R(h2T))
            pr = PB[nxt("pj", 2)]
            for k in range(16):
                P.op("pe", lambda e, k=k: e.matmul(out=pr.ap[:, 0:36], lhsT=h2T.ap[:, k, :], rhs=wr_t.ap[:, k, :], start=(k == 0), stop=False), rd=R(h2T, wr_t), wr=R(pr))
            P.op("pe", lambda e: e.matmul(out=pr.ap[:, 0:36], lhsT=ones_f.ap[0:1, :], rhs=brt.ap[0:1, :], start=False, stop=True), rd=R(ones_f, brt), wr=R(pr))
            lg = lg2[t % 2]
            P.op("dve", lambda e: e.tensor_copy(out=lg.ap[:, 0:36], in_=pr.ap[:, 0:36]), rd=R(pr), wr=R(lg))

        def ZA(t):
            route_a(P, lg2[t % 2], smr, gatew, b * NT + t)

        def ZB(t):
            gt = b * NT + t; hb = h2b[t % 3]
            route_b(P, smr, cnt_keep, eoff, ohbf, lowtri, ones_b, PB, nxt, slot_i, gt)
            for k in range(2):
                P.dma("pool", lambda e, k=k: e.indirect_dma_start(out=xs_d, out_offset=bass.IndirectOffsetOnAxis(ap=slot_i.ap[:, gt, k:k + 1], axis=0),
                                                                 in_=hb.ap, in_offset=None),
                      hb.r, rd=[hb.r, slot_i.r], wr=[r_xs])

        for stp in range(NT + 3):
            tx, ta, tb_, tz = stp, stp - 1, stp - 2, stp - 3
            okx = tx < NT; oka = 0 <= ta < NT; okb = 0 <= tb_ < NT; okz = 0 <= tz < NT
            if okb:
                YB(tb_)
            if okx:
                XL(tx)
            if okz:
                ZA(tz)
            if okx:
                XN(tx, 0)
            if okz:
                ZB(tz)
            if okx:
                XN(tx, 1)
            if oka:
                YA1(ta)
            if okx:
                XN(tx, 2)
            if oka:
                YA2(ta)
            if okx:
                XN(tx, 3); XC(tx)
        P.barrier()
    if dbg:
        A.off = base_off
        tmpb = A.bf(S)
        for b in range(NSEQ):
            for k in range(16):
                P.dma("sp", lambda e, b=b, k=k: e.dma_start(out=tmpb.ap, in_=ogT_d[b, k * 128:(k + 1) * 128, :]), tmpb.r, rd=[r_og[b]], wr=[tmpb.r])
                store("sp", dbg_d["ogT"][b, k * 128:(k + 1) * 128, :], tmpb)
        tmpf = A.f32(D)
        for gt in range(NTT):
            P.dma("sp", lambda e, gt=gt: e.dma_start(out=tmpf.ap, in_=x1_d[gt * 128:(gt + 1) * 128, :]), tmpf.r, rd=[r_x1[gt]], wr=[tmpf.r])
            store("sp", dbg_d["x1"][gt * 128:(gt + 1) * 128, :], tmpf)
        store("sp", dbg_d["slot"], slot_i, src_ap=slot_i.ap.rearrange("p t k -> p (t k)"))
        store("sp", dbg_d["gw"], gatew, src_ap=gatew.ap.rearrange("p t k -> p (t k)"))
        P.barrier()

    A.off = base_off
    wslot = [A.bf(16 * 512) for _ in range(6)]
    stg = [A.f32(2048) for _ in range(4)]
    xsb = [A.bf(D) for _ in range(4)]
    xsT = [A.bf(16 * CAP, "p (k s) -> p k s", k=16) for _ in range(2)]
    actT = [A.bf(4 * CAP, "p (f s) -> p f s", f=4) for _ in range(2)]
    sgt = [A.f32(CAP) for _ in range(2)]
    yt = [A.f32(D) for _ in range(2)]
    identb = A.bf(128)
    P.op("dve", lambda e: e.tensor_copy(out=identb.ap, in_=ident.ap), rd=R(ident), wr=R(identb))

    def cast_task(dst, c, src, pat, kw):
        def run():
            st = stg[nxt("stg", 4)]
            sv = st.ap.rearrange(pat, **kw) if pat else st.ap
            P.dma("sp", lambda e: e.dma_start(out=sv, in_=src), st.r, wr=[st.r])
            dv = dst.ap[:, c * 2048:(c + 1) * 2048]
            if nxt("casteng", 2) == 0:
                P.op("act", lambda e: e.activation(out=dv, in_=st.ap, func=AF.Copy), rd=R(st), wr=R(dst))
            else:
                P.op("dve", lambda e: e.tensor_copy(out=dv, in_=st.ap), rd=R(st), wr=R(dst))
        return run

    def expert_loads(ex):
        wg = wslot[nxt("ws", 6)]; wu = wslot[nxt("ws", 6)]; wdn = wslot[nxt("ws", 6)]
        gv = wg_d[ex].rearrange("(p k) f -> p k f", k=16); uv = wu_d[ex].rearrange("(p k) f -> p k f", k=16)
        dvw = wd_d[ex].rearrange("(k p) f -> p k f", p=128)
        tasks = []
        xbs = []
        for cb in range(CAPB):
            xb = xsb[nxt("xsb", 4)]
            xbs.append(xb)
            row0 = ex * CAP + cb * 128

            def xload(xb=xb, row0=row0):
                P.dma("sp", lambda e: e.dma_start(out=xb.ap, in_=xs_d[row0:row0 + 128, :]), xb.r, rd=[r_xs], wr=[xb.r])
            tasks.append(xload)
        for c in range(4):
            tasks.append(cast_task(wg, c, gv[:, 4 * c:4 * c + 4, :], "p (k f) -> p k f", dict(k=4)))
            tasks.append(cast_task(wu, c, uv[:, 4 * c:4 * c + 4, :], "p (k f) -> p k f", dict(k=4)))
        for c in range(4):
            tasks.append(cast_task(wdn, c, dvw[:, c, :], None, {}))
        return (wg, wu, wdn), xbs, tasks

    cur_w, cur_x, tasks0 = expert_loads(0)
    for tsk in tasks0:
        tsk()
    for ex in range(NEXP):
        wg, wu, wdn = cur_w
        xbs = cur_x
        if ex + 1 < NEXP:
            cur_w, cur_x, bg = expert_loads(ex + 1)
        else:
            bg = []

        def pump(n=1):
            for _ in range(n):
                if bg:
                    bg.pop(0)()

        wgv = wg.ap.rearrange("p (k f) -> p k f", k=16); wuv = wu.ap.rearrange("p (k f) -> p k f", k=16); wdv = wdn.ap.rearrange("p (k f) -> p k f", k=4)
        xT = xsT[ex % 2]; aT = actT[ex % 2]
        for cb in range(CAPB):
            xb = xbs[cb]
            for k8 in range(2):
                pbk = PB[nxt("pj", 2)]
                pv = pbk.ap.bitcast(BF16)
                for kk in range(8):
                    k = k8 * 8 + kk
                    P.op("pe", lambda e, pv=pv, kk=kk, k=k, xb=xb: e.transpose(out=pv[:, kk * 128:(kk + 1) * 128], in_=xb.ap.rearrange("s (p k) -> s k p", k=16)[:, k, :], identity=identb.ap),
                         rd=R(xb, identb), wr=R(pbk))
                dstv = xT.ap[:, k8 * 8:k8 * 8 + 8, cb * 128:(cb + 1) * 128]
                if k8 == 0:
                    P.op("act", lambda e, pv=pv, dstv=dstv: e.activation(out=dstv, in_=pv.rearrange("p (k s) -> p k s", k=8), func=AF.Copy), rd=R(pbk), wr=R(xT))
                else:
                    P.op("dve", lambda e, pv=pv, dstv=dstv: e.tensor_copy(out=dstv, in_=pv.rearrange("p (k s) -> p k s", k=8)), rd=R(pbk), wr=R(xT))
            pump(1)
        for fc in range(4):
            pg = PB[2 + nxt("pss", 2)]; pu = PB[4 + nxt("psd", 2)]
            for k in range(16):
                P.op("pe", lambda e, k=k, fc=fc, pg=pg: e.matmul(out=pg.ap[:, 0:CAP], lhsT=wgv[:, k, fc * 128:(fc + 1) * 128], rhs=xT.ap[:, k, :], start=(k == 0), stop=(k == 15)),
                     rd=R(wg, xT), wr=R(pg))
            pump(1)
            for k in range(16):
                P.op("pe", lambda e, k=k, fc=fc, pu=pu: e.matmul(out=pu.ap[:, 0:CAP], lhsT=wuv[:, k, fc * 128:(fc + 1) * 128], rhs=xT.ap[:, k, :], start=(k == 0), stop=(k == 15)),
                     rd=R(wu, xT), wr=R(pu))
            sgx = sgt[fc % 2]
            P.op("act", lambda e, pg=pg, sgx=sgx: e.activation(out=sgx.ap, in_=pg.ap[:, 0:CAP], func=AF.Silu), rd=R(pg), wr=R(sgx))
            P.op("dve", lambda e, pu=pu, sgx=sgx, fc=fc: e.tensor_tensor(out=aT.ap[:, fc, :], in0=pu.ap[:, 0:CAP], in1=sgx.ap, op=ALU.mult), rd=R(pu, sgx), wr=R(aT))
            pump(1)
        for cb in range(CAPB):
            y = yt[nxt("yt", 2)]
            for nb in range(4):
                py = PB[6 + nxt("pso", 2)]
                for fc in range(4):
                    P.op("pe", lambda e, fc=fc, nb=nb, cb=cb, py=py: e.matmul(out=py.ap, lhsT=aT.ap[:, fc, cb * 128:(cb + 1) * 128], rhs=wdv[:, fc, nb * 512:(nb + 1) * 512], start=(fc == 0), stop=(fc == 3)),
                         rd=R(aT, wdn), wr=R(py))
                if nb % 2 == 0:
                    P.op("act", lambda e, nb=nb, py=py, y=y: e.activation(out=y.ap[:, nb * 512:(nb + 1) * 512], in_=py.ap, func=AF.Copy), rd=R(py), wr=R(y))
                else:
                    P.op("dve", lambda e, nb=nb, py=py, y=y: e.tensor_copy(out=y.ap[:, nb * 512:(nb + 1) * 512], in_=py.ap), rd=R(py), wr=R(y))
                if nb % 2 == 1:
                    pump(1)
            row0 = ex * CAP + cb * 128
            store("act", ys_d[row0:row0 + 128, :], y, extra_wr=[r_ys[ex]])
        pump(len(bg))
    P.barrier()

    A.off = base_off
    G2 = [A.f32(D) for _ in range(NSEQ)]
    L2G = A.f32(D); L2B = A.f32(D)
    y1 = [A.f32(D) for _ in range(2)]; y2 = [A.f32(D) for _ in range(2)]; xx1 = [A.f32(D) for _ in range(2)]
    f2 = [A.f32(D) for _ in range(2)]; nrmc = [A.f32(D) for _ in range(2)]; o_out = [A.f32(D) for _ in range(2)]
    st6 = A.f32(24); mv = A.f32(2); sm = [A.f32(40) for _ in range(12)]
    for b in range(NSEQ):
        load("sp", G2[b], bcast_row(mod_d[b:b + 1, 5 * D:6 * D]), extra_rd=[r_mod])
        P.op("pool", lambda e, b=b: e.tensor_scalar_add(out=G2[b].ap, in0=G2[b].ap, scalar1=1.0), rd=R(G2[b]), wr=R(G2[b]))
    load("sp", L2G, bcast_row(ln2g_d)); load("sp", L2B, bcast_row(ln2b_d))

    def CL(gt):
        ya = y1[gt % 2]; yb = y2[gt % 2]; xa = xx1[gt % 2]
        P.dma("pool", lambda e: e.indirect_dma_start(out=ya.ap, out_offset=None, in_=ys_d, in_offset=bass.IndirectOffsetOnAxis(ap=slot_i.ap[:, gt, 0:1], axis=0)),
              ya.r, rd=R(slot_i) + r_ys, wr=[ya.r])
        P.dma("pool", lambda e: e.indirect_dma_start(out=yb.ap, out_offset=None, in_=ys_d, in_offset=bass.IndirectOffsetOnAxis(ap=slot_i.ap[:, gt, 1:2], axis=0)),
              yb.r, rd=R(slot_i) + r_ys, wr=[yb.r])
        P.dma("sp", lambda e: e.dma_start(out=xa.ap, in_=x1_d[gt * 128:(gt + 1) * 128, :]), xa.r, rd=[r_x1[gt]], wr=[xa.r])

    def C1a(gt):
        ya = y1[gt % 2]; yb = y2[gt % 2]; f_t = f2[gt % 2]; b = gt // NT
        P.op("act", lambda e: e.activation(out=f_t.ap, in_=ya.ap, func=AF.Copy, scale=gatew.ap[:, gt, 0:1]), rd=R(ya, gatew), wr=R(f_t))
        P.op("dve", lambda e: e.scalar_tensor_tensor(out=f_t.ap, in0=yb.ap, scalar=gatew.ap[:, gt, 1:2], in1=f_t.ap, op0=ALU.mult, op1=ALU.add), rd=R(yb, gatew, f_t), wr=R(f_t))
        P.op("dve", lambda e: e.tensor_tensor(out=f_t.ap, in0=f_t.ap, in1=G2[b].ap, op=ALU.mult), rd=R(f_t, G2[b]), wr=R(f_t))

    def C1b(gt):
        xa = xx1[gt % 2]; f_t = f2[gt % 2]
        P.op("dve", lambda e: e.scalar_tensor_tensor(out=f_t.ap, in0=xa.ap, scalar=ALPHA, in1=f_t.ap, op0=ALU.mult, op1=ALU.add), rd=R(xa, f_t), wr=R(f_t))

    def C2a(gt):
        layer_norm(P, f2[gt % 2], nrmc[gt % 2], st6, mv, sm)

    def C2b(gt):
        oo = o_out[gt % 2]; nrm = nrmc[gt % 2]
        P.op("dve", lambda e: e.tensor_tensor(out=oo.ap, in0=nrm.ap, in1=L2G.ap, op=ALU.mult), rd=R(nrm, L2G), wr=R(oo))
        P.op("dve", lambda e: e.tensor_tensor(out=oo.ap, in0=oo.ap, in1=L2B.ap, op=ALU.add), rd=R(oo, L2B), wr=R(oo))
        store("sp", out_d[gt * 128:(gt + 1) * 128, :], oo)

    CL(0)
    for stp in range(NTT + 2):
        if stp + 1 < NTT:
            CL(stp + 1)
        if stp < NTT:
            C1a(stp)
        if 0 <= stp - 1 < NTT:
            C2a(stp - 1)
        if stp < NTT:
            C1b(stp)
        if 0 <= stp - 2 < NTT:
            C2b(stp - 2)
    P.final_wait("sp")
    stats = P.emit()
    return nc, stats


def layer_norm(P, y, nrm, st6, mv, sm):
    R = lambda *ts: [t.r for t in ts]
    for i in range(4):
        P.op("dve", lambda e, i=i: e.bn_stats(out=st6.ap[:, i * 6:(i + 1) * 6], in_=y.ap[:, i * 512:(i + 1) * 512]), rd=R(y), wr=R(st6))
    P.op("dve", lambda e: e.bn_aggr(out=mv.ap, in_=st6.ap), rd=R(st6), wr=R(mv))
    rs = sm[10]; nm = sm[11]
    P.op("act", lambda e: e.activation(out=rs.ap[:, 0:1], in_=mv.ap[:, 1:2], func=AF.Ln, bias=EPS, scale=1.0), rd=R(mv), wr=R(rs))
    P.op("act", lambda e: e.activation(out=rs.ap[:, 0:1], in_=rs.ap[:, 0:1], func=AF.Exp, scale=-0.5), rd=R(rs), wr=R(rs))
    P.op("dve", lambda e: e.scalar_tensor_tensor(out=nm.ap[:, 0:1], in0=mv.ap[:, 0:1], scalar=-1.0, in1=rs.ap[:, 0:1], op0=ALU.mult, op1=ALU.mult), rd=R(mv, rs), wr=R(nm))
    P.op("act", lambda e: e.activation(out=nrm.ap, in_=y.ap, func=AF.Identity, scale=rs.ap[:, 0:1], bias=nm.ap[:, 0:1]), rd=R(y, rs, nm), wr=R(nrm))


def route_a(P, lgt, sm, gatew, gt):
    R = lambda *ts: [t.r for t in ts]
    lg = lgt
    _, gmx, goh, gpen, elm, m1, oh1, elm2, m2, oh2 = sm[0:10]
    tmp = sm[10]
    dv = lambda op, rd, wr: P.op("dve", op, rd=R(*rd), wr=R(*wr))
    dv(lambda e: e.reduce_max(out=gmx.ap[:, 0:1], in_=lg.ap[:, 0:4], axis=AX.X), [lg], [gmx])
    dv(lambda e: e.tensor_scalar(out=goh.ap[:, 0:4], in0=lg.ap[:, 0:4], scalar1=gmx.ap[:, 0:1], scalar2=None, op0=ALU.is_equal), [lg, gmx], [goh])
    dv(lambda e: e.tensor_scalar(out=gmx.ap[:, 1:2], in0=gmx.ap[:, 0:1], scalar1=-1.0, scalar2=None, op0=ALU.mult), [gmx], [gmx])
    P.op("act", lambda e: e.activation(out=tmp.ap[:, 0:4], in_=lg.ap[:, 0:4], func=AF.Exp, bias=gmx.ap[:, 1:2], scale=1.0, accum_out=tmp.ap[:, 4:5]), rd=R(lg, gmx), wr=R(tmp))
    dv(lambda e: e.tensor_scalar(out=gpen.ap[:, 0:4], in0=goh.ap[:, 0:4], scalar1=-1.0, scalar2=BIG, op0=ALU.add, op1=ALU.mult), [goh], [gpen])
    gp_b = bass.AP(gpen.ap.tensor, gpen.ap.offset, [[gpen.ap.ap[0][0], 128], [1, 4], [0, 8]])
    dv(lambda e: e.tensor_tensor(out=elm.ap[:, 0:32].rearrange("p (g j) -> p g j", g=4), in0=lg.ap[:, 4:36].rearrange("p (g j) -> p g j", g=4), in1=gp_b, op=ALU.add), [lg, gpen], [elm])
    dv(lambda e: e.reduce_max(out=m1.ap[:, 0:1], in_=elm.ap[:, 0:32], axis=AX.X), [elm], [m1])
    dv(lambda e: e.tensor_scalar(out=oh1.ap[:, 0:32], in0=elm.ap[:, 0:32], scalar1=m1.ap[:, 0:1], scalar2=None, op0=ALU.is_equal), [elm, m1], [oh1])
    dv(lambda e: e.scalar_tensor_tensor(out=elm2.ap[:, 0:32], in0=oh1.ap[:, 0:32], scalar=-BIG, in1=elm.ap[:, 0:32], op0=ALU.mult, op1=ALU.add), [oh1, elm], [elm2])
    dv(lambda e: e.reduce_max(out=m2.ap[:, 0:1], in_=elm2.ap[:, 0:32], axis=AX.X), [elm2], [m2])
    dv(lambda e: e.tensor_scalar(out=oh2.ap[:, 0:32], in0=elm2.ap[:, 0:32], scalar1=m2.ap[:, 0:1], scalar2=None, op0=ALU.is_equal), [elm2, m2], [oh2])
    dv(lambda e: e.tensor_tensor(out=m2.ap[:, 1:2], in0=m2.ap[:, 0:1], in1=m1.ap[:, 0:1], op=ALU.subtract), [m2, m1], [m2])
    P.op("act", lambda e: e.activation(out=m2.ap[:, 2:3], in_=m2.ap[:, 1:2], func=AF.Exp), rd=R(m2), wr=R(m2))
    dv(lambda e: e.reciprocal(out=tmp.ap[:, 5:6], in_=tmp.ap[:, 4:5]), [tmp], [tmp])
    dv(lambda e: e.tensor_scalar_add(out=m2.ap[:, 3:4], in0=m2.ap[:, 2:3], scalar1=1.0), [m2], [m2])
    dv(lambda e: e.reciprocal(out=m2.ap[:, 4:5], in_=m2.ap[:, 3:4]), [m2], [m2])
    dv(lambda e: e.tensor_tensor(out=gatew.ap[:, gt, 0:1], in0=m2.ap[:, 4:5], in1=tmp.ap[:, 5:6], op=ALU.mult), [m2, tmp], [gatew])
    dv(lambda e: e.tensor_tensor(out=gatew.ap[:, gt, 1:2], in0=gatew.ap[:, gt, 0:1], in1=m2.ap[:, 2:3], op=ALU.mult), [gatew, m2], [gatew])


def route_b(P, sm, cnt, eoff, ohbf, lowtri, ones_b, PB, nxt, slot_i, gt):
    R = lambda *ts: [t.r for t in ts]
    m1 = sm[5]; oh1 = sm[6]; oh2 = sm[9]; tmp2 = sm[11]
    dv = lambda op, rd, wr: P.op("dve", op, rd=R(*rd), wr=R(*wr))
    dv(lambda e: e.tensor_tensor(out=ohbf.ap[:, 0:32], in0=oh1.ap[:, 0:32], in1=oh2.ap[:, 0:32], op=ALU.add), [oh1, oh2], [ohbf])
    pp = PB[nxt("pj", 2)]
    P.op("pe", lambda e: e.matmul(out=pp.ap[:, 0:32], lhsT=lowtri.ap, rhs=ohbf.ap[:, 0:32], start=True, stop=True), rd=R(lowtri, ohbf), wr=R(pp))
    P.op("pe", lambda e: e.matmul(out=pp.ap[:, 32:64], lhsT=ones_b.ap, rhs=ohbf.ap[:, 0:32], start=True, stop=True, skip_group_check=True), rd=R(ones_b, ohbf), wr=R(pp))
    dv(lambda e: e.tensor_tensor(out=tmp2.ap[:, 0:32], in0=cnt.ap, in1=eoff.ap, op=ALU.add), [cnt, eoff], [tmp2])
    dv(lambda e: e.tensor_tensor(out=tmp2.ap[:, 0:32], in0=pp.ap[:, 0:32], in1=tmp2.ap[:, 0:32], op=ALU.add), [pp, tmp2], [tmp2])
    dv(lambda e: e.tensor_tensor(out=cnt.ap, in0=pp.ap[:, 32:64], in1=cnt.ap, op=ALU.add), [pp, cnt], [cnt])
    dv(lambda e: e.tensor_tensor(out=oh1.ap[:, 0:32], in0=oh1.ap[:, 0:32], in1=tmp2.ap[:, 0:32], op=ALU.mult), [oh1, tmp2], [oh1])
    dv(lambda e: e.tensor_tensor(out=oh2.ap[:, 0:32], in0=oh2.ap[:, 0:32], in1=tmp2.ap[:, 0:32], op=ALU.mult), [oh2, tmp2], [oh2])
    dv(lambda e: e.reduce_sum(out=m1.ap[:, 1:2], in_=oh1.ap[:, 0:32], axis=AX.X), [oh1], [m1])
    dv(lambda e: e.reduce_sum(out=m1.ap[:, 2:3], in_=oh2.ap[:, 0:32], axis=AX.X), [oh2], [m1])
    dv(lambda e: e.tensor_copy(out=slot_i.ap[:, gt, 0:2], in_=m1.ap[:, 1:3]), [m1], [slot_i])


_NC_CACHE = {}


def make_in_map(inputs, core, ncores, S, NSEQ, CAPB):
    f = lambda a: np.ascontiguousarray(np.asarray(a, dtype=np.float32))
    x = np.asarray(inputs["x"]); c = np.asarray(inputs["c"])
    b0 = core * NSEQ
    m = {}
    m["x"] = f(x[b0:b0 + NSEQ, :S]).reshape(NSEQ * S, D)
    m["c"] = f(c[b0:b0 + NSEQ])
    m["w_in"] = f(inputs["w_in"][0]); m["w_out"] = f(inputs["w_out"][0])
    sinks = np.asarray(inputs["sinks"][0], np.float32)
    m["sinks_fm"] = f(np.stack([sinks[2 * j + (np.arange(128) // 64)] for j in range(8)], axis=1))
    m["rel_bias"] = f(inputs["rel_bias"])
    m["na_fm"] = f(np.asarray(inputs["norm_a"][0]).reshape(8, 128).T)
    m["nb_fm"] = f(np.asarray(inputs["norm_b"][0]).reshape(8, 128).T)
    m["w_ada"] = f(inputs["w_ada"][0]); m["b_ada"] = f(inputs["b_ada"][0]).reshape(1, 6 * D)
    for k in ("ln1_g", "ln1_b", "ln2_g", "ln2_b"):
        m[k] = f(inputs[k][0]).reshape(1, D)
    m["w_grp"] = f(inputs["w_grp"][0]); m["b_grp"] = f(inputs["b_grp"][0]).reshape(1, 4)
    m["w_rtr"] = f(inputs["w_rtr"][0]); m["b_rtr"] = f(inputs["b_rtr"][0]).reshape(1, 32)
    m["w_gate"] = f(inputs["w_gate"][0]); m["w_up"] = f(inputs["w_up"][0]); m["w_down"] = f(inputs["w_down"][0])
    m.update(host_consts(CAPB * 128))
    return m


def kernel(**inputs):
    NCORES = 8
    S, NSEQ, CAPB = 2048, 2, 3
    key = (S, NSEQ, CAPB)
    if key not in _NC_CACHE:
        _NC_CACHE[key] = build_nc(S, NSEQ, CAPB)[0]
    nc = _NC_CACHE[key]
    shared = make_in_map(inputs, 0, NCORES, S, NSEQ, CAPB)
    in_maps = []
    for core in range(NCORES):
        m = dict(shared)
        b0 = core * NSEQ
        m["x"] = np.ascontiguousarray(np.asarray(inputs["x"], np.float32)[b0:b0 + NSEQ]).reshape(NSEQ * S, D)
        m["c"] = np.ascontiguousarray(np.asarray(inputs["c"], np.float32)[b0:b0 + NSEQ])
        in_maps.append(m)
    res = run_bass_kernel_spmd(nc, in_maps, core_ids=list(range(NCORES)))
    out = np.concatenate([np.asarray(r["out"]).reshape(NSEQ, S, D) for r in res.results], axis=0)
    return out.astype(np.float32)
```
